# Optimizing a Trainium2 kernel written in Bass

```python
import math
import jax
import jax.numpy as jnp
from jax import lax
import numpy as np

D_MODEL = 1024
BATCH = 8
SEQ = 4096
DEPTH = 2

N_HEADS = 8
N_KV_HEADS = 2
HEAD_DIM = 64
Q_PER_KV = N_HEADS // N_KV_HEADS
ATTN_WIDTH = N_HEADS * HEAD_DIM
KV_WIDTH = N_KV_HEADS * HEAD_DIM
N_BRANCH = 3
CMP_LEN = 32
CMP_STRIDE = 16
CMP_HIDDEN = 128
SEL_LEN = 64
SEL_TOPK = 16
N_LOCAL_SEL = 2
WINDOW = 512
Q_BLOCK = 64
CONV_WIDTH = D_MODEL // 2
CONV_KERNEL = 31
MIX_WIDTH = ATTN_WIDTH + CONV_WIDTH
IN_WIDTH = ATTN_WIDTH + 6 * KV_WIDTH + N_HEADS * N_BRANCH + 2 * CONV_WIDTH
ROPE_THETA = 500000.0
ROPE_DIM = HEAD_DIM // 4
D_FF = 7 * D_MODEL // 2
N_EXPERTS = 8
TOP_K = 2
MOE_BLOCK = 128
EPS = 1e-6

kernel_name = "hybrid_nsa_conformer_moe"


def rms_norm(x, g):
    xf = x.astype(jnp.float32)
    y = xf * lax.rsqrt(jnp.mean(xf * xf, axis=-1, keepdims=True) + EPS)
    return (y * g.astype(jnp.float32)).astype(x.dtype)


def layer_norm(x, g, b):
    xf = x.astype(jnp.float32)
    xc = xf - jnp.mean(xf, axis=-1, keepdims=True)
    y = xc * lax.rsqrt(jnp.mean(xc * xc, axis=-1, keepdims=True) + EPS)
    return (y * g.astype(jnp.float32) + b.astype(jnp.float32)).astype(x.dtype)


def masked_softmax(s, mask):
    s = jnp.where(mask, s.astype(jnp.float32), -jnp.inf)
    m = jnp.max(s, axis=-1, keepdims=True)
    m = jnp.where(jnp.isfinite(m), m, 0.0)
    p = jnp.exp(s - m)
    return p / jnp.maximum(jnp.sum(p, axis=-1, keepdims=True), jnp.finfo(jnp.float32).tiny)


def rope_partial(x, pos):
    half = ROPE_DIM // 2
    inv_freq = ROPE_THETA ** (-2.0 * jnp.arange(half, dtype=jnp.float32) / ROPE_DIM)
    ang = pos.astype(jnp.float32)[..., None, None] * inv_freq
    cos, sin = jnp.cos(ang), jnp.sin(ang)
    xr = x[..., :ROPE_DIM].astype(jnp.float32)
    x1, x2 = xr[..., :half], xr[..., half:]
    rot = jnp.concatenate([x1 * cos - x2 * sin, x2 * cos + x1 * sin], axis=-1).astype(x.dtype)
    return jnp.concatenate([rot, x[..., ROPE_DIM:]], axis=-1)


def swiglu(x, wg, wu, wd):
    return (jax.nn.silu(x @ wg) * (x @ wu)) @ wd


def in_proj_split_points():
    sizes = [ATTN_WIDTH] + [KV_WIDTH] * 6 + [N_HEADS * N_BRANCH, 2 * CONV_WIDTH]
    return [int(v) for v in np.cumsum(sizes)[:-1]]


def n_cmp_blocks(s):
    return (s - CMP_LEN) // CMP_STRIDE + 1


def cmp_block_ends(s):
    return np.arange(n_cmp_blocks(s)) * CMP_STRIDE + CMP_LEN - 1


def selection_map(s):
    c0 = np.arange(n_cmp_blocks(s)) * CMP_STRIDE
    s0 = np.arange(s // SEL_LEN) * SEL_LEN
    ov = np.minimum(c0[:, None] + CMP_LEN, s0[None, :] + SEL_LEN) - np.maximum(c0[:, None], s0[None, :])
    return jnp.asarray(np.clip(ov, 0, None) / CMP_LEN, dtype=jnp.float32)


def compress_blocks(kv, pos_emb, w1, w2):
    b, s = kv.shape[0], kv.shape[1]
    n_cmp = n_cmp_blocks(s)
    idx = np.arange(n_cmp)[:, None] * CMP_STRIDE + np.arange(CMP_LEN)[None, :]
    blocks = kv[:, idx] + pos_emb[None, None, :, None, :]
    blocks = blocks.transpose(0, 1, 3, 2, 4).reshape(b, n_cmp, N_KV_HEADS, CMP_LEN * HEAD_DIM)
    hid = jax.nn.gelu(jnp.einsum('bnkf,fh->bnkh', blocks, w1))
    return jnp.einsum('bnkh,hd->bnkd', hid, w2)


def nsa_attention(q, k_cmp, v_cmp, k_slc, v_slc, k_win, v_win, gates):
    b, s = q.shape[0], q.shape[1]
    n_sel = s // SEL_LEN
    top_n = min(SEL_TOPK, n_sel)
    scale = HEAD_DIM ** -0.5
    q = q.reshape(b, s, N_KV_HEADS, Q_PER_KV, HEAD_DIM)
    gates = gates.reshape(b, s, N_KV_HEADS, Q_PER_KV, N_BRANCH)
    cmp_end = jnp.asarray(cmp_block_ends(s))
    sel_map = selection_map(s)
    k_blk = k_slc.reshape(b, n_sel, SEL_LEN, N_KV_HEADS, HEAD_DIM).transpose(0, 3, 1, 2, 4)
    v_blk = v_slc.reshape(b, n_sel, SEL_LEN, N_KV_HEADS, HEAD_DIM).transpose(0, 3, 1, 2, 4)
    pad = ((0, 0), (WINDOW, 0), (0, 0), (0, 0))
    k_wpad = jnp.pad(k_win, pad)
    v_wpad = jnp.pad(v_win, pad)
    b_ix = jnp.arange(b)[:, None, None, None]
    h_ix = jnp.arange(N_KV_HEADS)[None, None, :, None]
    blk_ids = jnp.arange(n_sel)

    def query_block(c):
        t0 = c * Q_BLOCK
        t = t0 + jnp.arange(Q_BLOCK)
        qc = lax.dynamic_slice_in_dim(q, t0, Q_BLOCK, axis=1)
        gc = lax.dynamic_slice_in_dim(gates, t0, Q_BLOCK, axis=1)
        s_c = jnp.einsum('bqkgd,bnkd->bqkgn', qc, k_cmp) * scale
        mask_c = (cmp_end[None, :] <= t[:, None])[None, :, None, None, :]
        p_c = masked_softmax(s_c, mask_c)
        o_c = jnp.einsum('bqkgn,bnkd->bqkgd', p_c.astype(v_cmp.dtype), v_cmp)
        imp = jnp.einsum('bqkgn,nj->bqkj', p_c, sel_map)
        cur = (t // SEL_LEN)[:, None]
        causal_blk = blk_ids[None, :] <= cur
        forced = (blk_ids[None, :] == 0) | (causal_blk & (blk_ids[None, :] > cur - N_LOCAL_SEL))
        score = jnp.where(forced[None, :, None, :], jnp.inf,
                          jnp.where(causal_blk[None, :, None, :], imp, -jnp.inf))
        _, sel_idx = lax.top_k(score, top_n)
        k_sel = k_blk[b_ix, h_ix, sel_idx].reshape(b, Q_BLOCK, N_KV_HEADS, top_n * SEL_LEN, HEAD_DIM)
        v_sel = v_blk[b_ix, h_ix, sel_idx].reshape(b, Q_BLOCK, N_KV_HEADS, top_n * SEL_LEN, HEAD_DIM)
        tok = (sel_idx[..., None] * SEL_LEN + jnp.arange(SEL_LEN)).reshape(
            b, Q_BLOCK, N_KV_HEADS, 1, top_n * SEL_LEN)
        mask_s = tok <= t[None, :, None, None, None]
        s_s = jnp.einsum('bqkgd,bqkmd->bqkgm', qc, k_sel) * scale
        p_s = masked_softmax(s_s, mask_s)
        o_s = jnp.einsum('bqkgm,bqkmd->bqkgd', p_s.astype(v_sel.dtype), v_sel)
        k_w = lax.dynamic_slice_in_dim(k_wpad, t0, WINDOW + Q_BLOCK, axis=1)
        v_w = lax.dynamic_slice_in_dim(v_wpad, t0, WINDOW + Q_BLOCK, axis=1)
        src = t0 - WINDOW + jnp.arange(WINDOW + Q_BLOCK)
        mask_w = ((src[None, :] >= 0) & (src[None, :] <= t[:, None])
                  & (src[None, :] > t[:, None] - WINDOW))[None, :, None, None, :]
        s_w = jnp.einsum('bqkgd,bwkd->bqkgw', qc, k_w) * scale
        p_w = masked_softmax(s_w, mask_w)
        o_w = jnp.einsum('bqkgw,bwkd->bqkgd', p_w.astype(v_w.dtype), v_w)
        return gc[..., 0:1] * o_c + gc[..., 1:2] * o_s + gc[..., 2:3] * o_w

    out = lax.map(query_block, jnp.arange(s // Q_BLOCK))
    return out.transpose(1, 0, 2, 3, 4, 5).reshape(b, s, ATTN_WIDTH)


def conformer_conv(u, conv_w, conv_b, ln_g, ln_b):
    a, g = jnp.split(u, 2, axis=-1)
    h = a * jax.nn.sigmoid(g)
    h = jnp.pad(h, ((0, 0), (CONV_KERNEL - 1, 0), (0, 0)))
    y = lax.conv_general_dilated(h, conv_w[:, None, :], window_strides=(1,), padding='VALID',
                                 dimension_numbers=('NWC', 'WIO', 'NWC'),
                                 feature_group_count=CONV_WIDTH)
    y = y + conv_b
    return jax.nn.silu(layer_norm(y, ln_g, ln_b))


def moe_swiglu(h, router, wg, wu, wd):
    b, s, d = h.shape
    n_tok = b * s
    xt = h.reshape(n_tok, d)
    logits = (xt @ router).astype(jnp.float32)
    top_logit, top_e = lax.top_k(logits, TOP_K)
    gate = jax.nn.softmax(top_logit, axis=-1)
    n_asg = n_tok * TOP_K
    e_flat = top_e.reshape(n_asg)
    tok_flat = jnp.broadcast_to(jnp.arange(n_tok)[:, None], (n_tok, TOP_K)).reshape(n_asg)
    g_flat = gate.reshape(n_asg)
    order = jnp.argsort(e_flat)
    e_s, tok_s, g_s = e_flat[order], tok_flat[order], g_flat[order]
    counts = jnp.bincount(e_flat, length=N_EXPERTS)
    padded = (counts + MOE_BLOCK - 1) // MOE_BLOCK * MOE_BLOCK
    start = jnp.cumsum(counts) - counts
    pend = jnp.cumsum(padded)
    pstart = pend - padded
    dest = pstart[e_s] + jnp.arange(n_asg) - start[e_s]
    n_blocks = -(-n_asg // MOE_BLOCK) + N_EXPERTS
    n_slots = n_blocks * MOE_BLOCK
    slot_tok = jnp.full((n_slots,), n_tok, jnp.int32).at[dest].set(tok_s.astype(jnp.int32))
    slot_gate = jnp.zeros((n_slots,), jnp.float32).at[dest].set(g_s)
    blk_e = jnp.clip(jnp.searchsorted(pend, jnp.arange(n_blocks) * MOE_BLOCK, side='right'),
                     0, N_EXPERTS - 1)
    x_pad = jnp.concatenate([xt, jnp.zeros((1, d), xt.dtype)], axis=0)
    xs = x_pad[slot_tok].reshape(n_blocks, MOE_BLOCK, d)

    def expert_block(args):
        xb, e = args
        return swiglu(xb, wg[e], wu[e], wd[e])

    ys = lax.map(expert_block, (xs, blk_e)).reshape(n_slots, d)
    ys = ys * slot_gate[:, None].astype(ys.dtype)
    out = jax.ops.segment_sum(ys, slot_tok, num_segments=n_tok + 1)[:n_tok]
    return out.reshape(b, s, d)


def setup_inputs(seed: int = 0) -> dict:
    key = jax.random.key(seed)
    keys = list(jax.random.split(key, 32))

    def nrm(i, shape, scale):
        return scale * jax.random.normal(keys[i], shape, jnp.float32)

    n_dense = (DEPTH + 1) // 2
    n_moe = DEPTH // 2
    x = nrm(0, (BATCH, SEQ, D_MODEL), 1.0)
    offsets = jax.random.randint(keys[1], (BATCH, 1), 0, 2048, dtype=jnp.int32)
    positions = offsets + jnp.arange(SEQ, dtype=jnp.int32)[None, :]
    return {
        "x": x,
        "positions": positions,
        "attn_norm_g": 1.0 + nrm(2, (DEPTH, D_MODEL), 0.02),
        "ffn_norm_g": 1.0 + nrm(3, (DEPTH, D_MODEL), 0.02),
        "w_in": nrm(4, (DEPTH, D_MODEL, IN_WIDTH), D_MODEL ** -0.5),
        "w_out": nrm(5, (DEPTH, MIX_WIDTH, D_MODEL), MIX_WIDTH ** -0.5),
        "q_norm_g": 1.0 + nrm(6, (DEPTH, HEAD_DIM), 0.02),
        "k_norm_g": 1.0 + nrm(7, (DEPTH, N_BRANCH, HEAD_DIM), 0.02),
        "cmp_pos_k": nrm(8, (DEPTH, CMP_LEN, HEAD_DIM), 0.1),
        "cmp_w1_k": nrm(9, (DEPTH, CMP_LEN * HEAD_DIM, CMP_HIDDEN), (CMP_LEN * HEAD_DIM) ** -0.5),
        "cmp_w2_k": nrm(10, (DEPTH, CMP_HIDDEN, HEAD_DIM), CMP_HIDDEN ** -0.5),
        "cmp_pos_v": nrm(11, (DEPTH, CMP_LEN, HEAD_DIM), 0.1),
        "cmp_w1_v": nrm(12, (DEPTH, CMP_LEN * HEAD_DIM, CMP_HIDDEN), (CMP_LEN * HEAD_DIM) ** -0.5),
        "cmp_w2_v": nrm(13, (DEPTH, CMP_HIDDEN, HEAD_DIM), CMP_HIDDEN ** -0.5),
        "conv_w": nrm(14, (DEPTH, CONV_KERNEL, CONV_WIDTH), CONV_KERNEL ** -0.5),
        "conv_b": nrm(15, (DEPTH, CONV_WIDTH), 0.02),
        "conv_ln_g": 1.0 + nrm(16, (DEPTH, CONV_WIDTH), 0.02),
        "conv_ln_b": nrm(17, (DEPTH, CONV_WIDTH), 0.02),
        "ffn_w_gate": nrm(18, (n_dense, D_MODEL, D_FF), D_MODEL ** -0.5),
        "ffn_w_up": nrm(19, (n_dense, D_MODEL, D_FF), D_MODEL ** -0.5),
        "ffn_w_down": nrm(20, (n_dense, D_FF, D_MODEL), D_FF ** -0.5),
        "moe_router": nrm(21, (n_moe, D_MODEL, N_EXPERTS), D_MODEL ** -0.5),
        "moe_w_gate": nrm(22, (n_moe, N_EXPERTS, D_MODEL, D_FF), D_MODEL ** -0.5),
        "moe_w_up": nrm(23, (n_moe, N_EXPERTS, D_MODEL, D_FF), D_MODEL ** -0.5),
        "moe_w_down": nrm(24, (n_moe, N_EXPERTS, D_FF, D_MODEL), D_FF ** -0.5),
    }


def reference(x, positions, attn_norm_g, ffn_norm_g, w_in, w_out, q_norm_g, k_norm_g,
              cmp_pos_k, cmp_w1_k, cmp_w2_k, cmp_pos_v, cmp_w1_v, cmp_w2_v,
              conv_w, conv_b, conv_ln_g, conv_ln_b,
              ffn_w_gate, ffn_w_up, ffn_w_down,
              moe_router, moe_w_gate, moe_w_up, moe_w_down):
    b, s, _ = x.shape
    pos_cmp = positions[:, cmp_block_ends(s)]
    split_points = in_proj_split_points()
    for layer in range(DEPTH):
        h = rms_norm(x, attn_norm_g[layer])
        proj = jnp.einsum('bsd,de->bse', h, w_in[layer])
        q, k_c, v_c, k_s, v_s, k_w, v_w, g_logit, u = jnp.split(proj, split_points, axis=-1)
        q = rope_partial(rms_norm(q.reshape(b, s, N_HEADS, HEAD_DIM), q_norm_g[layer]), positions)
        kv_shape = (b, s, N_KV_HEADS, HEAD_DIM)
        k_cmp = compress_blocks(k_c.reshape(kv_shape), cmp_pos_k[layer], cmp_w1_k[layer], cmp_w2_k[layer])
        k_cmp = rope_partial(rms_norm(k_cmp, k_norm_g[layer, 0]), pos_cmp)
        v_cmp = compress_blocks(v_c.reshape(kv_shape), cmp_pos_v[layer], cmp_w1_v[layer], cmp_w2_v[layer])
        k_slc = rope_partial(rms_norm(k_s.reshape(kv_shape), k_norm_g[layer, 1]), positions)
        k_win = rope_partial(rms_norm(k_w.reshape(kv_shape), k_norm_g[layer, 2]), positions)
        gates = jax.nn.sigmoid(g_logit.astype(jnp.float32)).astype(x.dtype).reshape(b, s, N_HEADS, N_BRANCH)
        attn_out = nsa_attention(q, k_cmp, v_cmp, k_slc, v_s.reshape(kv_shape),
                                 k_win, v_w.reshape(kv_shape), gates)
        conv_out = conformer_conv(u, conv_w[layer], conv_b[layer], conv_ln_g[layer], conv_ln_b[layer])
        mixed = jnp.concatenate([attn_out, conv_out], axis=-1)
        x = x + jnp.einsum('bsm,md->bsd', mixed, w_out[layer])
        h = rms_norm(x, ffn_norm_g[layer])
        if layer % 2 == 0:
            i = layer // 2
            x = x + swiglu(h, ffn_w_gate[i], ffn_w_up[i], ffn_w_down[i])
        else:
            i = layer // 2
            x = x + moe_swiglu(h, moe_router[i], moe_w_gate[i], moe_w_up[i], moe_w_down[i])
    return x
```

```python
import math
import numpy as np
import concourse.bass as bass
import concourse.mybir as mybir
from concourse.bass_utils import run_bass_kernel_spmd

F32 = mybir.dt.float32
BF16 = mybir.dt.bfloat16
I32 = mybir.dt.int32
AF = mybir.ActivationFunctionType
ALU = mybir.AluOpType
AX = mybir.AxisListType

S = 4096
D = 1024
NT = S // 128
NST = S // 512
DFF = 3584
NFT = DFF // 128
NE = 8
EPS = 1e-6
NEG = -30000.0
ENGS = ('pe', 'act', 'dve', 'pool', 'sp')
REGS = None


class Res:
    __slots__ = ('name', 'w', 'rs', 'excl')

    def __init__(self, name='', excl=False):
        self.name = name
        self.w = None
        self.rs = {}
        self.excl = excl


class Sched:
    NEAR = 4

    def __init__(self, n_dma_sems=32):
        self.streams = {e: [] for e in ENGS}
        self.cnt = {e: 0 for e in ENGS}
        self.seen = {e: {} for e in ENGS}
        self.dcnt = [0] * n_dma_sems
        self.dnext = 0
        self.final = []
        self.serialize_dma = False
        self.inflight = {}

    def op(self, eng, fn, reads=(), writes=()):
        idx = self.cnt[eng] + 1
        me = ('e', eng)
        deps = {}
        same_raw = 0
        for r in reads:
            if r.w is not None:
                k, v = r.w
                if k == me:
                    if v > same_raw:
                        same_raw = v
                elif deps.get(k, 0) < v:
                    deps[k] = v
            if r.excl:
                for k, v in r.rs.items():
                    if k != me and deps.get(k, 0) < v:
                        deps[k] = v
        for r in writes:
            if r.w is not None:
                k, v = r.w
                if k != me and deps.get(k, 0) < v:
                    deps[k] = v
            for k, v in r.rs.items():
                if k != me and deps.get(k, 0) < v:
                    deps[k] = v
        waits = []
        if same_raw and idx - same_raw <= self.NEAR:
            waits.append((me, same_raw))
        sn = self.seen[eng]
        for k, v in deps.items():
            if sn.get(k, 0) < v:
                sn[k] = v
                waits.append((k, v))
        self.streams[eng].append((waits, fn, me))
        self.cnt[eng] = idx
        tok = (me, idx)
        for r in reads:
            if r.rs.get(me, 0) < idx:
                r.rs[me] = idx
        for r in writes:
            r.w = tok
            r.rs = {}
        return tok

    def dma(self, q, fn, reads=(), writes=(), final=False, nd=128):
        k = self.dnext
        self.dnext = (self.dnext + 1) % len(self.dcnt)
        v = self.dcnt[k] + 16
        dk = ('d', k)
        deps = {}
        if self.dcnt[k] > 0:
            deps[dk] = self.dcnt[k]
        fl = self.inflight.setdefault(q, [])
        cap_n = 6 if q == 'pool' else 12
        cap_d = 3000 if q == 'pool' else 1 << 30
        while fl and (len(fl) >= cap_n or sum(x[2] for x in fl) + nd > cap_d):
            ok, ov, _ = fl.pop(0)
            if deps.get(ok, 0) < ov:
                deps[ok] = ov
        fl.append((dk, v, nd))
        for r in reads:
            if r.w is not None:
                k2, v2 = r.w
                if deps.get(k2, 0) < v2:
                    deps[k2] = v2
        for r in writes:
            if r.w is not None:
                k2, v2 = r.w
                if deps.get(k2, 0) < v2:
                    deps[k2] = v2
            for k2, v2 in r.rs.items():
                if deps.get(k2, 0) < v2:
                    deps[k2] = v2
        sn = self.seen[q]
        waits = []
        for k2, v2 in deps.items():
            if sn.get(k2, 0) < v2:
                sn[k2] = v2
                waits.append((k2, v2))
        self.streams[q].append((waits, fn, dk))
        self.dcnt[k] = v
        tok = (dk, v)
        for r in reads:
            if r.rs.get(dk, 0) < v:
                r.rs[dk] = v
        for r in writes:
            r.w = tok
            r.rs = {}
        if final:
            self.final.append(tok)
        if self.serialize_dma:
            sn[dk] = v
            self.streams[q].append(([(dk, v)], None, None))
        return tok

    def barrier(self):
        snap_e = dict(self.cnt)
        snap_d = list(self.dcnt)
        for e in ENGS:
            waits = []
            sn = self.seen[e]
            for e2 in ENGS:
                k = ('e', e2)
                if e2 != e and snap_e[e2] > 0 and sn.get(k, 0) < snap_e[e2]:
                    sn[k] = snap_e[e2]
                    waits.append((k, snap_e[e2]))
            for i, v in enumerate(snap_d):
                k = ('d', i)
                if v > 0 and sn.get(k, 0) < v:
                    sn[k] = v
                    waits.append((k, v))
            self.streams[e].append((waits, None, None))

    def emit(self, nc, stack):
        esem = {e: stack.enter_context(nc.semaphore('es_' + e)) for e in ENGS}
        dsem = [stack.enter_context(nc.semaphore('ds_%d' % i)) for i in range(len(self.dcnt))]

        def semof(k):
            return esem[k[1]] if k[0] == 'e' else dsem[k[1]]

        fin = {}
        for k, v in self.final:
            if fin.get(k, 0) < v:
                fin[k] = v

        def make(name):
            def body(e):
                if name == 'pool':
                    global REGS
                    REGS = {'zero': e.to_reg(0.0), 'neg': e.to_reg(NEG)}
                for waits, fn, sig in self.streams[name]:
                    for (k, v) in waits:
                        e.wait_ge(semof(k), v)
                    if fn is None:
                        continue
                    ins = fn(e)
                    if sig[0] == 'e':
                        ins.then_inc(esem[sig[1]], 1)
                    else:
                        ins.then_inc(dsem[sig[1]], 16)
                if name == 'sp':
                    for k, v in fin.items():
                        e.wait_ge(semof(k), v)
            return body

        with nc.Block() as block:
            block.tensor(make('pe'))
            block.scalar(make('act'))
            block.vector(make('dve'))
            block.gpsimd(make('pool'))
            block.sync(make('sp'))


def _dtsize(dt):
    return {F32: 4, BF16: 2, I32: 4}[dt]


class Arena:
    def __init__(self, ap_u8, size):
        self.ap = ap_u8
        self.size = size
        self.off = 0

    def alloc(self, free_shape, dt):
        n = 1
        for s in free_shape:
            n *= s
        nb = n * _dtsize(dt)
        nb_al = (nb + 63) // 64 * 64
        assert self.off + nb_al <= self.size, ("SBUF arena overflow", self.off, nb_al, self.size)
        a = self.ap[:, self.off:self.off + nb].bitcast(dt)
        self.off += nb_al
        if len(free_shape) > 1:
            names = ' '.join('a%d' % i for i in range(len(free_shape)))
            kw = {'a%d' % i: free_shape[i] for i in range(1, len(free_shape))}
            a = a.rearrange('p (%s) -> p %s' % (names, names), **kw)
        return a


def _ROPE_INV_FREQ():
    return [float(np.float32(500000.0) ** np.float32(-2.0 * i / 16.0)) for i in range(8)]


W_BLOCKS = [
    (0, 0, 512),
    (512, 768, 128),
    (640, 1024, 128),
    (768, 896, 128),
    (896, 1152, 128),
    (1024, 1280, 24),
    (1048, 512, 128),
    (1176, 640, 128),
    (1304, 1304, 1024),
]
WIN = 2328


class Builder:
    def __init__(self, nc, stack, dbg=None):
        self.bg_tasks = []
        self.final_out = None
        self.nc = nc
        self.stack = stack
        self.sc = Sched()
        self.dbg = dbg or {}
        self.dbg_out = {}
        arena_t = stack.enter_context(nc.sbuf_tensor('arena', [128, 175 * 1024], mybir.dt.uint8))
        self.arena = Arena(arena_t, 175 * 1024)
        self.psb = []
        self.psr = []
        for i in range(8):
            t = stack.enter_context(nc.psum_tensor('psb%d' % i, [128, 512], F32))
            self.psb.append(t)
            self.psr.append(Res('ps%d' % i, excl=True))

    def din(self, name, shape, dt=F32):
        return self.nc.dram_tensor(name, list(shape), dt, kind='ExternalInput').ap()

    def dout(self, name, shape, dt=F32):
        return self.nc.dram_tensor(name, list(shape), dt, kind='ExternalOutput').ap()

    def dscr(self, name, shape, dt=F32):
        return self.nc.dram_tensor(name, list(shape), dt).ap()

    def dump(self, name, ap, res_list, shape, dt=F32):
        o = self.dout('dbg_' + name, shape, dt)
        self.dbg_out['dbg_' + name] = (shape, dt)
        self.sc.dma('sp', lambda e, o=o, ap=ap: e.dma_start(out=o, in_=ap), reads=res_list, writes=[Res()],
                    final=True)

    def mm(self, out, lhsT, rhs, start, stop, reads, writes):
        self.sc.op('pe', lambda e: e.matmul(out, lhsT, rhs, start=start, stop=stop), reads, writes)

    def tr(self, out, in_, ident, reads, writes):
        self.sc.op('pe', lambda e: e.transpose(out, in_, ident), reads, writes)

    def act(self, out, in_, func, reads, writes, **kw):
        self.sc.op('act', lambda e: e.activation(out, in_, func, **kw), reads, writes)

    def ts(self, eng, out, in0, s1, s2, op0, op1, reads, writes):
        if op1 is None:
            self.sc.op(eng, lambda e: e.tensor_scalar(out, in0, s1, None, op0), reads, writes)
        else:
            self.sc.op(eng, lambda e: e.tensor_scalar(out, in0, s1, s2, op0, op1), reads, writes)

    def tt(self, eng, out, in0, in1, op, reads, writes):
        self.sc.op(eng, lambda e: e.tensor_tensor(out, in0, in1, op), reads, writes)

    def stt(self, out, in0, scalar, in1, op0, op1, reads, writes):
        self.sc.op('dve', lambda e: e.scalar_tensor_tensor(out, in0, scalar, in1, op0, op1), reads, writes)

    def cp(self, eng, out, in_, reads, writes):
        if eng == 'act':
            self.sc.op('act', lambda e: e.copy(out, in_), reads, writes)
        else:
            self.sc.op(eng, lambda e: e.tensor_copy(out, in_), reads, writes)

    def memset(self, eng, ap, val, writes):
        self.sc.op(eng, lambda e: e.memset(ap, val), (), writes)

    def dma(self, q, out, in_, reads, writes, final=False, nd=128, **kw):
        return self.sc.dma(q, lambda e: e.dma_start(out=out, in_=in_, **kw), reads, writes, final=final, nd=nd)

    def psum_bf(self, i):
        return self.psb[i][:, :].bitcast(BF16)

    def phase0(self, inp):
        b, A = self, self.arena
        self.ones_f = A.alloc([128], F32)
        self.ident_f = A.alloc([128], F32)
        self.ident_b = A.alloc([128], BF16)
        r1, r2, r3 = Res(), Res(), Res()
        b.memset('pool', self.ones_f, 1.0, [r1])
        ones_f, ident_f = self.ones_f, self.ident_f
        b.sc.op('pool', lambda e: e.affine_select(out=ident_f, in_=ones_f, pattern=[[-1, 128]],
                                                  compare_op=ALU.is_equal, fill=0.0 if REGS is None else REGS['zero'], base=0,
                                                  channel_multiplier=1), [r1], [r2])
        b.cp('pool', self.ident_b, self.ident_f, [r2], [r3])
        self.r_ident = r3
        self.r_identf = r2

        self.causal01 = A.alloc([128], BF16)
        self.low01 = A.alloc([128], BF16)
        self.ones_b = A.alloc([128], BF16)
        self.r_masks = Res()
        b.memset('pool', self.ones_b, 1.0, [self.r_masks])
        ones_b, causal01, low01 = self.ones_b, self.causal01, self.low01
        self.sc.op('pool', lambda e: e.affine_select(out=causal01, in_=ones_b, pattern=[[1, 128]],
                   compare_op=ALU.is_ge, fill=0.0 if REGS is None else REGS['zero'], base=0, channel_multiplier=-1), [self.r_masks], [self.r_masks])
        self.sc.op('pool', lambda e: e.affine_select(out=low01, in_=ones_b, pattern=[[-1, 128]],
                   compare_op=ALU.is_ge, fill=0.0 if REGS is None else REGS['zero'], base=-1, channel_multiplier=1), [self.r_masks], [self.r_masks])
        self.zeros_b4 = A.alloc([4, 128], BF16)
        self.causalN = A.alloc([4, 128], BF16)
        self.lowN = A.alloc([4, 128], BF16)
        b.memset('pool', self.zeros_b4, 0.0, [self.r_masks])
        zeros_b4, causalN, lowN = self.zeros_b4, self.causalN, self.lowN
        self.sc.op('pool', lambda e: e.affine_select(out=causalN, in_=zeros_b4, pattern=[[0, 4], [1, 128]],
                   compare_op=ALU.is_ge, fill=NEG if REGS is None else REGS['neg'], base=0,
                   channel_multiplier=-1), [self.r_masks], [self.r_masks])
        self.sc.op('pool', lambda e: e.affine_select(out=lowN, in_=zeros_b4, pattern=[[0, 4], [-1, 128]],
                   compare_op=ALU.is_ge, fill=NEG if REGS is None else REGS['neg'], base=-1,
                   channel_multiplier=1), [self.r_masks], [self.r_masks])
        self.sinT = A.alloc([34, 8], F32)
        self.cosT = A.alloc([34, 8], F32)
        mark0 = A.off
        pos_i = A.alloc([34], I32)
        rp = Res()
        b.dma('sp', pos_i[:, 0:32], inp['pos_t'], [], [rp])
        b.dma('sp', pos_i[:, 32:34], inp['posc_t'], [], [rp])
        pos_f = A.alloc([34], F32)
        rpf = Res()
        b.cp('dve', pos_f, pos_i, [rp], [rpf])
        invf = A.alloc([8], F32)
        rif = Res()
        for i, v in enumerate(_ROPE_INV_FREQ()):
            b.memset('pool', invf[:, i:i + 1], v, [rif])
        ang = A.alloc([34, 8], F32)
        angc = A.alloc([34, 8], F32)
        ra, rac = Res(), Res()
        b.tt('dve', ang, pos_f.unsqueeze(2).to_broadcast([128, 34, 8]),
             invf.unsqueeze(1).to_broadcast([128, 34, 8]), ALU.mult, [rpf, rif], [ra])
        b.ts('dve', angc, ang, math.pi / 2, None, ALU.add, None, [ra], [rac])
        self.r_rope = Res()
        MAGIC = 12582912.0
        TWO_PI = 2.0 * math.pi
        for src, rsrc, dst in ((ang, ra, self.sinT), (angc, rac, self.cosT)):
            u = A.alloc([34, 8], F32)
            k2 = A.alloc([34, 8], F32)
            ru, rk = Res(), Res()
            b.ts('dve', u, src, 1.0 / TWO_PI, MAGIC, ALU.mult, ALU.add, [rsrc], [ru])
            b.ts('dve', k2, u, -MAGIC, TWO_PI, ALU.add, ALU.mult, [ru], [rk])
            b.tt('dve', u, src, k2, ALU.subtract, [rsrc, rk], [ru])
            b.ts('dve', k2, u, -3.1415, 3.1415, ALU.max, ALU.min, [ru], [rk])
            b.act(dst, k2, AF.Sin, [rk], [self.r_rope])
        b.sc.barrier()
        A.off = mark0

    def load_layer_small(self, inp, l):
        b, A = self, self.arena
        P = {}
        r = Res()
        P['r'] = r
        P['gin'] = A.alloc([8], F32)
        b.dma('sp', P['gin'], inp['gin_t'][l], [], [r])
        P['gffn'] = A.alloc([8], F32)
        b.dma('sp', P['gffn'], inp['gffn_t'][l], [], [r])
        gq = A.alloc([64], F32)
        gk = A.alloc([3, 64], F32)
        r0 = Res()
        b.dma('sp', gq, inp['q_norm_g'][l].partition_broadcast(128), [], [r0])
        b.dma('sp', gk, inp['k_norm_g'][l].rearrange('a b -> (a b)').partition_broadcast(128)
              .rearrange('p (a b) -> p a b', b=64), [], [r0])
        P['gk'] = gk
        gqk = A.alloc([12, 64], F32)
        b.ts('dve', gqk[:, 0:8, :], gq.unsqueeze(1).to_broadcast([128, 8, 64]), 0.125, None, ALU.mult, None,
             [r0], [r])
        b.cp('dve', gqk[:, 8:10, :], gk[:, 1:2, :].to_broadcast([128, 2, 64]), [r0], [r])
        b.cp('dve', gqk[:, 10:12, :], gk[:, 2:3, :].to_broadcast([128, 2, 64]), [r0], [r])
        P['gqk'] = gqk
        return P

    def phaseA(self, inp, l, xsrc, P, st_list=None):
        b, A, sc = self, self.arena, self.sc
        self.KE = [A.alloc([S], BF16) for _ in range(2)]
        self.r_E = Res()
        for kh in range(2):
            reg = self.KE[kh][64:128, :] if kh == 0 else self.KE[kh][0:64, :]
            b.memset('pool', reg, 1.0, [self.r_E])
            sc_ = self.sc
            sc_.op('pool', lambda e, reg=reg: e.affine_select(out=reg, in_=reg, pattern=[[1, S]],
                   compare_op=ALU.is_ge, fill=0.0 if REGS is None else REGS['zero'], base=0, channel_multiplier=-64), [self.r_E], [self.r_E])
            sc_.op('pool', lambda e, reg=reg: e.affine_select(out=reg, in_=reg, pattern=[[-1, S]],
                   compare_op=ALU.is_ge, fill=0.0 if REGS is None else REGS['zero'], base=63, channel_multiplier=64), [self.r_E], [self.r_E])
        self.QT = A.alloc([4, S], BF16)
        self.KwT = A.alloc([S], BF16)
        self.VsA = A.alloc([NT, 2, 65], BF16)
        self.VwA = A.alloc([NT, 2, 65], BF16)
        self.gates = A.alloc([NT, 24], F32)
        self.KcmpT = A.alloc([256], BF16)
        self.VcA = A.alloc([2, 2, 128], BF16)
        self.markKc = A.off
        self.r_q = [Res() for _ in range(NT)]
        self.r_ks = [Res() for _ in range(NT)]
        self.r_kw = [Res() for _ in range(NT)]
        self.r_vs = [Res() for _ in range(NT)]
        self.r_vw = [Res() for _ in range(NT)]
        self.r_g = [Res() for _ in range(NT)]
        self.r_kc = [Res() for _ in range(NST)]
        self.r_vc = [Res() for _ in range(NST)]
        rones = Res()
        b.memset('pool', self.VsA[:, :, :, 64:65], 1.0, [rones])
        b.memset('pool', self.VwA[:, :, :, 64:65], 1.0, [rones])
        for t in range(NT):
            self.r_vs[t].w = rones.w
            self.r_vw[t].w = rones.w
        mark = A.off
        W = A.alloc([8, WIN], BF16)
        rW = [Res() for _ in W_BLOCKS]
        wsrc = inp['w_in'][l].rearrange('(k p) c -> p k c', p=128)
        for i, (do, so, n) in enumerate(W_BLOCKS):
            b.dma('pool', W[:, :, do:do + n], wsrc[:, :, so:so + n], [], [rW[i]], nd=1024)
        XB = [A.alloc([D], F32) for _ in range(2)]
        rXB = [Res() for _ in range(2)]
        XN = [A.alloc([D], BF16) for _ in range(2)]
        rXN = [Res() for _ in range(2)]
        HT = [A.alloc([8, 512], BF16) for _ in range(2)]
        rHT = [[Res() for _ in range(4)] for _ in range(2)]
        KCt = [A.alloc([512], BF16) for _ in range(2)]
        rKCt = [Res() for _ in range(2)]
        ss = [A.alloc([1], F32) for _ in range(2)]
        sd = [A.alloc([1], F32) for _ in range(2)]
        rstd = [A.alloc([1], F32) for _ in range(2)]
        rss = [Res() for _ in range(2)]
        rsd = [Res() for _ in range(2)]
        rrs = [Res() for _ in range(2)]

        SQ = A.alloc([12, 64], F32)
        rSQ = Res()
        ss12 = A.alloc([12], F32)
        sd12 = A.alloc([12], F32)
        rs12 = A.alloc([12], F32)
        r12a, r12b, r12c = Res(), Res(), Res()
        T1s = [A.alloc([12, 64], F32) for _ in range(2)]
        rT1s = [Res() for _ in range(2)]
        RA = A.alloc([4, 12, 8], F32)
        rRA = [Res() for _ in range(4)]
        QBs = [A.alloc([12, 64], BF16) for _ in range(2)]
        rQBs = [Res() for _ in range(2)]
        SIG = [A.alloc([512], BF16) for _ in range(4)]
        rSIG = [Res() for _ in range(4)]
        HGt = [A.alloc([512], BF16) for _ in range(2)]
        rHGt = [Res() for _ in range(2)]
        zt = A.alloc([32], BF16)
        rz = Res()
        b.memset('pool', zt, 0.0, [rz])
        hg = self.hg_scr
        for ct in range(4):
            b.dma('sp', hg[ct][:, 0:30], zt[:, 0:30], [rz], [Res()])
        ps, pr = self.psb, self.psr
        PT, PA, PB, PC, PQ, PF0, PF1 = 0, 1, 2, 3, 4, 5, 6
        psT = self.psum_bf(PT).rearrange('p (k t) -> p k t', t=128)[:, 0:8, :]
        psQ = self.psum_bf(PQ).rearrange('p (k t) -> p k t', t=128)[:, 0:6, :]
        gin_b = P['gin'].unsqueeze(2).to_broadcast([128, 8, 128])
        gqk = P['gqk']
        ident_b = self.ident_b
        fstate = {'fidx': 0, 'kci': 0}
        sts = list(st_list if st_list is not None else range(NST))
        tiles = [(st, j) for st in sts for j in range(4)]

        def stageX(st, j):
            hb = st % 2
            tt_ = st * 4 + j
            xb = tt_ % 2
            b.bg(1)
            b.dma('sp', XB[xb], xsrc[tt_ * 128:(tt_ + 1) * 128, :], [], [rXB[xb]])
            b.act(XN[xb], XB[xb], AF.Square, [rXB[xb]], [rXN[xb], rss[xb]], accum_out=ss[xb])
            b.act(sd[xb], ss[xb], AF.Sqrt, [rss[xb]], [rsd[xb]], scale=1.0 / D, bias=EPS)
            sc.op('dve', lambda e, o=rstd[xb], i=sd[xb]: e.reciprocal(o, i), [rsd[xb]], [rrs[xb]])
            b.act(XN[xb], XB[xb], AF.Copy, [rXB[xb], rrs[xb]], [rXN[xb]], scale=rstd[xb])
            for k in range(8):
                b.tr(psT[:, k, :], XN[xb][:, k * 128:(k + 1) * 128], ident_b, [rXN[xb], self.r_ident], [pr[PT]])
            b.tt('dve', HT[hb][:, :, j * 128:(j + 1) * 128], psT, gin_b, ALU.mult, [pr[PT], P['r']], [rHT[hb][j]])

        def stageYmm(st, j):
            hb = st % 2
            for bank, c0, c1, rw in ((PA, 0, 512, [rW[0]]), (PB, 512, 1024, rW[1:5]), (PC, 1024, 1048, [rW[5]])):
                for k in range(8):
                    b.mm(ps[bank][:, 0:c1 - c0], HT[hb][:, k, j * 128:(j + 1) * 128], W[:, k, c0:c1],
                         k == 0, k == 7, [rHT[hb][j]] + rw, [pr[bank]])

        def stageYpost(st, j):
            tt_ = st * 4 + j
            T1, rT1 = T1s[tt_ % 2], rT1s[tt_ % 2]
            b.act(SQ[:, 0:8, :], ps[PA][:, 0:512].rearrange('p (h d) -> p h d', d=64), AF.Square, [pr[PA]], [rSQ])
            b.act(SQ[:, 8:12, :], ps[PB][:, 0:256].rearrange('p (h d) -> p h d', d=64), AF.Square, [pr[PB]], [rSQ])
            sc.op('dve', lambda e: e.tensor_reduce(ss12, SQ, AX.X, ALU.add), [rSQ], [r12a])
            b.act(sd12, ss12, AF.Sqrt, [r12a], [r12b], scale=1.0 / 64, bias=EPS)
            sc.op('dve', lambda e: e.reciprocal(rs12, sd12), [r12b], [r12c])
            b.tt('dve', T1[:, 0:8, :], ps[PA][:, 0:512].rearrange('p (h d) -> p h d', d=64),
                 rs12[:, 0:8].unsqueeze(2).to_broadcast([128, 8, 64]), ALU.mult, [pr[PA], r12c], [rT1])
            b.tt('dve', T1[:, 8:12, :], ps[PB][:, 0:256].rearrange('p (h d) -> p h d', d=64),
                 rs12[:, 8:12].unsqueeze(2).to_broadcast([128, 4, 64]), ALU.mult, [pr[PB], r12c], [rT1])
            b.cp('dve', self.VsA[:, tt_, :, 0:64], ps[PB][:, 256:384].rearrange('p (h d) -> p h d', d=64),
                 [pr[PB]], [self.r_vs[tt_]])
            b.cp('dve', self.VwA[:, tt_, :, 0:64], ps[PB][:, 384:512].rearrange('p (h d) -> p h d', d=64),
                 [pr[PB]], [self.r_vw[tt_]])
            b.cp('act', self.gates[:, tt_, :], ps[PC][:, 0:24], [pr[PC]], [self.r_g[tt_]])

        def stageYpostB(st, j):
            tt_ = st * 4 + j
            QB, rQB = QBs[tt_ % 2], rQBs[tt_ % 2]
            T1, rT1 = T1s[tt_ % 2], rT1s[tt_ % 2]
            T2, rT2 = T1, rT1
            b.tt('pool', T2, T1, gqk, ALU.mult, [rT1, P['r']], [rT2])
            cosb = self.cosT[:, tt_:tt_ + 1, :].to_broadcast([128, 12, 8])
            sinb = self.sinT[:, tt_:tt_ + 1, :].to_broadcast([128, 12, 8])
            x1 = T2[:, :, 0:8]
            x2 = T2[:, :, 8:16]
            b.tt('dve', RA[:, 0], x1, cosb, ALU.mult, [rT2, self.r_rope], [rRA[0]])
            b.tt('pool', RA[:, 1], x2, sinb, ALU.mult, [rT2, self.r_rope], [rRA[1]])
            b.tt('dve', RA[:, 2], x2, cosb, ALU.mult, [rT2, self.r_rope], [rRA[2]])
            b.tt('pool', RA[:, 3], x1, sinb, ALU.mult, [rT2, self.r_rope], [rRA[3]])
            qdst = QB[:, 0:8, :].rearrange('p (pr hf) d -> p hf pr d', hf=2)

            def split(apx):
                return apx[:, 0:8].rearrange('p (hf pr) d -> p hf pr d', hf=2), apx[:, 8:12]
            r1q, r1k = split(RA[:, 0])
            s1q, s1k = split(RA[:, 1])
            r2q, r2k = split(RA[:, 2])
            s2q, s2k = split(RA[:, 3])
            b.tt('dve', qdst[:, :, :, 0:8], r1q, s1q, ALU.subtract, [rRA[0], rRA[1]], [rQB])
            b.tt('dve', QB[:, 8:12, 0:8], r1k, s1k, ALU.subtract, [rRA[0], rRA[1]], [rQB])
            b.tt('dve', qdst[:, :, :, 8:16], r2q, s2q, ALU.add, [rRA[2], rRA[3]], [rQB])
            b.tt('dve', QB[:, 8:12, 8:16], r2k, s2k, ALU.add, [rRA[2], rRA[3]], [rQB])
            t2q, t2k = split(T2)
            b.cp('pool', qdst[:, :, :, 16:64], t2q[:, :, :, 16:64], [rT2], [rQB])
            b.cp('pool', QB[:, 8:12, 16:64], t2k[:, :, 16:64], [rT2], [rQB])

        def stageYpost2(st, j):
            tt_ = st * 4 + j
            QB, rQB = QBs[tt_ % 2], rQBs[tt_ % 2]
            QBf = QB.rearrange('p h d -> p (h d)')
            for c in range(6):
                b.tr(psQ[:, c, :], QBf[:, c * 128:(c + 1) * 128], ident_b, [rQB, self.r_ident], [pr[PQ]])
            b.cp('act', self.QT[:, :, tt_ * 128:(tt_ + 1) * 128], psQ[:, 0:4, :], [pr[PQ]], [self.r_q[tt_]])
            b.cp('act', self.KE[0][0:64, tt_ * 128:(tt_ + 1) * 128], psQ[0:64, 4, :], [pr[PQ]], [self.r_ks[tt_]])
            b.cp('act', self.KE[1][64:128, tt_ * 128:(tt_ + 1) * 128], psQ[64:128, 4, :], [pr[PQ]],
                 [self.r_ks[tt_]])
            b.cp('act', self.KwT[:, tt_ * 128:(tt_ + 1) * 128], psQ[:, 5, :], [pr[PQ]], [self.r_kw[tt_]])

        def f_order():
            order = [('kc', 1048, rW[6]), ('vc', 1176, rW[7])]
            for ct in range(4):
                order.append(('g%d' % ct, 1304 + 512 + ct * 128, rW[8]))
            for ct in range(4):
                order.append(('a%d' % ct, 1304 + ct * 128, rW[8]))
            return order
        FGROUPS = (f_order()[0:4], f_order()[4:7], f_order()[7:10], [])

        def stageF(st, grp):
            hb = st % 2
            for name, c0, rw in FGROUPS[grp]:
                bank = PF0 if fstate['fidx'] % 2 == 0 else PF1
                fstate['fidx'] += 1
                for k in range(8):
                    b.mm(ps[bank][:, :], W[:, k, c0:c0 + 128], HT[hb][:, k, :], k == 0, k == 7,
                         rHT[hb] + [rw], [pr[bank]])
                if name in ('kc', 'vc'):
                    kb = fstate['kci'] % 2
                    fstate['kci'] += 1
                    b.cp('act', KCt[kb], ps[bank][:, :], [pr[bank]], [rKCt[kb]])
                    dstd = self.kc_scr if name == 'kc' else self.vc_scr
                    b.dma('sp', dstd[:, st * 512:(st + 1) * 512], KCt[kb], [rKCt[kb]], [Res()])
                elif name[0] == 'g':
                    ct = int(name[1])
                    b.act(SIG[ct], ps[bank][:, :], AF.Sigmoid, [pr[bank]], [rSIG[ct]])
                else:
                    ct = int(name[1])
                    sb = ct % 2
                    b.tt('dve', HGt[sb], ps[bank][:, :], SIG[ct], ALU.mult, [pr[bank], rSIG[ct]], [rHGt[sb]])
                    b.dma('sp', hg[ct][:, 30 + st * 512:30 + (st + 1) * 512], HGt[sb], [rHGt[sb]], [Res()])

        stageX(*tiles[0])
        for i, (st, j) in enumerate(tiles):
            if i + 1 < len(tiles):
                stageX(*tiles[i + 1])
            stageYmm(st, j)
            si = sts.index(st)
            if si > 0:
                stageF(sts[si - 1], j)
            if i > 1:
                stageYpost2(*tiles[i - 2])
            stageYpost(st, j)
            if i > 0:
                stageYpostB(*tiles[i - 1])
        stageYpostB(*tiles[-1])
        if len(tiles) > 1:
            stageYpost2(*tiles[-2])
        stageYpost2(*tiles[-1])
        for g in range(4):
            stageF(sts[-1], g)
        rg_all = Res()
        b.act(self.gates, self.gates, AF.Sigmoid, self.r_g, [rg_all])
        for t in range(NT):
            self.r_g[t] = rg_all
        self.markA = mark


INPUT_SPECS = [
    ('x', [S, D], F32), ('pos_t', [128, 32], I32), ('posc_t', [128, 2], I32),
    ('gin_t', [2, 128, 8], F32), ('gffn_t', [2, 128, 8], F32),
    ('w_in', [2, D, WIN], F32), ('w_out', [2, D, D], F32),
    ('q_norm_g', [2, 64], F32), ('k_norm_g', [2, 3, 64], F32),
    ('cmp_pos_k_t', [2, 128, 32], F32), ('cmp_w1_k', [2, 2048, 128], F32), ('cmp_w2_k', [2, 128, 64], F32),
    ('cmp_pos_v_t', [2, 128, 32], F32), ('cmp_w1_v', [2, 2048, 128], F32), ('cmp_w2_v', [2, 128, 64], F32),
    ('conv_w_t', [2, 128, 4, 31], F32), ('conv_b_t', [2, 128, 4], F32),
    ('conv_lng_t', [2, 128, 4], F32), ('conv_lnb_t', [2, 128, 4], F32),
    ('ffn_w_gate', [D, DFF], F32), ('ffn_w_up', [D, DFF], F32), ('ffn_w_down', [DFF, D], F32),
    ('moe_router', [D, NE], F32), ('moe_w_gate', [NE, D, DFF], F32), ('moe_w_up', [NE, D, DFF], F32),
    ('moe_w_down', [NE, DFF, D], F32),
]


def build_program(dbg=None):
    from contextlib import ExitStack
    nc = bass.Bass('TRN2', target_bir_lowering=False)
    dbg = dbg or {}
    with ExitStack() as stack:
        stack.enter_context(nc.allow_low_precision('bf16 matmul operands, fp32 accumulation'))
        stack.enter_context(nc.allow_non_contiguous_dma('small parameter loads'))
        B = Builder(nc, stack, dbg)
        use = dbg.get('inputs')
        inp = {}
        for name, shape, dt in INPUT_SPECS:
            if use is None or name in use:
                inp[name] = B.din(name, shape, dt)
        out = B.dout('out', [S, D], F32)
        B.hg_scr = [B.dscr('hg%d' % c, [128, S + 30], BF16) for c in range(4)]
        B.conv_scr = [B.dscr('cv%d' % c, [128, S], BF16) for c in range(4)]
        B.kc_scr = B.dscr('kc_scr', [128, S], BF16)
        B.vc_scr = B.dscr('vc_scr', [128, S], BF16)
        B.phase0(inp)
        B.build_selmap()
        if dbg.get('stage', 'full') == 'full':
            nl = dbg.get('layers', 2)
            B.final_out = out
            sparse = dbg.get('sparse', True)
            if sparse:
                B.setup_weight_conversion_sparse(inp)
            else:
                B.setup_weight_conversion(inp)
            xmid = [B.dscr('xmid%d' % i, [S, D], F32) for i in range(2)]
            xl1 = B.dscr('xl1', [S, D], F32)
            for l in range(nl):
                A = B.arena
                base = A.off
                xin = inp['x'] if l == 0 else xl1
                xout = out if l == nl - 1 else xl1
                P = B.load_layer_small(inp, l)
                markP = A.off
                B.phaseA(inp, l, xin, P)
                B.sc.barrier()
                A.off = B.markA
                B.phase_cmp(inp, l, P)
                A.off = B.markKc
                B.phase_conv(inp, l)
                B.phaseB(inp, l, xin, xmid[l])
                A.off = markP
                if l == 1:
                    B.bg(10000)
                    B.sc.barrier()
                if l == 1 and sparse:
                    B.phaseC_sparse(inp, l, xmid[l], xout, P)
                else:
                    B.phaseC(inp, l, xmid[l], xout, l == 1, P)
                A.off = base
        if dbg.get('stage') == 'B':
            P = B.load_layer_small(inp, 0)
            B.phaseA(inp, 0, inp['x'], P)
            B.sc.barrier()
            B.arena.off = B.markA
            B.phase_cmp(inp, 0, P)
            B.arena.off = B.markKc
            B.phase_conv(inp, 0)
            B.final_out = out
            dbg_attn = B.dout('dbg_attn', [S, 512], BF16)
            B.phaseB(inp, 0, inp['x'], out, qt_list=dbg.get('qt_list'), dbg_attn=dbg_attn)
        if dbg.get('stage') == 'conv':
            P = B.load_layer_small(inp, 0)
            B.phaseA(inp, 0, inp['x'], P)
            B.sc.barrier()
            B.arena.off = B.markKc
            B.phase_conv(inp, 0)
            cvo = B.dout('dbg_cv', [4, 128, S], BF16)
            for c in range(4):
                B.sc.dma('sp', lambda e, c=c: e.dma_start(out=cvo[c], in_=B.conv_scr[c]), [], [Res()], final=True)
        if dbg.get('stage') == 'cmp':
            P = B.load_layer_small(inp, 0)
            B.phaseA(inp, 0, inp['x'], P)
            B.sc.barrier()
            B.arena.off = B.markA
            B.phase_cmp(inp, 0, P)
            B.dump('KcmpT', B.KcmpT, [], [128, 256], BF16)
            B.dump('VcA', B.VcA, [], [128, 2, 2, 128], BF16)
        if dbg.get('stage') == 'A':
            P = B.load_layer_small(inp, 0)
            B.phaseA(inp, 0, inp['x'], P, st_list=dbg.get('st_list'))
            B.sc.barrier()
            B.dump('QT', B.QT, [], [128, 4, S], BF16)
            B.dump('KE0', B.KE[0], [], [128, S], BF16)
            B.dump('KE1', B.KE[1], [], [128, S], BF16)
            B.dump('KwT', B.KwT, [], [128, S], BF16)
            B.dump('VsA', B.VsA, [], [128, NT, 2, 65], BF16)
            B.dump('gates', B.gates, [], [128, NT, 24], F32)
            B.dump('cosT', B.cosT, [], [128, 34, 8], F32)
            B.dump('sinT', B.sinT, [], [128, 34, 8], F32)
            hgo = B.dout('dbg_hg', [4, 128, S + 30], BF16)
            B.dbg_out['dbg_hg'] = None
            for c in range(4):
                B.sc.dma('sp', lambda e, c=c: e.dma_start(out=hgo[c], in_=B.hg_scr[c]), [], [Res()], final=True)
        B.sc.emit(nc, stack)
    return nc, B


def host_inputs(inputs, bidx):
    pos = np.asarray(inputs['positions'][bidx])
    cmp_end = np.arange(255) * 16 + 31
    posc = np.zeros(256, np.int32)
    posc[:255] = pos[cmp_end]
    d = {
        'x': np.ascontiguousarray(inputs['x'][bidx]),
        'pos_t': np.ascontiguousarray(pos.reshape(32, 128).T),
        'posc_t': np.ascontiguousarray(posc.reshape(2, 128).T),
        'gin_t': np.ascontiguousarray(np.asarray(inputs['attn_norm_g']).reshape(2, 8, 128).transpose(0, 2, 1)),
        'gffn_t': np.ascontiguousarray(np.asarray(inputs['ffn_norm_g']).reshape(2, 8, 128).transpose(0, 2, 1)),
        'conv_w_t': np.ascontiguousarray(np.asarray(inputs['conv_w']).reshape(2, 31, 4, 128).transpose(0, 3, 2, 1)),
        'conv_b_t': np.ascontiguousarray(np.asarray(inputs['conv_b']).reshape(2, 4, 128).transpose(0, 2, 1)),
        'conv_lng_t': np.ascontiguousarray(np.asarray(inputs['conv_ln_g']).reshape(2, 4, 128).transpose(0, 2, 1)),
        'conv_lnb_t': np.ascontiguousarray(np.asarray(inputs['conv_ln_b']).reshape(2, 4, 128).transpose(0, 2, 1)),
        'ffn_w_gate': np.asarray(inputs['ffn_w_gate'])[0], 'ffn_w_up': np.asarray(inputs['ffn_w_up'])[0],
        'ffn_w_down': np.asarray(inputs['ffn_w_down'])[0],
        'moe_router': np.asarray(inputs['moe_router'])[0], 'moe_w_gate': np.asarray(inputs['moe_w_gate'])[0],
        'moe_w_up': np.asarray(inputs['moe_w_up'])[0], 'moe_w_down': np.asarray(inputs['moe_w_down'])[0],
    }
    for nm in ('cmp_pos_k', 'cmp_pos_v'):
        t = np.asarray(inputs[nm]).transpose(0, 2, 1)
        d[nm + '_t'] = np.ascontiguousarray(np.concatenate([t, t], axis=1))
    for k in ('w_in', 'w_out', 'q_norm_g', 'k_norm_g', 'cmp_w1_k', 'cmp_w2_k', 'cmp_w1_v', 'cmp_w2_v'):
        d[k] = np.asarray(inputs[k])
    return d


def _phase_cmp(self, inp, l, P):
    b, A, sc = self, self.arena, self.sc
    ps, pr = self.psb, self.psr
    self.r_kcmp = Res()
    self.r_vca = Res()
    mark = A.off
    for nt in range(2):
        for kh in range(2):
            b.cp('pool', self.VcA[:, nt, kh, 64:128], self.selmap[:, nt, :], [self.r_selmap], [self.r_vca])
    ident_b = self.ident_b
    sc.serialize_dma = bool(self.dbg.get('ser'))
    KVT = {}
    for which in ('k', 'v'):
        KVT[which] = A.alloc([S], BF16)
        rk = Res()
        b.dma('sp', KVT[which], self.kc_scr if which == 'k' else self.vc_scr, [], [rk])
        KVT[which + 'r'] = [rk]
    for which in ('k', 'v'):
        if self.dbg.get('cmp_stop', 99) <= 0:
            continue
        src = KVT[which]
        rsrc = KVT[which + 'r']
        W1 = A.alloc([32, 128], BF16)
        rW1 = Res()
        w1src = inp['cmp_w1_' + which][l].rearrange('(l d) h -> d l h', d=64)
        skip = self.dbg.get('cmp_skip', ())
        if 'w1' not in skip:
            b.dma('pool', W1[0:64], w1src, [], [rW1], nd=2048)
            b.dma('pool', W1[64:128], w1src, [], [rW1], nd=2048)
        posT = A.alloc([32], BF16)
        if 'pos' not in skip:
            b.dma('pool', posT, inp['cmp_pos_%s_t' % which][l], [], [rW1])
        W2 = A.alloc([64], BF16)
        if 'w2' not in skip:
            b.dma('pool', W2, inp['cmp_w2_' + which][l], [], [rW1])
        PH, PBI, PO, PTR = 0, 1, 2, 3
        PHS = (0, 4)
        stop = self.dbg.get('cmp_stop', 99)
        if stop <= 1:
            continue
        for kh in range(2):
            p0, p1 = kh * 64, (kh + 1) * 64
            for li in range(32):
                b.mm(ps[PHS[kh]][:, 0:255], W1[p0:p1, li, :], src[p0:p1, li:li + 4065:16],
                     li == 0, li == 31, [rW1] + rsrc, [pr[PHS[kh]]])
        for li in range(32):
            b.mm(ps[PBI][:, 0:1], W1[0:64, li, :], posT[0:64, li:li + 1], li == 0, li == 31, [rW1], [pr[PBI]])
        if stop <= 2:
            continue
        bias = A.alloc([1], F32)
        rb = Res()
        b.cp('dve', bias, ps[PBI][:, 0:1], [pr[PBI]], [rb])
        X = A.alloc([512], F32)
        X2 = A.alloc([512], F32)
        X3 = A.alloc([512], F32)
        HID = A.alloc([512], BF16)
        rX, rX2, rX3, rH = Res(), Res(), Res(), Res()
        for kh in range(2):
            b.ts('dve', X[:, kh * 256:(kh + 1) * 256], ps[PHS[kh]][:, 0:256], bias, None, ALU.add, None,
                 [pr[PHS[kh]], rb], [rX])
        b.tt('dve', X2, X, X, ALU.mult, [rX], [rX2])
        b.ts('dve', X3, X2, 0.044715, 1.0, ALU.mult, ALU.add, [rX2], [rX3])
        b.tt('dve', X2, X3, X, ALU.mult, [rX3, rX], [rX2])
        b.act(X3, X2, AF.Tanh, [rX2], [rX3], scale=math.sqrt(2.0 / math.pi))
        b.ts('dve', X2, X3, 1.0, 0.5, ALU.add, ALU.mult, [rX3], [rX2])
        b.tt('dve', HID, X2, X, ALU.mult, [rX2, rX], [rH])
        if stop <= 3:
            continue
        pso = ps[PO][:, 0:256].rearrange('p (a k d) -> p a k d', a=2, k=2)
        for nt in range(2):
            nn = 128 if nt == 0 else 127
            for kh in range(2):
                b.mm(pso[0:nn, nt, kh, :], HID[:, kh * 256 + nt * 128:kh * 256 + nt * 128 + nn], W2, True, True,
                     [rH, rW1], [pr[PO]])
        if stop <= 4:
            continue
        if which == 'v':
            for nt in range(2):
                nn = 128 if nt == 0 else 127
                b.cp('dve', self.VcA[0:nn, nt, :, 0:64], pso[0:nn, nt, :, :], [pr[PO]], [self.r_vca])
        else:
            SQ = A.alloc([4, 64], F32)
            s4 = A.alloc([4], F32)
            d4 = A.alloc([4], F32)
            r4 = A.alloc([4], F32)
            T1 = A.alloc([4, 64], F32)
            T2 = A.alloc([4, 64], F32)
            RA = A.alloc([4, 4, 8], F32)
            KB = A.alloc([2, 2, 64], BF16)
            rq, rs4, rd4, rr4, rT1, rT2, rRA, rKB = (Res() for _ in range(8))
            psf = ps[PO][:, 0:256].rearrange('p (h d) -> p h d', d=64)
            b.memset('pool', SQ, 1.0, [rq])
            b.memset('pool', T1, 0.0, [rT1])
            for nt in range(2):
                nn = 128 if nt == 0 else 127
                h0 = nt * 2
                b.act(SQ[0:nn, h0:h0 + 2, :], psf[0:nn, h0:h0 + 2, :], AF.Square, [pr[PO]], [rq])
            sc.op('dve', lambda e: e.tensor_reduce(s4, SQ, AX.X, ALU.add), [rq], [rs4])
            b.act(d4, s4, AF.Sqrt, [rs4], [rd4], scale=1.0 / 64, bias=EPS)
            sc.op('dve', lambda e: e.reciprocal(r4, d4), [rd4], [rr4])
            for nt in range(2):
                nn = 128 if nt == 0 else 127
                h0 = nt * 2
                b.tt('dve', T1[0:nn, h0:h0 + 2, :], psf[0:nn, h0:h0 + 2, :],
                     r4[0:nn, h0:h0 + 2].unsqueeze(2).to_broadcast([nn, 2, 64]), ALU.mult, [pr[PO], rr4], [rT1])
            b.tt('dve', T2, T1, P['gk'][:, 0:1, :].to_broadcast([128, 4, 64]), ALU.mult, [rT1, P['r']], [rT2])
            T2v = T2.rearrange('p (a k) d -> p a k d', a=2)
            RAv = RA.rearrange('p r (a k) d -> p r a k d', a=2)
            KBv = KB
            cosb = self.cosT[:, 32:34, :].unsqueeze(2).to_broadcast([128, 2, 2, 8])
            sinb = self.sinT[:, 32:34, :].unsqueeze(2).to_broadcast([128, 2, 2, 8])
            x1 = T2v[:, :, :, 0:8]
            x2 = T2v[:, :, :, 8:16]
            b.tt('dve', RAv[:, 0], x1, cosb, ALU.mult, [rT2, self.r_rope], [rRA])
            b.tt('dve', RAv[:, 1], x2, sinb, ALU.mult, [rT2, self.r_rope], [rRA])
            b.tt('dve', RAv[:, 2], x2, cosb, ALU.mult, [rT2, self.r_rope], [rRA])
            b.tt('dve', RAv[:, 3], x1, sinb, ALU.mult, [rT2, self.r_rope], [rRA])
            b.tt('dve', KBv[:, :, :, 0:8], RAv[:, 0], RAv[:, 1], ALU.subtract, [rRA], [rKB])
            b.tt('dve', KBv[:, :, :, 8:16], RAv[:, 2], RAv[:, 3], ALU.add, [rRA], [rKB])
            b.cp('dve', KBv[:, :, :, 16:64], T2v[:, :, :, 16:64], [rT2], [rKB])
            psq = self.psum_bf(PTR).rearrange('p (k t) -> p k t', t=128)
            for nt in range(2):
                b.tr(psq[:, nt, :], KB[:, nt].rearrange('p k d -> p (k d)'), ident_b, [rKB, self.r_ident], [pr[PTR]])
            b.cp('dve', self.KcmpT.rearrange('p (a n) -> p a n', a=2), psq[:, 0:2, :], [pr[PTR]], [self.r_kcmp])
    sc.barrier()
    A.off = mark


Builder.phase_cmp = _phase_cmp


def _build_selmap(self):
    b, A, sc = self, self.arena, self.sc
    self.selmap = A.alloc([2, 64], F32)
    self.r_selmap = Res()
    mark = A.off
    ones = A.alloc([64], F32)
    half = A.alloc([64], F32)
    t1 = A.alloc([2, 64], F32)
    t2 = A.alloc([2, 64], F32)
    t3 = A.alloc([2, 64], F32)
    r0, r1, r2, r3 = Res(), Res(), Res(), Res()
    b.memset('pool', ones, 1.0, [r0])
    b.memset('pool', half, 0.5, [r0])
    for nt in range(2):
        base = 128 * nt
        o1, o2, o3 = t1[:, nt, :], t2[:, nt, :], t3[:, nt, :]
        sc.op('pool', lambda e, o=o1, base=base: e.affine_select(out=o, in_=ones, pattern=[[-4, 64]],
              compare_op=ALU.is_ge, fill=0.0 if REGS is None else REGS['zero'], base=base, channel_multiplier=1), [r0], [r1])
        sc.op('pool', lambda e, o=o1, base=base: e.affine_select(out=o, in_=o, pattern=[[4, 64]],
              compare_op=ALU.is_ge, fill=0.0 if REGS is None else REGS['zero'], base=3 - base, channel_multiplier=-1), [r1], [r1])
        sc.op('pool', lambda e, o=o2, base=base: e.affine_select(out=o, in_=half, pattern=[[-4, 64]],
              compare_op=ALU.is_equal, fill=0.0 if REGS is None else REGS['zero'], base=base - 3, channel_multiplier=1), [r0], [r2])
        sc.op('pool', lambda e, o=o3, base=base: e.affine_select(out=o, in_=half, pattern=[[-4, 64]],
              compare_op=ALU.is_equal, fill=0.0 if REGS is None else REGS['zero'], base=base + 1, channel_multiplier=1), [r0], [r3])
    b.tt('pool', t1, t1, t2, ALU.subtract, [r1, r2], [r1])
    b.tt('pool', self.selmap, t1, t3, ALU.add, [r1, r3], [self.r_selmap])
    sc.barrier()
    A.off = mark


Builder.build_selmap = _build_selmap


def _phase_conv(self, inp, l):
    b, A, sc = self, self.arena, self.sc
    ps, pr = self.psb, self.psr
    mark = A.off
    cw = A.alloc([4, 31], F32)
    cb = A.alloc([4], F32)
    lg = A.alloc([4], F32)
    lb = A.alloc([4], F32)
    rp = Res()
    b.dma('sp', cw, inp['conv_w_t'][l], [], [rp])
    b.dma('sp', cb, inp['conv_b_t'][l], [], [rp])
    b.dma('sp', lg, inp['conv_lng_t'][l], [], [rp])
    b.dma('sp', lb, inp['conv_lnb_t'][l], [], [rp])
    DG = A.alloc([4, 31, 128], BF16)
    rDG = [Res() for _ in range(4)]
    i = 0
    for ct in range(4):
        for j in range(31):
            eng = 'dve'
            i += 1
            b.ts(eng, DG[:, ct, j, :], self.ident_b, cw[:, ct, j:j + 1], None, ALU.mult, None,
                 [self.r_ident, rp], [rDG[ct]])
    HG = A.alloc([4, S + 30], BF16)
    rHG = [Res() for _ in range(4)]
    for ct in range(4):
        b.dma('sp', HG[:, ct, :], self.hg_scr[ct], [], [rHG[ct]])
    onesm = A.alloc([128], F32)
    rones = Res()
    b.memset('pool', onesm, 1.0 / 512.0, [rones])
    YS = [A.alloc([512], F32) for _ in range(4)]
    YQ = [A.alloc([512], F32) for _ in range(2)] * 2
    rYS = [Res() for _ in range(4)]
    rYQ = [Res() for _ in range(2)] * 2
    MEAN = A.alloc([512], F32)
    MSQ = A.alloc([512], F32)
    VAR = A.alloc([512], F32)
    RSTD = A.alloc([512], F32)
    rM, rMS, rV, rR = Res(), Res(), Res(), Res()
    Z = [VAR] * 2
    rZ = [rV] * 2
    CT = [A.alloc([512], BF16)] * 2
    rCT = [Res()] * 2
    zi = 0
    for tc in range(NST):
        for ct in range(4):
            for j in range(31):
                b.mm(ps[ct][:, :], DG[:, ct, j, :], HG[:, ct, tc * 512 + j:tc * 512 + j + 512], j == 0, j == 30,
                     [rDG[ct], rHG[ct]], [pr[ct]])
        for ct in range(4):
            b.act(YS[ct], ps[ct][:, :], AF.Identity, [pr[ct], rp], [rYS[ct]], bias=cb[:, ct:ct + 1])
            b.act(YQ[ct], ps[ct][:, :], AF.Square, [pr[ct], rp], [rYQ[ct]], bias=cb[:, ct:ct + 1])
            b.mm(ps[5][:, :], onesm, YQ[ct], ct == 0, ct == 3, [rones, rYQ[ct]], [pr[5]])
        for ct in range(4):
            b.mm(ps[4][:, :], onesm, YS[ct], ct == 0, ct == 3, [rones, rYS[ct]], [pr[4]])
        b.cp('act', MEAN, ps[4][:, :], [pr[4]], [rM])
        b.tt('dve', MSQ, ps[4][:, :], MEAN, ALU.mult, [pr[4], rM], [rMS])
        b.tt('dve', VAR, ps[5][:, :], MSQ, ALU.subtract, [pr[5], rMS], [rV])
        b.act(MSQ, VAR, AF.Sqrt, [rV], [rMS], bias=EPS)
        sc.op('dve', lambda e: e.reciprocal(RSTD, MSQ), [rMS], [rR])
        for ct in range(4):
            zb = zi % 2
            zi += 1
            b.tt('dve', Z[zb], YS[ct], MEAN, ALU.subtract, [rYS[ct], rM], [rZ[zb]])
            b.tt('dve', Z[zb], Z[zb], RSTD, ALU.mult, [rZ[zb], rR], [rZ[zb]])
            b.act(CT[zb], Z[zb], AF.Silu, [rZ[zb], rp], [rCT[zb]], scale=lg[:, ct:ct + 1], bias=lb[:, ct:ct + 1])
            b.dma('sp', self.conv_scr[ct][:, tc * 512:(tc + 1) * 512], CT[zb], [rCT[zb]], [Res()])
    sc.barrier()
    A.off = mark


Builder.phase_conv = _phase_conv


def _phaseB(self, inp, l, xsrc, xdst, qt_list=None, dbg_attn=None):
    b, A, sc = self, self.arena, self.sc
    ps, pr = self.psb, self.psr
    mark = A.off
    ident_b = self.ident_b
    Wout = A.alloc([8, D], BF16)
    rWo = Res()
    wsrc = inp['w_out'][l].rearrange('(m p) d -> p m d', p=128)
    for h in range(2):
        b.dma('pool', Wout[:, :, h * 512:(h + 1) * 512], wsrc[:, :, h * 512:(h + 1) * 512], [], [rWo], nd=1024)
    NB = 2
    R = [[A.alloc([4, 128], BF16) for _ in range(2)] for _ in range(NB)]
    RZ = [[A.alloc([4, 128], BF16) for _ in range(2)] for _ in range(NB)]
    rRq = [[Res() for _ in range(2)] for _ in range(NB)]
    rRm = [[Res() for _ in range(2)] for _ in range(NB)]
    rRZ = [[Res() for _ in range(2)] for _ in range(NB)]
    for i in range(NB):
        b.memset('pool', RZ[i][0][64:128], 0.0, [rRZ[i][0]])
        b.memset('pool', RZ[i][1][0:64], 0.0, [rRZ[i][1]])
    NP = 6
    PT_ = [A.alloc([512], BF16) for _ in range(NP)]
    rPT = [Res() for _ in range(NP)]
    cmask = [[A.alloc([4, 128], BF16) for _ in range(2)] for _ in range(NB)]
    rcm = [[Res() for _ in range(2)] for _ in range(NB)]
    XT = [A.alloc([D], F32) for _ in range(3)]
    rXT = [Res() for _ in range(3)]
    CV = [A.alloc([4, 128], BF16) for _ in range(3)]
    rCV = [Res() for _ in range(3)]
    ATT = A.alloc([512], BF16)
    rATT = Res()
    ATT_T = A.alloc([4, 128], BF16)
    rATT_T = Res()
    MM_ = A.alloc([128], BF16)
    rMM = Res()
    ACC = [[A.alloc([4, 64], F32) for _ in range(2)] for _ in range(NB)]
    r_acc = [[Res() for _ in range(2)] for _ in range(NB)]
    rsumC = [[A.alloc([4], F32) for _ in range(2)] for _ in range(NB)]
    rinvC = [[A.alloc([4], F32) for _ in range(2)] for _ in range(NB)]
    r_rsumC = [[Res() for _ in range(2)] for _ in range(NB)]
    r_rinvC = [[Res() for _ in range(2)] for _ in range(NB)]
    rsum2 = [A.alloc([2, 4], F32) for _ in range(2)]
    rinv = [A.alloc([3, 4], F32) for _ in range(2)]
    coef = [A.alloc([3, 4], F32) for _ in range(2)]
    r_rsum2 = [Res() for _ in range(2)]
    r_rinv = [Res() for _ in range(2)]
    r_coef = [Res() for _ in range(2)]
    IMP = [A.alloc([64], F32) for _ in range(2)]
    IMPM = [A.alloc([64], F32) for _ in range(2)]
    IMP2 = [A.alloc([64], F32) for _ in range(2)]
    M8 = [A.alloc([16], F32) for _ in range(2)]
    SELC = [A.alloc([64], F32) for _ in range(2)]
    FRC = [A.alloc([64], F32) for _ in range(2)]
    r_imp = [Res() for _ in range(2)]
    r_impm = [Res() for _ in range(2)]
    r_imp2 = [Res() for _ in range(2)]
    r_m8 = [Res() for _ in range(2)]
    r_selc = [Res() for _ in range(2)]
    r_frc = [Res() for _ in range(2)]
    TMP = [A.alloc([4, 64], F32) for _ in range(2)]
    r_tmp = [Res() for _ in range(2)]
    ST = (0, 1)
    OC, OS, OW, TRP, OUT0, OUT1 = 2, 3, 4, 5, 6, 7
    psOC = ps[OC][:, 0:512].rearrange('p (g c) -> p g c', g=4)
    psOST = ps[OS][0:65, :]
    psOWT = ps[OW][0:65, :]
    psOS = ps[OUT0][:, 0:260].rearrange('p (g c) -> p g c', g=4)
    psOW = ps[OUT1][:, 0:260].rearrange('p (g c) -> p g c', g=4)
    OTs = [A.alloc([512], F32) for _ in range(2)]
    rOTs = [Res() for _ in range(2)]
    psTR = self.psum_bf(TRP).rearrange('p (k t) -> p k t', t=128)
    gates_v = self.gates.rearrange('p t (h r) -> p t h r', r=3)
    state = {'sti': 0, 'pti': 0, 'pend': None}

    def flush():
        if state['pend'] is not None:
            state['pend']()
            state['pend'] = None

    def tile_step(lhsT, rhs, rl, rr, nn, post_mask, rmask, acc_view, vrhs, rv, first, width, accbank,
                  transposed=False):
        sb = ST[state['sti'] % 2]
        state['sti'] += 1
        pb = state['pti'] % NP
        state['pti'] += 1
        if post_mask is None:
            b.mm(ps[sb][0:nn, :], lhsT, rhs, True, True, rl + rr, [pr[sb]])
        else:
            b.mm(ps[sb][0:nn, :], lhsT, rhs, True, False, rl + rr, [pr[sb]])
            b.mm(ps[sb][0:nn, :], ident_b[0:nn, 0:nn], post_mask[0:nn].rearrange('p g q -> p (g q)'), False, True,
                 [self.r_ident] + rmask, [pr[sb]])
        b.act(PT_[pb][0:nn, :], ps[sb][0:nn, :], AF.Exp, [pr[sb]], [rPT[pb]])
        flush()

        def pv():
            if transposed:
                b.mm(acc_view, vrhs, PT_[pb][0:nn, :], first, False, [rPT[pb]] + rv, [pr[accbank]])
                return
            for g in range(4):
                b.mm(acc_view[:, g, 0:width], PT_[pb][0:nn, g * 128:(g + 1) * 128], vrhs, first and g == 0, False,
                     [rPT[pb]] + rv, [pr[accbank]])
        state['pend'] = pv

    def s1pre(qi, qt):
        t0 = qt * 128
        rb_ = qi % NB
        xb = qi % 3
        tsl = slice(t0, t0 + 128)
        b.bg(2)
        b.dma('sp', XT[xb], xsrc[t0:t0 + 128, :], [], [rXT[xb]])
        for ct in range(4):
            b.dma('sp', CV[xb][:, ct, :], self.conv_scr[ct][:, tsl], [], [rCV[xb]])
        b.cp('pool', R[rb_][0][0:64], self.QT[0:64, :, tsl], [self.r_q[qt]], [rRq[rb_][0]])
        b.cp('pool', R[rb_][1][64:128], self.QT[64:128, :, tsl], [self.r_q[qt]], [rRq[rb_][1]])
        b.cp('pool', RZ[rb_][0][0:64], self.QT[0:64, :, tsl], [self.r_q[qt]], [rRZ[rb_][0]])
        b.cp('pool', RZ[rb_][1][64:128], self.QT[64:128, :, tsl], [self.r_q[qt]], [rRZ[rb_][1]])

    def s1a(qi, qt, kh):
        t0 = qt * 128
        rb_ = qi % NB
        nmax = 8 * qt + 6
        ntiles = [0] if nmax < 128 else [0, 1]
        if True:
            rz = RZ[rb_][kh].rearrange('p g q -> p (g q)')
            for ni, nt_ in enumerate(ntiles):
                nn = 128 if nt_ == 0 else 127
                nbase = 128 * nt_
                full = (16 * (nbase + nn - 1) + 31) <= t0
                pm, rmk = None, []
                if not full:
                    cm_ = cmask[rb_][nt_]
                    if kh == 0:
                        zb4 = self.zeros_b4
                        sc.op('pool', lambda e, cm_=cm_, base=t0 - 16 * nbase - 31, zb4=zb4: e.affine_select(
                            out=cm_, in_=zb4, pattern=[[0, 4], [1, 128]], compare_op=ALU.is_ge,
                            fill=NEG if REGS is None else REGS['neg'], base=base,
                            channel_multiplier=-16), [self.r_masks], [rcm[rb_][nt_]])
                    pm, rmk = cm_, [rcm[rb_][nt_]]
                tile_step(self.KcmpT[:, nbase:nbase + nn], rz, [self.r_kcmp], [rRZ[rb_][kh]], nn, pm, rmk,
                          psOC, self.VcA[0:nn, nt_, kh, :], [self.r_vca], ni == 0, 128, OC)
            flush()
            sc.op('dve', lambda e, o=rsumC[rb_][kh], i=psOC[:, :, 64:128]: e.tensor_reduce(o, i, AX.X, ALU.add),
                  [pr[OC]], [r_rsumC[rb_][kh]])
            b.ts('dve', rinvC[rb_][kh], rsumC[rb_][kh], 1e-30, None, ALU.max, None, [r_rsumC[rb_][kh]],
                 [r_rinvC[rb_][kh]])
            sc.op('dve', lambda e, o=rinvC[rb_][kh]: e.reciprocal(o, o), [r_rinvC[rb_][kh]], [r_rinvC[rb_][kh]])
            b.cp('act', ACC[rb_][kh], psOC[:, :, 0:64], [pr[OC]], [r_acc[rb_][kh]])

    def s1topk(qi, qt, kh):
        rb_ = qi % NB
        if True:
            mdst = MM_[:, 64:128] if kh == 0 else MM_[:, 0:64]
            if qt >= 8:
                b.ts('dve', IMP[kh], psOC[:, 0, 64:128], rinvC[rb_][kh][:, 0:1], None, ALU.mult, None,
                     [pr[OC], r_rinvC[rb_][kh]], [r_imp[kh]])
                for g in range(1, 4):
                    b.stt(IMP[kh], psOC[:, g, 64:128], rinvC[rb_][kh][:, g:g + 1], IMP[kh], ALU.mult, ALU.add,
                          [pr[OC], r_rinvC[rb_][kh], r_imp[kh]], [r_imp[kh]])
                for half in range(2):
                    cur = 2 * qt + half
                    hs = slice(half * 64, half * 64 + 64)
                    sc.op('pool', lambda e, o=IMPM[kh][hs, :], i=IMP[kh][hs, :], base=cur - 2: e.affine_select(
                        out=o, in_=i, pattern=[[-1, 64]], compare_op=ALU.is_ge,
                        fill=NEG if REGS is None else REGS['neg'], base=base,
                        channel_multiplier=0), [r_imp[kh]], [r_impm[kh]])
                b.memset('pool', IMPM[kh][:, 0:1], NEG, [r_impm[kh]])
                sc.op('dve', lambda e, o=M8[kh][:, 0:8], i=IMPM[kh]: e.max(o, i), [r_impm[kh]], [r_m8[kh]])
                sc.op('dve', lambda e, o=IMP2[kh], a=M8[kh][:, 0:8], v=IMPM[kh]: e.match_replace(o, a, v, NEG),
                      [r_impm[kh], r_m8[kh]], [r_imp2[kh]])
                sc.op('dve', lambda e, o=M8[kh][:, 8:16], i=IMP2[kh]: e.max(o, i), [r_imp2[kh]], [r_m8[kh]])
                b.ts('dve', SELC[kh], IMPM[kh], M8[kh][:, 12:13], None, ALU.is_ge, None, [r_impm[kh], r_m8[kh]],
                     [r_selc[kh]])
                b.memset('pool', FRC[kh], 0.0, [r_frc[kh]])
                b.memset('pool', FRC[kh][:, 0:1], 1.0, [r_frc[kh]])
                b.memset('pool', FRC[kh][0:64, 2 * qt - 1:2 * qt + 1], 1.0, [r_frc[kh]])
                b.memset('pool', FRC[kh][64:128, 2 * qt:2 * qt + 2], 1.0, [r_frc[kh]])
                b.tt('dve', mdst, SELC[kh], FRC[kh], ALU.max, [r_selc[kh], r_frc[kh]], [rMM])
            else:
                b.memset('pool', mdst, 0.0, [rMM])
                b.memset('pool', mdst[0:64, 0:2 * qt + 1], 1.0, [rMM])
                b.memset('pool', mdst[64:128, 0:2 * qt + 2], 1.0, [rMM])
    def s1b(qi, qt):
        rb_ = qi % NB
        b.tr(psTR[:, 0, :], MM_, ident_b, [rMM, self.r_ident], [pr[TRP]])
        b.ts('dve', R[rb_][0][64:128], psTR[64:128, 0:1, :].to_broadcast([64, 4, 128]), -1.0, -NEG, ALU.add,
             ALU.mult, [pr[TRP]], [rRm[rb_][0]])
        b.ts('dve', R[rb_][1][0:64], psTR[0:64, 0:1, :].to_broadcast([64, 4, 128]), -1.0, -NEG, ALU.add,
             ALU.mult, [pr[TRP]], [rRm[rb_][1]])

    def s2win(qi, qt, kh):
        rb_ = qi % NB
        if True:
            rz = RZ[rb_][kh].rearrange('p g q -> p (g q)')
            kts = list(range(max(0, qt - 4), qt + 1))
            for i, kt in enumerate(kts):
                if kt == qt:
                    pm, rmk = self.causalN, [self.r_masks]
                elif kt == qt - 4:
                    pm, rmk = self.lowN, [self.r_masks]
                else:
                    pm, rmk = None, []
                tile_step(self.KwT[:, kt * 128:(kt + 1) * 128], rz, [self.r_kw[kt]], [rRZ[rb_][kh]], 128, pm,
                          rmk, psOWT, self.VwA[:, kt, kh, :], [self.r_vw[kt]], i == 0, 65, OW, transposed=True)

    def s2sel(qi, qt, kh):
        rb_ = qi % NB
        if True:
            rr = R[rb_][kh].rearrange('p g q -> p (g q)')
            for kt in range(qt + 1):
                pm, rmk = (self.causalN, [self.r_masks]) if kt == qt else (None, [])
                tile_step(self.KE[kh][:, kt * 128:(kt + 1) * 128], rr, [self.r_ks[kt], self.r_E],
                          [rRq[rb_][kh], rRm[rb_][kh]], 128, pm, rmk, psOST, self.VsA[:, kt, kh, :],
                          [self.r_vs[kt]], kt == 0, 65, OS, transposed=True)
            flush()
            b.cp('act', OTs[0][0:65, :], psOWT, [pr[OW]], [rOTs[0]])
            b.cp('dve', OTs[1][0:65, :], psOST, [pr[OS]], [rOTs[1]])
            for src_i, dstv, dbank in ((0, psOW, OUT1), (1, psOS, OUT0)):
                for g in range(4):
                    b.tr(dstv[:, g, :], OTs[src_i][0:65, g * 128:(g + 1) * 128], self.ident_f[0:65, 0:65],
                         [rOTs[src_i], self.r_identf], [pr[dbank]])
            b.cp('dve', rsum2[kh][:, 0, :], psOS[:, :, 64], [pr[OUT0]], [r_rsum2[kh]])
            b.cp('dve', rsum2[kh][:, 1, :], psOW[:, :, 64], [pr[OUT1]], [r_rsum2[kh]])
            sc.op('dve', lambda e, o=rinv[kh][:, 1:3, :], i=rsum2[kh]: e.reciprocal(o, i), [r_rsum2[kh]],
                  [r_rinv[kh]])
            b.cp('dve', rinv[kh][:, 0, :], rinvC[rb_][kh], [r_rinvC[rb_][kh]], [r_rinv[kh]])
            gv = gates_v[:, qt, kh * 4:(kh + 1) * 4, :].rearrange('p h r -> p r h')
            b.tt('dve', coef[kh], rinv[kh], gv, ALU.mult, [r_rinv[kh], self.r_g[qt]], [r_coef[kh]])
            acc = ACC[rb_][kh]
            racc = r_acc[rb_][kh]
            b.tt('dve', acc, acc, coef[kh][:, 0, :].unsqueeze(2).to_broadcast([128, 4, 64]), ALU.mult,
                 [racc, r_coef[kh]], [racc])
            b.tt('dve', TMP[kh], psOS[:, :, 0:64], coef[kh][:, 1, :].unsqueeze(2).to_broadcast([128, 4, 64]),
                 ALU.mult, [pr[OUT0], r_coef[kh]], [r_tmp[kh]])
            b.tt('dve', acc, acc, TMP[kh], ALU.add, [racc, r_tmp[kh]], [racc])
            b.tt('dve', TMP[kh], psOW[:, :, 0:64], coef[kh][:, 2, :].unsqueeze(2).to_broadcast([128, 4, 64]),
                 ALU.mult, [pr[OUT1], r_coef[kh]], [r_tmp[kh]])
            b.tt('dve', ATT[:, kh * 256:(kh + 1) * 256].rearrange('p (g d) -> p g d', g=4), acc, TMP[kh],
                 ALU.add, [racc, r_tmp[kh]], [rATT])
    def s2tail(qi, qt):
        t0 = qt * 128
        xb = qi % 3
        if dbg_attn is not None:
            b.dma('sp', dbg_attn[t0:t0 + 128, :], ATT, [rATT], [Res()], final=True)
        for c in range(4):
            b.tr(psTR[:, 1 + c, :], ATT[:, c * 128:(c + 1) * 128], ident_b, [rATT, self.r_ident], [pr[TRP]])
        b.cp('act', ATT_T, psTR[:, 1:5, :], [pr[TRP]], [rATT_T])
        for h, bank in enumerate((OUT0, OUT1)):
            for m in range(8):
                lhs = ATT_T[:, m, :] if m < 4 else CV[xb][:, m - 4, :]
                rl = [rATT_T] if m < 4 else [rCV[xb]]
                b.mm(ps[bank][:, :], lhs, Wout[:, m, h * 512:(h + 1) * 512], m == 0, m == 7, rl + [rWo], [pr[bank]])
            b.tt('dve', XT[xb][:, h * 512:(h + 1) * 512], XT[xb][:, h * 512:(h + 1) * 512], ps[bank][:, :], ALU.add,
                 [rXT[xb], pr[bank]], [rXT[xb]])
        b.dma('sp', xdst[t0:t0 + 128, :], XT[xb], [rXT[xb]], [Res()], final=(xdst is self.final_out))

    qts = list(qt_list if qt_list is not None else range(NT))
    s1pre(0, qts[0])
    for kh in range(2):
        s1a(0, qts[0], kh)
        s1topk(0, qts[0], kh)
    s1b(0, qts[0])
    for qi, qt in enumerate(qts):
        nxt = qi + 1 < len(qts)
        if nxt:
            s1pre(qi + 1, qts[qi + 1])
        for kh in range(2):
            if nxt:
                s1a(qi + 1, qts[qi + 1], kh)
            s2win(qi, qt, kh)
            if kh == 0 and qi > 0:
                s2tail(qi - 1, qts[qi - 1])
            if nxt:
                s1topk(qi + 1, qts[qi + 1], kh)
            s2sel(qi, qt, kh)
        if nxt:
            s1b(qi + 1, qts[qi + 1])
    s2tail(len(qts) - 1, qts[-1])
    sc.barrier()
    A.off = mark


Builder.phaseB = _phaseB


def _setup_weight_conversion(self, inp):
    self.wb = {}
    self.bg_tasks = []
    self.r_wb = {}
    specs = [('ffn', None)] + [('moe', e) for e in range(NE)]
    for kind, e in specs:
        key = 'd' if kind == 'ffn' else e
        for nm, shape in (('gate', [D, DFF]), ('up', [D, DFF]), ('down', [DFF, D])):
            src = inp['%s_w_%s' % (kind, nm)] if kind == 'ffn' else inp['moe_w_%s' % nm][e]
            dst = self.dscr('wb_%s_%s' % (key, nm), shape, BF16)
            self.wb[(key, nm)] = dst
            r = Res()
            self.r_wb[(key, nm)] = r
            rows = shape[0]
            step = 256 if shape[1] > 2048 else 1024
            for r0 in range(0, rows, step):
                r1 = min(rows, r0 + step)
                ndesc = (r1 - r0) * (2 if shape[1] > 2048 else 1)

                def task(dst=dst, src=src, r0=r0, r1=r1, r=r, ndesc=ndesc):
                    self.dma('pool', dst[r0:r1, :], src[r0:r1, :], [], [Res()], nd=ndesc, max_dma_last_dim=4096)
                self.bg_tasks.append(task)


def _bg(self, n=1):
    for _ in range(n):
        if self.bg_tasks:
            self.bg_tasks.pop(0)()


Builder.setup_weight_conversion = _setup_weight_conversion
Builder.bg = _bg


def _phaseC(self, inp, l, xsrc, xdst, moe, P, st_list=None):
    b, A, sc = self, self.arena, self.sc
    ps, pr = self.psb, self.psr
    mark = A.off
    ident_b, ident_f = self.ident_b, self.ident_f
    XF = [A.alloc([D], F32) for _ in range(4)]
    rXF = [Res() for _ in range(4)]
    XN = A.alloc([D], F32 if moe else BF16)
    rXN = Res()
    H2T = A.alloc([8, 512], BF16)
    rH2T = [Res() for _ in range(4)]
    ss = A.alloc([1], F32)
    sd = A.alloc([1], F32)
    rstd = A.alloc([1], F32)
    rss, rsd, rrs = Res(), Res(), Res()
    ACTT = A.alloc([NFT, 512], BF16)
    rACT = [Res() for _ in range(NFT)]
    WD = A.alloc([NFT, D], BF16)
    rWD = Res()
    NW = 2
    WG = [A.alloc([8, 512], BF16) for _ in range(NW)]
    WU = [A.alloc([8, 512], BF16) for _ in range(NW)]
    rWG = [Res() for _ in range(NW)]
    rWU = [Res() for _ in range(NW)]
    SL = [A.alloc([512], F32) for _ in range(2)]
    rSL = [Res() for _ in range(2)]
    if moe:
        H32 = A.alloc([8, 128], F32)
        rH32 = Res()
        RT = A.alloc([8, NE], F32)
        rRT = Res()
        b.dma('sp', RT, inp['moe_router'].rearrange('(k p) e -> p k e', p=128), [], [rRT])
        LG = A.alloc([NE], F32)
        M8 = A.alloc([8], F32)
        PP = A.alloc([2], F32)
        G0 = A.alloc([NE], F32)
        GATE = A.alloc([4, NE], F32)
        rLG, rM8, rPP, rG0 = Res(), Res(), Res(), Res()
        rGATE = [Res() for _ in range(4)]
    T0, T1 = 0, 1
    GU = (2, 3, 4, 5)
    DN = (6, 7)
    gui = 0
    dni = 0
    wi = 0
    gffn_b = P['gffn'].unsqueeze(2).to_broadcast([128, 8, 128])
    experts = list(range(NE)) if moe else ['d']
    if not moe:
        XP = [A.alloc([D], F32) for _ in range(2)]
        rXP = [Res() for _ in range(2)]
        XN2 = [XN, A.alloc([D], BF16)]
        rXN2 = [rXN, Res()]
        sts = list(st_list if st_list is not None else range(NST))
        psT = self.psum_bf(T0).rearrange('p (k t) -> p k t', t=128)[:, 0:8, :]

        def norm(st, j):
            tt_ = st * 4 + j
            xb = tt_ % 2
            b.dma('sp', XP[xb], xsrc[tt_ * 128:(tt_ + 1) * 128, :], [], [rXP[xb]])
            b.act(XN2[xb], XP[xb], AF.Square, [rXP[xb]], [rXN2[xb], rss], accum_out=ss)
            b.act(sd, ss, AF.Sqrt, [rss], [rsd], scale=1.0 / D, bias=EPS)
            sc.op('dve', lambda e: e.reciprocal(rstd, sd), [rsd], [rrs])
            b.act(XN2[xb], XP[xb], AF.Copy, [rXP[xb], rrs], [rXN2[xb]], scale=rstd)

        def trans(st, j):
            xb = (st * 4 + j) % 2
            for k in range(8):
                b.tr(psT[:, k, :], XN2[xb][:, k * 128:(k + 1) * 128], ident_b, [rXN2[xb], self.r_ident], [pr[T0]])
            b.tt('dve', H2T[:, :, j * 128:(j + 1) * 128], psT, gffn_b, ALU.mult, [pr[T0], P['r']], [rH2T[j]])

        for j in range(4):
            norm(sts[0], j)
            trans(sts[0], j)
        wg_d, wu_d, wd_d = self.wb[('d', 'gate')], self.wb[('d', 'up')], self.wb[('d', 'down')]
        wgv = wg_d.rearrange('(k p) f -> p k f', p=128)
        wuv = wu_d.rearrange('(k p) f -> p k f', p=128)
        for si, st in enumerate(sts):
            nxt = sts[si + 1] if si + 1 < len(sts) else None
            for j in range(4):
                tt_ = st * 4 + j
                b.dma('sp', XF[j], xsrc[tt_ * 128:(tt_ + 1) * 128, :], [], [rXF[j]])
            for c in range(7):
                wb_ = wi % NW
                wi += 1
                b.dma('sp', WG[wb_], wgv[:, :, c * 512:(c + 1) * 512], [], [rWG[wb_]], nd=1024)
                b.dma('sp', WU[wb_], wuv[:, :, c * 512:(c + 1) * 512], [], [rWU[wb_]], nd=1024)
                if c == 0:
                    b.dma('sp', WD, wd_d.rearrange('(ft p) d -> p ft d', p=128), [], [rWD], nd=3584)
                for fi in range(4):
                    ft = c * 4 + fi
                    gb, ub = GU[(gui * 2) % 4], GU[(gui * 2 + 1) % 4]
                    sl = gui % 2
                    gui += 1
                    for k in range(8):
                        b.mm(ps[gb][:, :], WG[wb_][:, k, fi * 128:(fi + 1) * 128], H2T[:, k, :], k == 0, k == 7,
                             [rWG[wb_]] + rH2T, [pr[gb]])
                    for k in range(8):
                        b.mm(ps[ub][:, :], WU[wb_][:, k, fi * 128:(fi + 1) * 128], H2T[:, k, :], k == 0, k == 7,
                             [rWU[wb_]] + rH2T, [pr[ub]])
                    b.act(SL[sl], ps[gb][:, :], AF.Silu, [pr[gb]], [rSL[sl]])
                    b.tt('dve', ACTT[:, ft, :], ps[ub][:, :], SL[sl], ALU.mult, [pr[ub], rSL[sl]], [rACT[ft]])
            for j in range(4):
                if nxt is not None:
                    norm(nxt, j)
                for h in range(2):
                    db = DN[dni % 2]
                    dni += 1
                    for ft in range(NFT):
                        b.mm(ps[db][:, :], ACTT[:, ft, j * 128:(j + 1) * 128], WD[:, ft, h * 512:(h + 1) * 512],
                             ft == 0, ft == NFT - 1, [rACT[ft], rWD], [pr[db]])
                    xs = XF[j][:, h * 512:(h + 1) * 512]
                    b.tt('dve', xs, xs, ps[db][:, :], ALU.add, [pr[db], rXF[j]], [rXF[j]])
                if nxt is not None:
                    trans(nxt, j)
                tt_ = st * 4 + j
                b.dma('sp', xdst[tt_ * 128:(tt_ + 1) * 128, :], XF[j], [rXF[j]], [Res()],
                      final=(xdst is self.final_out))
        sc.barrier()
        A.off = mark
        return
    for st in (st_list if st_list is not None else range(NST)):
        for j in range(4):
            tt_ = st * 4 + j
            b.dma('sp', XF[j], xsrc[tt_ * 128:(tt_ + 1) * 128, :], [], [rXF[j]])
            b.act(XN, XF[j], AF.Square, [rXF[j]], [rXN, rss], accum_out=ss)
            b.act(sd, ss, AF.Sqrt, [rss], [rsd], scale=1.0 / D, bias=EPS)
            sc.op('dve', lambda e: e.reciprocal(rstd, sd), [rsd], [rrs])
            b.act(XN, XF[j], AF.Copy, [rXF[j], rrs], [rXN], scale=rstd)
            if not moe:
                psT = self.psum_bf(T0).rearrange('p (k t) -> p k t', t=128)[:, 0:8, :]
                for k in range(8):
                    b.tr(psT[:, k, :], XN[:, k * 128:(k + 1) * 128], ident_b, [rXN, self.r_ident], [pr[T0]])
                b.tt('dve', H2T[:, :, j * 128:(j + 1) * 128], psT, gffn_b, ALU.mult, [pr[T0], P['r']], [rH2T[j]])
            else:
                for k in range(8):
                    bank = T0 if k < 4 else T1
                    b.tr(ps[bank][:, (k % 4) * 128:(k % 4 + 1) * 128], XN[:, k * 128:(k + 1) * 128], ident_f,
                         [rXN, self.r_identf], [pr[bank]])
                for hk, bank in ((0, T0), (1, T1)):
                    b.tt('dve', H32[:, hk * 4:(hk + 1) * 4, :], ps[bank][:, :].rearrange('p (k t) -> p k t', t=128),
                         P['gffn'][:, hk * 4:(hk + 1) * 4].unsqueeze(2).to_broadcast([128, 4, 128]), ALU.mult,
                         [pr[bank], P['r']], [rH32])
                b.cp('act', H2T[:, :, j * 128:(j + 1) * 128], H32, [rH32], [rH2T[j]])
                for k in range(8):
                    b.mm(ps[T0][:, 0:NE], H32[:, k, :], RT[:, k, :], k == 0, k == 7, [rH32, rRT], [pr[T0]])
                b.cp('dve', LG, ps[T0][:, 0:NE], [pr[T0]], [rLG])
                sc.op('dve', lambda e: e.max(M8, LG), [rLG], [rM8])
                b.tt('dve', PP[:, 1:2], M8[:, 1:2], M8[:, 0:1], ALU.subtract, [rM8], [rPP])
                b.act(PP[:, 1:2], PP[:, 1:2], AF.Sigmoid, [rPP], [rPP])
                b.ts('dve', PP[:, 0:1], PP[:, 1:2], -1.0, 1.0, ALU.mult, ALU.add, [rPP], [rPP])
                b.ts('dve', G0, LG, M8[:, 0:1], PP[:, 0:1], ALU.is_equal, ALU.mult, [rLG, rM8, rPP], [rG0])
                b.ts('dve', GATE[:, j, :], LG, M8[:, 1:2], PP[:, 1:2], ALU.is_equal, ALU.mult, [rLG, rM8, rPP],
                     [rGATE[j]])
                b.tt('dve', GATE[:, j, :], GATE[:, j, :], G0, ALU.add, [rGATE[j], rG0], [rGATE[j]])
        for ex in experts:
            wg_d, wu_d, wd_d = self.wb[(ex, 'gate')], self.wb[(ex, 'up')], self.wb[(ex, 'down')]
            wgv = wg_d.rearrange('(k p) f -> p k f', p=128)
            wuv = wu_d.rearrange('(k p) f -> p k f', p=128)
            for c in range(7):
                wb_ = wi % NW
                wi += 1
                b.dma('sp', WG[wb_], wgv[:, :, c * 512:(c + 1) * 512], [], [rWG[wb_]], nd=1024)
                b.dma('sp', WU[wb_], wuv[:, :, c * 512:(c + 1) * 512], [], [rWU[wb_]], nd=1024)
                if c == 0:
                    b.dma('sp', WD, wd_d.rearrange('(ft p) d -> p ft d', p=128), [], [rWD], nd=3584)
                for fi in range(4):
                    ft = c * 4 + fi
                    gb, ub = GU[(gui * 2) % 4], GU[(gui * 2 + 1) % 4]
                    sl = gui % 2
                    gui += 1
                    for k in range(8):
                        b.mm(ps[gb][:, :], WG[wb_][:, k, fi * 128:(fi + 1) * 128], H2T[:, k, :], k == 0, k == 7,
                             [rWG[wb_]] + rH2T, [pr[gb]])
                    for k in range(8):
                        b.mm(ps[ub][:, :], WU[wb_][:, k, fi * 128:(fi + 1) * 128], H2T[:, k, :], k == 0, k == 7,
                             [rWU[wb_]] + rH2T, [pr[ub]])
                    b.act(SL[sl], ps[gb][:, :], AF.Silu, [pr[gb]], [rSL[sl]])
                    b.tt('dve', ACTT[:, ft, :], ps[ub][:, :], SL[sl], ALU.mult, [pr[ub], rSL[sl]], [rACT[ft]])
            for j in range(4):
                for h in range(2):
                    db = DN[dni % 2]
                    dni += 1
                    for ft in range(NFT):
                        b.mm(ps[db][:, :], ACTT[:, ft, j * 128:(j + 1) * 128], WD[:, ft, h * 512:(h + 1) * 512],
                             ft == 0, ft == NFT - 1, [rACT[ft], rWD], [pr[db]])
                    xs = XF[j][:, h * 512:(h + 1) * 512]
                    if moe:
                        b.stt(xs, ps[db][:, :], GATE[:, j, ex:ex + 1], xs, ALU.mult, ALU.add,
                              [pr[db], rGATE[j], rXF[j]], [rXF[j]])
                    else:
                        b.tt('dve', xs, xs, ps[db][:, :], ALU.add, [pr[db], rXF[j]], [rXF[j]])
        for j in range(4):
            tt_ = st * 4 + j
            b.dma('sp', xdst[tt_ * 128:(tt_ + 1) * 128, :], XF[j], [rXF[j]], [Res()],
                  final=(xdst is self.final_out))
    sc.barrier()
    A.off = mark


Builder.phaseC = _phaseC


_CACHE = {}


def kernel(**inputs):
    n = 8
    if 'nc' not in _CACHE:
        _CACHE['nc'] = build_program()[0]
    nc = _CACHE['nc']
    shared = host_inputs(inputs, 0)
    in_maps = []
    for c in range(n):
        m = dict(shared)
        pos = np.asarray(inputs['positions'][c])
        posc = np.zeros(256, np.int32)
        posc[:255] = pos[np.arange(255) * 16 + 31]
        m['x'] = np.ascontiguousarray(inputs['x'][c])
        m['pos_t'] = np.ascontiguousarray(pos.reshape(32, 128).T)
        m['posc_t'] = np.ascontiguousarray(posc.reshape(2, 128).T)
        in_maps.append(m)
    res = run_bass_kernel_spmd(nc, in_maps, core_ids=list(range(n)))
    return np.stack([np.asarray(res.results[c]['out']) for c in range(n)], axis=0).astype(np.float32)


BLK = 512
NBLK = 2 * S // BLK + NE - 1
NSLOT = NBLK * BLK


def _setup_weight_conversion_sparse(self, inp):
    self.wb = {}
    self.bg_tasks = []
    for nm, shape in (('gate', [D, DFF]), ('up', [D, DFF]), ('down', [DFF, D])):
        src = inp['ffn_w_%s' % nm]
        dst = self.dscr('wb_d_%s' % nm, shape, BF16)
        self.wb[('d', nm)] = dst
        rows = shape[0]
        step = 256 if shape[1] > 2048 else 1024
        for r0 in range(0, rows, step):
            r1 = min(rows, r0 + step)
            ndesc = (r1 - r0) * (2 if shape[1] > 2048 else 1)

            def task(dst=dst, src=src, r0=r0, r1=r1, ndesc=ndesc):
                self.dma('pool', dst[r0:r1, :], src[r0:r1, :], [], [Res()], nd=ndesc, max_dma_last_dim=4096)
            self.bg_tasks.append(task)
    self.wgS = self.dscr('wgS', [7 * NE * 128, 8 * 512], BF16)
    self.wuS = self.dscr('wuS', [7 * NE * 128, 8 * 512], BF16)
    self.wdS = [self.dscr('wdS%d' % h, [NE * 128, 14 * D], BF16) for h in range(2)]
    for e in range(NE):
        for nm, dstT in (('gate', self.wgS), ('up', self.wuS)):
            srcv = inp['moe_w_%s' % nm][e].rearrange('(k p) f -> p k f', p=128)
            for c in range(7):
                r0 = c * 1024 + e * 128
                dstv = dstT[r0:r0 + 128, :].rearrange('p (k f) -> p k f', k=8)

                def task(dstv=dstv, srcv=srcv, c=c):
                    self.dma('pool', dstv, srcv[:, :, c * 512:(c + 1) * 512], [], [Res()], nd=1024)
                self.bg_tasks.append(task)
        srcd = inp['moe_w_down'][e].rearrange('(ft p) d -> p ft d', p=128)
        for h in range(2):
            dstd = self.wdS[h][e * 128:(e + 1) * 128, :].rearrange('p (ft d) -> p ft d', ft=14)

            def task(dstd=dstd, srcd=srcd, h=h):
                self.dma('pool', dstd, srcd[:, h * 14:(h + 1) * 14, :], [], [Res()], nd=1792)
            self.bg_tasks.append(task)


Builder.setup_weight_conversion_sparse = _setup_weight_conversion_sparse


def _phaseC_sparse(self, inp, l, xsrc, xdst, P):
    b, A, sc = self, self.arena, self.sc
    ps, pr = self.psb, self.psr
    mark = A.off
    ident_b, ident_f = self.ident_b, self.ident_f
    Xs = self.dscr('moe_xs', [NSLOT, D], BF16)
    Ys = self.dscr('moe_ys', [NSLOT, D], F32)
    DEST = [A.alloc([NT], I32) for _ in range(2)]
    PALL = A.alloc([NT, 2], F32)
    IDXW = A.alloc([NBLK, 7], I32)
    IDXD = A.alloc([NBLK], I32)
    rDEST, rPALL, rIDX = Res(), Res(), Res()
    mark1 = A.off
    XNB = A.alloc([NT, D], BF16)
    rXNB = [Res() for _ in range(NT)]
    XF = [A.alloc([D], F32) for _ in range(2)]
    rXF = [Res() for _ in range(2)]
    XN = A.alloc([D], F32)
    rXN = Res()
    ss = A.alloc([1], F32)
    sd = A.alloc([1], F32)
    rstd = A.alloc([1], F32)
    rss, rsd, rrs = Res(), Res(), Res()
    H32 = A.alloc([8, 128], F32)
    rH32 = Res()
    RT = A.alloc([8, NE], F32)
    rRT = Res()
    b.dma('sp', RT, inp['moe_router'].rearrange('(k p) e -> p k e', p=128), [], [rRT])
    LG = A.alloc([NE], F32)
    M8 = A.alloc([8], F32)
    rLG, rM8 = Res(), Res()
    OH = [A.alloc([NT, NE], F32) for _ in range(2)]
    rOH = Res()
    T0, T1 = 0, 1
    XNf = [XN, A.alloc([D], F32)]
    rXNf = [rXN, Res()]
    ss2 = [ss, A.alloc([1], F32)]
    sd2 = [sd, A.alloc([1], F32)]
    rstd2 = [rstd, A.alloc([1], F32)]
    rss2, rsd2, rrs2 = [rss, Res()], [rsd, Res()], [rrs, Res()]
    DD = A.alloc([NT], F32)
    rDD = Res()

    def pX(tt_):
        xb = tt_ % 2
        b.dma('sp', XF[xb], xsrc[tt_ * 128:(tt_ + 1) * 128, :], [], [rXF[xb]])
        b.act(XNf[xb], XF[xb], AF.Square, [rXF[xb]], [rXNf[xb], rss2[xb]], accum_out=ss2[xb])
        b.act(sd2[xb], ss2[xb], AF.Sqrt, [rss2[xb]], [rsd2[xb]], scale=1.0 / D, bias=EPS)
        sc.op('dve', lambda e, o=rstd2[xb], i=sd2[xb]: e.reciprocal(o, i), [rsd2[xb]], [rrs2[xb]])
        b.act(XNf[xb], XF[xb], AF.Copy, [rXF[xb], rrs2[xb]], [rXNf[xb]], scale=rstd2[xb])
        b.act(XNB[:, tt_, :], XF[xb], AF.Copy, [rXF[xb], rrs2[xb]], [rXNB[tt_]], scale=rstd2[xb])

    def pR(tt_):
        xb = tt_ % 2
        for k in range(8):
            bank = T0 if k < 4 else T1
            b.tr(ps[bank][:, (k % 4) * 128:(k % 4 + 1) * 128], XNf[xb][:, k * 128:(k + 1) * 128], ident_f,
                 [rXNf[xb], self.r_identf], [pr[bank]])
        for hk, bank in ((0, T0), (1, T1)):
            b.tt('dve', H32[:, hk * 4:(hk + 1) * 4, :], ps[bank][:, :].rearrange('p (k t) -> p k t', t=128),
                 P['gffn'][:, hk * 4:(hk + 1) * 4].unsqueeze(2).to_broadcast([128, 4, 128]), ALU.mult,
                 [pr[bank], P['r']], [rH32])
        for k in range(8):
            b.mm(ps[T0][:, 0:NE], H32[:, k, :], RT[:, k, :], k == 0, k == 7, [rH32, rRT], [pr[T0]])
        b.cp('dve', LG, ps[T0][:, 0:NE], [pr[T0]], [rLG])
        sc.op('dve', lambda e: e.max(M8, LG), [rLG], [rM8])
        b.tt('dve', DD[:, tt_:tt_ + 1], M8[:, 1:2], M8[:, 0:1], ALU.subtract, [rM8], [rDD])
        b.ts('dve', OH[0][:, tt_, :], LG, M8[:, 0:1], None, ALU.is_equal, None, [rLG, rM8], [rOH])
        b.ts('dve', OH[1][:, tt_, :], LG, M8[:, 1:2], None, ALU.is_equal, None, [rLG, rM8], [rOH])

    pX(0)
    for tt_ in range(NT):
        if tt_ + 1 < NT:
            pX(tt_ + 1)
        pR(tt_)
    b.act(PALL[:, :, 1], DD, AF.Sigmoid, [rDD], [rPALL])
    b.ts('dve', PALL[:, :, 0], PALL[:, :, 1], -1.0, 1.0, ALU.mult, ALU.add, [rPALL], [rPALL])
    LS = A.alloc([128], F32)
    ONESF = A.alloc([128], F32)
    rc = Res()
    b.memset('pool', ONESF, 1.0, [rc])
    sc.op('pool', lambda e: e.affine_select(out=LS, in_=ONESF, pattern=[[1, 128]], compare_op=ALU.is_ge,
                                            fill=0.0 if REGS is None else REGS['zero'], base=-1,
                                            channel_multiplier=-1), [rc], [rc])
    THR = A.alloc([8, NE], F32)
    BIDX = A.alloc([NBLK, NE], F32)
    PIDX = A.alloc([1], F32)
    sc.op('pool', lambda e: e.iota(THR, [[BLK, 8], [0, NE]], base=0, channel_multiplier=0,
                                   allow_small_or_imprecise_dtypes=True), [], [rc])
    sc.op('pool', lambda e: e.iota(BIDX, [[1, NBLK], [0, NE]], base=0, channel_multiplier=0,
                                   allow_small_or_imprecise_dtypes=True), [], [rc])
    sc.op('pool', lambda e: e.iota(PIDX, [[0, 1]], base=0, channel_multiplier=1,
                                   allow_small_or_imprecise_dtypes=True), [], [rc])
    MSK = A.alloc([NT, NE], F32)
    SA = A.alloc([NT, NE], F32)
    SB = A.alloc([NT, NE], F32)
    rM, rSA, rSB = Res(), Res(), Res()
    b.tt('dve', MSK, OH[0], OH[1], ALU.add, [rOH], [rM])
    CP = A.alloc([NE], F32)
    rCP = Res()
    sc.op('dve', lambda e: e.tensor_reduce(CP, MSK.rearrange('p t e -> p e t'), AX.X, ALU.add), [rM], [rCP])
    b.mm(ps[T1][:, 0:NE], LS, CP, True, True, [rc, rCP], [pr[T1]])
    b.mm(ps[T1][:, NE:2 * NE], ONESF, CP, True, True, [rc, rCP], [pr[T1]])
    BT = A.alloc([2 * NE], F32)
    rBT = Res()
    b.cp('dve', BT, ps[T1][:, 0:2 * NE], [pr[T1]], [rBT])
    b.cp('dve', SA, MSK, [rM], [rSA])
    cur, rcur, oth, roth = SA, rSA, SB, rSB
    for s_ in (1, 2, 4, 8, 16):
        b.cp('dve', oth[:, 0:s_, :], cur[:, 0:s_, :], [rcur], [roth])
        b.tt('dve', oth[:, s_:, :], cur[:, s_:, :], cur[:, 0:NT - s_, :], ALU.add, [rcur], [roth])
        cur, rcur, oth, roth = oth, roth, cur, rcur
    b.tt('dve', oth, cur, MSK, ALU.subtract, [rcur, rM], [roth])
    b.tt('dve', cur, oth, BT[:, 0:NE].unsqueeze(1).to_broadcast([128, NT, NE]), ALU.add, [roth, rBT], [rcur])
    RANK, rRANK = cur, rcur
    SCR, rSCR = oth, roth
    NBM = A.alloc([8, NE], F32)
    NBv = A.alloc([NE], F32)
    PB = A.alloc([NE], F32)
    PB2 = A.alloc([NE], F32)
    PEND = A.alloc([NE], F32)
    rN = Res()
    b.tt('dve', NBM, BT[:, NE:2 * NE].unsqueeze(1).to_broadcast([128, 8, NE]), THR, ALU.is_gt, [rBT, rc], [rN])
    sc.op('dve', lambda e: e.tensor_reduce(NBv, NBM.rearrange('p m e -> p e m'), AX.X, ALU.add), [rN], [rN])
    b.cp('dve', PB, NBv, [rN], [rN])
    src_, dst_ = PB, PB2
    for s_ in (1, 2, 4):
        b.cp('dve', dst_[:, 0:s_], src_[:, 0:s_], [rN], [rN])
        b.tt('dve', dst_[:, s_:], src_[:, s_:], src_[:, 0:NE - s_], ALU.add, [rN], [rN])
        src_, dst_ = dst_, src_
    b.cp('dve', PEND, src_, [rN], [rN])
    b.tt('dve', dst_, src_, NBv, ALU.subtract, [rN], [rN])
    PBX = dst_
    b.ts('dve', src_, PBX, float(BLK), None, ALU.mult, None, [rN], [rN])
    PS_ = src_
    b.tt('dve', SCR, RANK, PS_.unsqueeze(1).to_broadcast([128, NT, NE]), ALU.add, [rRANK, rN], [rSCR])
    DF = A.alloc([NT], F32)
    rDF = Res()
    for k in range(2):
        b.tt('dve', RANK, SCR, OH[k], ALU.mult, [rSCR, rOH], [rRANK])
        sc.op('dve', lambda e: e.tensor_reduce(DF, RANK, AX.X, ALU.add), [rRANK], [rDF])
        b.cp('dve', DEST[k], DF, [rDF], [rDEST])
    CMPB = A.alloc([NBLK, NE], F32)
    BE = A.alloc([NBLK], F32)
    BE2 = A.alloc([NBLK], F32)
    rB = Res()
    b.tt('dve', CMPB, PEND.unsqueeze(1).to_broadcast([128, NBLK, NE]), BIDX, ALU.is_le, [rN, rc], [rB])
    sc.op('dve', lambda e: e.tensor_reduce(BE, CMPB, AX.X, ALU.add), [rB], [rB])
    b.ts('dve', BE2, BE, float(NE - 1), 128.0, ALU.min, ALU.mult, [rB], [rB])
    b.ts('dve', BE, BE2, PIDX[:, 0:1], None, ALU.add, None, [rB, rc], [rB])
    b.cp('dve', IDXD, BE, [rB], [rIDX])
    for c in range(7):
        b.ts('dve', BE2, BE, float(c * 1024), None, ALU.add, None, [rB], [rB])
        b.cp('dve', IDXW[:, :, c], BE2, [rB], [rIDX])
    for tt_ in range(NT):
        for k in range(2):
            sc.dma('pool', lambda e, tt_=tt_, k=k: e.indirect_dma_start(
                out=Xs[:, :], out_offset=bass.IndirectOffsetOnAxis(ap=DEST[k][:, tt_:tt_ + 1], axis=0),
                in_=XNB[:, tt_, :], in_offset=None), [rXNB[tt_], rDEST], [Res()], nd=128)
    if self.dbg.get('moe_dump'):
        self.dump('dest0', DEST[0], [rDEST], [128, NT], I32)
        self.dump('dest1', DEST[1], [rDEST], [128, NT], I32)
        self.dump('pall', PALL, [rPALL], [128, NT, 2], F32)
        self.dump('idxd', IDXD, [rIDX], [128, NBLK], I32)
        self.dump('idxw', IDXW, [rIDX], [128, NBLK, 7], I32)
    sc.barrier()
    A.off = mark1
    XS = [A.alloc([D], BF16) for _ in range(4)]
    rXS = [Res() for _ in range(4)]
    H2T = A.alloc([8, 512], BF16)
    rH2T = [Res() for _ in range(4)]
    ACTT = A.alloc([NFT, 512], BF16)
    rACT = [Res() for _ in range(NFT)]
    WD = A.alloc([NFT, D], BF16)
    rWD = Res()
    NW = 2
    WG = [A.alloc([8, 512], BF16) for _ in range(NW)]
    WU = [A.alloc([8, 512], BF16) for _ in range(NW)]
    rWG = [Res() for _ in range(NW)]
    rWU = [Res() for _ in range(NW)]
    SL = [A.alloc([512], F32) for _ in range(2)]
    rSL = [Res() for _ in range(2)]
    YB = [A.alloc([D], F32) for _ in range(2)]
    rYB = [Res() for _ in range(2)]
    rWDh = [Res() for _ in range(2)]
    GU = (2, 3, 4, 5)
    DN = (6, 7)
    gui = dni = wi = yi = 0
    gffn_b = P['gffn'].unsqueeze(2).to_broadcast([128, 8, 128])
    psT = self.psum_bf(T0).rearrange('p (k t) -> p k t', t=128)[:, 0:8, :]
    XS2 = [XS, [A.alloc([D], BF16) for _ in range(4)]]
    rXS2 = [rXS, [Res() for _ in range(4)]]

    def load_xs(blk):
        for j in range(4):
            r0 = blk * BLK + j * 128
            b.dma('sp', XS2[blk % 2][j], Xs[r0:r0 + 128, :], [], [rXS2[blk % 2][j]])

    def trans_xs(blk, j):
        xs_, rxs_ = XS2[blk % 2][j], rXS2[blk % 2][j]
        for k in range(8):
            b.tr(psT[:, k, :], xs_[:, k * 128:(k + 1) * 128], ident_b, [rxs_, self.r_ident], [pr[T0]])
        b.tt('dve', H2T[:, :, j * 128:(j + 1) * 128], psT, gffn_b, ALU.mult, [pr[T0], P['r']], [rH2T[j]])

    load_xs(0)
    for j in range(4):
        trans_xs(0, j)
    for blk in range(NBLK):
        if blk + 1 < NBLK:
            load_xs(blk + 1)
        def gather_chunk(blk, c):
            wb2 = (blk * 7 + c) % NW
            sc.dma('pool', lambda e, wb2=wb2, blk=blk, c=c: e.indirect_dma_start(
                out=WG[wb2].rearrange('p k f -> p (k f)'), out_offset=None, in_=self.wgS[:, :],
                in_offset=bass.IndirectOffsetOnAxis(ap=IDXW[:, blk, c:c + 1], axis=0)), [rIDX], [rWG[wb2]], nd=512)
            sc.dma('pool', lambda e, wb2=wb2, blk=blk, c=c: e.indirect_dma_start(
                out=WU[wb2].rearrange('p k f -> p (k f)'), out_offset=None, in_=self.wuS[:, :],
                in_offset=bass.IndirectOffsetOnAxis(ap=IDXW[:, blk, c:c + 1], axis=0)), [rIDX], [rWU[wb2]], nd=512)

        def gather_wd(blk, h):
            sc.dma('pool', lambda e, blk=blk, h=h: e.indirect_dma_start(
                out=WD[:, h * 14:(h + 1) * 14, :].rearrange('p ft d -> p (ft d)'), out_offset=None,
                in_=self.wdS[h][:, :], in_offset=bass.IndirectOffsetOnAxis(ap=IDXD[:, blk:blk + 1], axis=0)),
                [rIDX], [rWDh[h]], nd=1024)

        if blk == 0:
            gather_chunk(0, 0)
            gather_chunk(0, 1)
        gather_wd(blk, 0)
        for c in range(7):
            wb_ = (blk * 7 + c) % NW
            if c == 2:
                gather_wd(blk, 1)
            for fi in range(4):
                ft = c * 4 + fi
                gb, ub = GU[(gui * 2) % 4], GU[(gui * 2 + 1) % 4]
                sl = gui % 2
                gui += 1
                for k in range(8):
                    b.mm(ps[gb][:, :], WG[wb_][:, k, fi * 128:(fi + 1) * 128], H2T[:, k, :], k == 0, k == 7,
                         [rWG[wb_]] + rH2T, [pr[gb]])
                for k in range(8):
                    b.mm(ps[ub][:, :], WU[wb_][:, k, fi * 128:(fi + 1) * 128], H2T[:, k, :], k == 0, k == 7,
                         [rWU[wb_]] + rH2T, [pr[ub]])
                b.act(SL[sl], ps[gb][:, :], AF.Silu, [pr[gb]], [rSL[sl]])
                b.tt('dve', ACTT[:, ft, :], ps[ub][:, :], SL[sl], ALU.mult, [pr[ub], rSL[sl]], [rACT[ft]])
            nc_, nb_ = c + 2, blk
            if nc_ >= 7:
                nc_, nb_ = nc_ - 7, blk + 1
            if nb_ < NBLK:
                gather_chunk(nb_, nc_)
        for j in range(4):
            yb = yi % 2
            yi += 1
            for h in range(2):
                db = DN[dni % 2]
                dni += 1
                for ft in range(NFT):
                    b.mm(ps[db][:, :], ACTT[:, ft, j * 128:(j + 1) * 128], WD[:, ft, h * 512:(h + 1) * 512],
                         ft == 0, ft == NFT - 1, [rACT[ft], rWDh[ft // 14]], [pr[db]])
                if h == 0:
                    b.cp('act', YB[yb][:, 0:512], ps[db][:, :], [pr[db]], [rYB[yb]])
                else:
                    b.cp('dve', YB[yb][:, 512:1024], ps[db][:, :], [pr[db]], [rYB[yb]])
            r0 = blk * BLK + j * 128
            b.dma('sp', Ys[r0:r0 + 128, :], YB[yb], [rYB[yb]], [Res()])
            if blk + 1 < NBLK:
                trans_xs(blk + 1, j)
    sc.barrier()
    A.off = mark1
    NC3 = 6
    XC = [A.alloc([D], F32) for _ in range(NC3)]
    Y0 = [A.alloc([D], F32) for _ in range(NC3)]
    Y1 = [A.alloc([D], F32) for _ in range(NC3)]
    rXC = [Res() for _ in range(NC3)]
    rY0 = [Res() for _ in range(NC3)]
    rY1 = [Res() for _ in range(NC3)]
    for tt_ in range(NT):
        xb = tt_ % NC3
        b.dma('sp', XC[xb], xsrc[tt_ * 128:(tt_ + 1) * 128, :], [], [rXC[xb]])
        for k, (Yk, rYk) in enumerate(((Y0, rY0), (Y1, rY1))):
            sc.dma('pool', lambda e, Yk=Yk, k=k, tt_=tt_, xb=xb: e.indirect_dma_start(
                out=Yk[xb][:, :], out_offset=None, in_=Ys[:, :],
                in_offset=bass.IndirectOffsetOnAxis(ap=DEST[k][:, tt_:tt_ + 1], axis=0)), [rDEST], [rYk[xb]], nd=128)
        b.stt(XC[xb], Y0[xb], PALL[:, tt_, 0:1], XC[xb], ALU.mult, ALU.add, [rY0[xb], rPALL, rXC[xb]], [rXC[xb]])
        b.stt(XC[xb], Y1[xb], PALL[:, tt_, 1:2], XC[xb], ALU.mult, ALU.add, [rY1[xb], rPALL, rXC[xb]], [rXC[xb]])
        b.dma('sp', xdst[tt_ * 128:(tt_ + 1) * 128, :], XC[xb], [rXC[xb]], [Res()],
              final=(xdst is self.final_out))
    sc.barrier()
    A.off = mark


Builder.phaseC_sparse = _phaseC_sparse
```

```python
import math
import numpy as np
import concourse.bass as bass
import concourse.mybir as mybir
from concourse.bass_utils import run_bass_kernel_spmd

F32 = mybir.dt.float32
BF16 = mybir.dt.bfloat16
I32 = mybir.dt.int32
AF = mybir.ActivationFunctionType
ALU = mybir.AluOpType
AX = mybir.AxisListType

S = 4096
D = 1024
NT = S // 128
NST = S // 512
DFF = 3584
NFT = DFF // 128
NE = 8
EPS = 1e-6
NEG = -30000.0
ENGS = ('pe', 'act', 'dve', 'pool', 'sp')
REGS = None


class Res:
    __slots__ = ('name', 'w', 'rs', 'excl')

    def __init__(self, name='', excl=False):
        self.name = name
        self.w = None
        self.rs = {}
        self.excl = excl


class Sched:
    NEAR = 4

    def __init__(self, n_dma_sems=32):
        self.streams = {e: [] for e in ENGS}
        self.cnt = {e: 0 for e in ENGS}
        self.seen = {e: {} for e in ENGS}
        self.dcnt = [0] * n_dma_sems
        self.dnext = 0
        self.final = []
        self.serialize_dma = False
        self.inflight = {}

    def op(self, eng, fn, reads=(), writes=()):
        idx = self.cnt[eng] + 1
        me = ('e', eng)
        deps = {}
        same_raw = 0
        for r in reads:
            if r.w is not None:
                k, v = r.w
                if k == me:
                    if v > same_raw:
                        same_raw = v
                elif deps.get(k, 0) < v:
                    deps[k] = v
            if r.excl:
                for k, v in r.rs.items():
                    if k != me and deps.get(k, 0) < v:
                        deps[k] = v
        for r in writes:
            if r.w is not None:
                k, v = r.w
                if k != me and deps.get(k, 0) < v:
                    deps[k] = v
            for k, v in r.rs.items():
                if k != me and deps.get(k, 0) < v:
                    deps[k] = v
        waits = []
        if same_raw and idx - same_raw <= self.NEAR:
            waits.append((me, same_raw))
        sn = self.seen[eng]
        for k, v in deps.items():
            if sn.get(k, 0) < v:
                sn[k] = v
                waits.append((k, v))
        self.streams[eng].append((waits, fn, me))
        self.cnt[eng] = idx
        tok = (me, idx)
        for r in reads:
            if r.rs.get(me, 0) < idx:
                r.rs[me] = idx
        for r in writes:
            r.w = tok
            r.rs = {}
        return tok

    def dma(self, q, fn, reads=(), writes=(), final=False, nd=128):
        k = self.dnext
        self.dnext = (self.dnext + 1) % len(self.dcnt)
        v = self.dcnt[k] + 16
        dk = ('d', k)
        deps = {}
        if self.dcnt[k] > 0:
            deps[dk] = self.dcnt[k]
        fl = self.inflight.setdefault(q, [])
        cap_n = 10 if q == 'pool' else 12
        cap_d = 9000 if q == 'pool' else 1 << 30
        while fl and (len(fl) >= cap_n or sum(x[2] for x in fl) + nd > cap_d):
            ok, ov, _ = fl.pop(0)
            if deps.get(ok, 0) < ov:
                deps[ok] = ov
        fl.append((dk, v, nd))
        for r in reads:
            if r.w is not None:
                k2, v2 = r.w
                if deps.get(k2, 0) < v2:
                    deps[k2] = v2
        for r in writes:
            if r.w is not None:
                k2, v2 = r.w
                if deps.get(k2, 0) < v2:
                    deps[k2] = v2
            for k2, v2 in r.rs.items():
                if deps.get(k2, 0) < v2:
                    deps[k2] = v2
        sn = self.seen[q]
        waits = []
        for k2, v2 in deps.items():
            if sn.get(k2, 0) < v2:
                sn[k2] = v2
                waits.append((k2, v2))
        self.streams[q].append((waits, fn, dk))
        self.dcnt[k] = v
        tok = (dk, v)
        for r in reads:
            if r.rs.get(dk, 0) < v:
                r.rs[dk] = v
        for r in writes:
            r.w = tok
            r.rs = {}
        if final:
            self.final.append(tok)
        if self.serialize_dma:
            sn[dk] = v
            self.streams[q].append(([(dk, v)], None, None))
        return tok

    def barrier(self):
        snap_e = dict(self.cnt)
        snap_d = list(self.dcnt)
        for e in ENGS:
            waits = []
            sn = self.seen[e]
            for e2 in ENGS:
                k = ('e', e2)
                if e2 != e and snap_e[e2] > 0 and sn.get(k, 0) < snap_e[e2]:
                    sn[k] = snap_e[e2]
                    waits.append((k, snap_e[e2]))
            for i, v in enumerate(snap_d):
                k = ('d', i)
                if v > 0 and sn.get(k, 0) < v:
                    sn[k] = v
                    waits.append((k, v))
            self.streams[e].append((waits, None, None))

    def emit(self, nc, stack):
        esem = {e: stack.enter_context(nc.semaphore('es_' + e)) for e in ENGS}
        dsem = [stack.enter_context(nc.semaphore('ds_%d' % i)) for i in range(len(self.dcnt))]

        def semof(k):
            return esem[k[1]] if k[0] == 'e' else dsem[k[1]]

        fin = {}
        for k, v in self.final:
            if fin.get(k, 0) < v:
                fin[k] = v

        def make(name):
            def body(e):
                if name == 'pool':
                    global REGS
                    REGS = {'zero': e.to_reg(0.0), 'neg': e.to_reg(NEG)}
                for waits, fn, sig in self.streams[name]:
                    for (k, v) in waits:
                        e.wait_ge(semof(k), v)
                    if fn is None:
                        continue
                    ins = fn(e)
                    if sig[0] == 'e':
                        ins.then_inc(esem[sig[1]], 1)
                    else:
                        ins.then_inc(dsem[sig[1]], 16)
                if name == 'sp':
                    for k, v in fin.items():
                        e.wait_ge(semof(k), v)
            return body

        with nc.Block() as block:
            block.tensor(make('pe'))
            block.scalar(make('act'))
            block.vector(make('dve'))
            block.gpsimd(make('pool'))
            block.sync(make('sp'))


def _dtsize(dt):
    return {F32: 4, BF16: 2, I32: 4}[dt]


class Arena:
    def __init__(self, ap_u8, size):
        self.ap = ap_u8
        self.size = size
        self.off = 0

    def alloc(self, free_shape, dt):
        n = 1
        for s in free_shape:
            n *= s
        nb = n * _dtsize(dt)
        nb_al = (nb + 63) // 64 * 64
        assert self.off + nb_al <= self.size, ("SBUF arena overflow", self.off, nb_al, self.size)
        a = self.ap[:, self.off:self.off + nb].bitcast(dt)
        self.off += nb_al
        if len(free_shape) > 1:
            names = ' '.join('a%d' % i for i in range(len(free_shape)))
            kw = {'a%d' % i: free_shape[i] for i in range(1, len(free_shape))}
            a = a.rearrange('p (%s) -> p %s' % (names, names), **kw)
        return a


def _ROPE_INV_FREQ():
    return [float(np.float32(500000.0) ** np.float32(-2.0 * i / 16.0)) for i in range(8)]


W_BLOCKS = [
    (0, 0, 512),
    (512, 768, 128),
    (640, 1024, 128),
    (768, 896, 128),
    (896, 1152, 128),
    (1024, 1280, 24),
    (1048, 512, 128),
    (1176, 640, 128),
    (1304, 1304, 1024),
]
WIN = 2328


class Builder:
    def __init__(self, nc, stack, dbg=None):
        self.bg_tasks = []
        self.final_out = None
        self.nc = nc
        self.stack = stack
        self.sc = Sched()
        self.dbg = dbg or {}
        self.dbg_out = {}
        arena_t = stack.enter_context(nc.sbuf_tensor('arena', [128, 175 * 1024], mybir.dt.uint8))
        self.arena = Arena(arena_t, 175 * 1024)
        self.psb = []
        self.psr = []
        for i in range(8):
            t = stack.enter_context(nc.psum_tensor('psb%d' % i, [128, 512], F32))
            self.psb.append(t)
            self.psr.append(Res('ps%d' % i, excl=True))

    def din(self, name, shape, dt=F32):
        return self.nc.dram_tensor(name, list(shape), dt, kind='ExternalInput').ap()

    def dout(self, name, shape, dt=F32):
        return self.nc.dram_tensor(name, list(shape), dt, kind='ExternalOutput').ap()

    def dscr(self, name, shape, dt=F32):
        return self.nc.dram_tensor(name, list(shape), dt).ap()

    def dump(self, name, ap, res_list, shape, dt=F32):
        o = self.dout('dbg_' + name, shape, dt)
        self.dbg_out['dbg_' + name] = (shape, dt)
        self.sc.dma('sp', lambda e, o=o, ap=ap: e.dma_start(out=o, in_=ap), reads=res_list, writes=[Res()],
                    final=True)

    def mm(self, out, lhsT, rhs, start, stop, reads, writes):
        self.sc.op('pe', lambda e: e.matmul(out, lhsT, rhs, start=start, stop=stop), reads, writes)

    def tr(self, out, in_, ident, reads, writes):
        self.sc.op('pe', lambda e: e.transpose(out, in_, ident), reads, writes)

    def act(self, out, in_, func, reads, writes, **kw):
        self.sc.op('act', lambda e: e.activation(out, in_, func, **kw), reads, writes)

    def ts(self, eng, out, in0, s1, s2, op0, op1, reads, writes):
        if op1 is None:
            self.sc.op(eng, lambda e: e.tensor_scalar(out, in0, s1, None, op0), reads, writes)
        else:
            self.sc.op(eng, lambda e: e.tensor_scalar(out, in0, s1, s2, op0, op1), reads, writes)

    def tt(self, eng, out, in0, in1, op, reads, writes):
        self.sc.op(eng, lambda e: e.tensor_tensor(out, in0, in1, op), reads, writes)

    def stt(self, out, in0, scalar, in1, op0, op1, reads, writes):
        self.sc.op('dve', lambda e: e.scalar_tensor_tensor(out, in0, scalar, in1, op0, op1), reads, writes)

    def cp(self, eng, out, in_, reads, writes):
        if eng == 'act':
            self.sc.op('act', lambda e: e.copy(out, in_), reads, writes)
        else:
            self.sc.op(eng, lambda e: e.tensor_copy(out, in_), reads, writes)

    def memset(self, eng, ap, val, writes):
        self.sc.op(eng, lambda e: e.memset(ap, val), (), writes)

    def dma(self, q, out, in_, reads, writes, final=False, nd=128, **kw):
        return self.sc.dma(q, lambda e: e.dma_start(out=out, in_=in_, **kw), reads, writes, final=final, nd=nd)

    def psum_bf(self, i):
        return self.psb[i][:, :].bitcast(BF16)

    def phase0(self, inp):
        b, A = self, self.arena
        self.ones_f = A.alloc([128], F32)
        self.ident_f = A.alloc([128], F32)
        self.ident_b = A.alloc([128], BF16)
        r1, r2, r3 = Res(), Res(), Res()
        b.memset('pool', self.ones_f, 1.0, [r1])
        ones_f, ident_f = self.ones_f, self.ident_f
        b.sc.op('pool', lambda e: e.affine_select(out=ident_f, in_=ones_f, pattern=[[-1, 128]],
                                                  compare_op=ALU.is_equal, fill=0.0 if REGS is None else REGS['zero'], base=0,
                                                  channel_multiplier=1), [r1], [r2])
        b.cp('pool', self.ident_b, self.ident_f, [r2], [r3])
        self.r_ident = r3
        self.r_identf = r2

        self.causal01 = A.alloc([128], BF16)
        self.low01 = A.alloc([128], BF16)
        self.ones_b = A.alloc([128], BF16)
        self.r_masks = Res()
        b.memset('pool', self.ones_b, 1.0, [self.r_masks])
        ones_b, causal01, low01 = self.ones_b, self.causal01, self.low01
        self.sc.op('pool', lambda e: e.affine_select(out=causal01, in_=ones_b, pattern=[[1, 128]],
                   compare_op=ALU.is_ge, fill=0.0 if REGS is None else REGS['zero'], base=0, channel_multiplier=-1), [self.r_masks], [self.r_masks])
        self.sc.op('pool', lambda e: e.affine_select(out=low01, in_=ones_b, pattern=[[-1, 128]],
                   compare_op=ALU.is_ge, fill=0.0 if REGS is None else REGS['zero'], base=-1, channel_multiplier=1), [self.r_masks], [self.r_masks])
        self.zeros_b4 = A.alloc([4, 128], BF16)
        self.causalN = A.alloc([4, 128], BF16)
        self.lowN = A.alloc([4, 128], BF16)
        b.memset('pool', self.zeros_b4, 0.0, [self.r_masks])
        zeros_b4, causalN, lowN = self.zeros_b4, self.causalN, self.lowN
        self.sc.op('pool', lambda e: e.affine_select(out=causalN, in_=zeros_b4, pattern=[[0, 4], [1, 128]],
                   compare_op=ALU.is_ge, fill=NEG if REGS is None else REGS['neg'], base=0,
                   channel_multiplier=-1), [self.r_masks], [self.r_masks])
        self.sc.op('pool', lambda e: e.affine_select(out=lowN, in_=zeros_b4, pattern=[[0, 4], [-1, 128]],
                   compare_op=ALU.is_ge, fill=NEG if REGS is None else REGS['neg'], base=-1,
                   channel_multiplier=1), [self.r_masks], [self.r_masks])
        self.sinT = A.alloc([34, 8], F32)
        self.cosT = A.alloc([34, 8], F32)
        mark0 = A.off
        pos_i = A.alloc([34], I32)
        rp = Res()
        b.dma('sp', pos_i[:, 0:32], inp['pos_t'], [], [rp])
        b.dma('sp', pos_i[:, 32:34], inp['posc_t'], [], [rp])
        pos_f = A.alloc([34], F32)
        rpf = Res()
        b.cp('dve', pos_f, pos_i, [rp], [rpf])
        invf = A.alloc([8], F32)
        rif = Res()
        for i, v in enumerate(_ROPE_INV_FREQ()):
            b.memset('pool', invf[:, i:i + 1], v, [rif])
        ang = A.alloc([34, 8], F32)
        angc = A.alloc([34, 8], F32)
        ra, rac = Res(), Res()
        b.tt('dve', ang, pos_f.unsqueeze(2).to_broadcast([128, 34, 8]),
             invf.unsqueeze(1).to_broadcast([128, 34, 8]), ALU.mult, [rpf, rif], [ra])
        b.ts('dve', angc, ang, math.pi / 2, None, ALU.add, None, [ra], [rac])
        self.r_rope = Res()
        MAGIC = 12582912.0
        TWO_PI = 2.0 * math.pi
        for src, rsrc, dst in ((ang, ra, self.sinT), (angc, rac, self.cosT)):
            u = A.alloc([34, 8], F32)
            k2 = A.alloc([34, 8], F32)
            ru, rk = Res(), Res()
            b.ts('dve', u, src, 1.0 / TWO_PI, MAGIC, ALU.mult, ALU.add, [rsrc], [ru])
            b.ts('dve', k2, u, -MAGIC, TWO_PI, ALU.add, ALU.mult, [ru], [rk])
            b.tt('dve', u, src, k2, ALU.subtract, [rsrc, rk], [ru])
            b.ts('dve', k2, u, -3.1415, 3.1415, ALU.max, ALU.min, [ru], [rk])
            b.act(dst, k2, AF.Sin, [rk], [self.r_rope])
        b.sc.barrier()
        A.off = mark0

    def load_layer_small(self, inp, l):
        b, A = self, self.arena
        P = {}
        r = Res()
        P['r'] = r
        P['gin'] = A.alloc([8], F32)
        b.dma('sp', P['gin'], inp['gin_t'][l], [], [r])
        P['gffn'] = A.alloc([8], F32)
        b.dma('sp', P['gffn'], inp['gffn_t'][l], [], [r])
        gq = A.alloc([64], F32)
        gk = A.alloc([3, 64], F32)
        r0 = Res()
        b.dma('sp', gq, inp['q_norm_g'][l].partition_broadcast(128), [], [r0])
        b.dma('sp', gk, inp['k_norm_g'][l].rearrange('a b -> (a b)').partition_broadcast(128)
              .rearrange('p (a b) -> p a b', b=64), [], [r0])
        P['gk'] = gk
        gqk = A.alloc([12, 64], F32)
        b.ts('dve', gqk[:, 0:8, :], gq.unsqueeze(1).to_broadcast([128, 8, 64]), 0.125, None, ALU.mult, None,
             [r0], [r])
        b.cp('dve', gqk[:, 8:10, :], gk[:, 1:2, :].to_broadcast([128, 2, 64]), [r0], [r])
        b.cp('dve', gqk[:, 10:12, :], gk[:, 2:3, :].to_broadcast([128, 2, 64]), [r0], [r])
        P['gqk'] = gqk
        return P

    def phaseA(self, inp, l, xsrc, P, st_list=None):
        b, A, sc = self, self.arena, self.sc
        self.KE = [A.alloc([S], BF16) for _ in range(2)]
        self.r_E = Res()
        for kh in range(2):
            reg = self.KE[kh][64:128, :] if kh == 0 else self.KE[kh][0:64, :]
            b.memset('pool', reg, 1.0, [self.r_E])
            sc_ = self.sc
            sc_.op('pool', lambda e, reg=reg: e.affine_select(out=reg, in_=reg, pattern=[[1, S]],
                   compare_op=ALU.is_ge, fill=0.0 if REGS is None else REGS['zero'], base=0, channel_multiplier=-64), [self.r_E], [self.r_E])
            sc_.op('pool', lambda e, reg=reg: e.affine_select(out=reg, in_=reg, pattern=[[-1, S]],
                   compare_op=ALU.is_ge, fill=0.0 if REGS is None else REGS['zero'], base=63, channel_multiplier=64), [self.r_E], [self.r_E])
        self.QT = A.alloc([4, S], BF16)
        self.KwT = A.alloc([S], BF16)
        self.VsA = A.alloc([NT, 2, 65], BF16)
        self.VwA = A.alloc([NT, 2, 65], BF16)
        self.gates = A.alloc([NT, 24], F32)
        self.KcmpT = A.alloc([256], BF16)
        self.VcA = A.alloc([2, 2, 128], BF16)
        self.markKc = A.off
        self.r_q = [Res() for _ in range(NT)]
        self.r_ks = [Res() for _ in range(NT)]
        self.r_kw = [Res() for _ in range(NT)]
        self.r_vs = [Res() for _ in range(NT)]
        self.r_vw = [Res() for _ in range(NT)]
        self.r_g = [Res() for _ in range(NT)]
        self.r_kc = [Res() for _ in range(NST)]
        self.r_vc = [Res() for _ in range(NST)]
        rones = Res()
        b.memset('pool', self.VsA[:, :, :, 64:65], 1.0, [rones])
        b.memset('pool', self.VwA[:, :, :, 64:65], 1.0, [rones])
        for t in range(NT):
            self.r_vs[t].w = rones.w
            self.r_vw[t].w = rones.w
        mark = A.off
        W = A.alloc([8, WIN], BF16)
        rW = [Res() for _ in W_BLOCKS]
        wsrc = inp['w_in'][l].rearrange('(k p) c -> p k c', p=128)
        for i, (do, so, n) in enumerate(W_BLOCKS):
            b.dma('pool', W[:, :, do:do + n], wsrc[:, :, so:so + n], [], [rW[i]], nd=1024)
        XB = [A.alloc([D], F32) for _ in range(2)]
        rXB = [Res() for _ in range(2)]
        XN = [A.alloc([D], BF16) for _ in range(2)]
        rXN = [Res() for _ in range(2)]
        HT = [A.alloc([8, 512], BF16) for _ in range(2)]
        rHT = [[Res() for _ in range(4)] for _ in range(2)]
        KCt = [A.alloc([512], BF16) for _ in range(2)]
        rKCt = [Res() for _ in range(2)]
        ss = [A.alloc([1], F32) for _ in range(2)]
        sd = [A.alloc([1], F32) for _ in range(2)]
        rstd = [A.alloc([1], F32) for _ in range(2)]
        rss = [Res() for _ in range(2)]
        rsd = [Res() for _ in range(2)]
        rrs = [Res() for _ in range(2)]

        SQ = A.alloc([12, 64], F32)
        rSQ = Res()
        ss12 = A.alloc([12], F32)
        sd12 = A.alloc([12], F32)
        rs12 = A.alloc([12], F32)
        r12a, r12b, r12c = Res(), Res(), Res()
        T1s = [A.alloc([12, 64], F32) for _ in range(2)]
        rT1s = [Res() for _ in range(2)]
        RA = A.alloc([4, 12, 8], F32)
        rRA = [Res() for _ in range(4)]
        QBs = [A.alloc([12, 64], BF16) for _ in range(2)]
        rQBs = [Res() for _ in range(2)]
        SIG = [A.alloc([512], BF16) for _ in range(4)]
        rSIG = [Res() for _ in range(4)]
        HGt = [A.alloc([512], BF16) for _ in range(2)]
        rHGt = [Res() for _ in range(2)]
        zt = A.alloc([32], BF16)
        rz = Res()
        b.memset('pool', zt, 0.0, [rz])
        hg = self.hg_scr
        for ct in range(4):
            b.dma('sp', hg[ct][:, 0:30], zt[:, 0:30], [rz], [Res()])
        ps, pr = self.psb, self.psr
        PT, PA, PB, PC, PQ, PF0, PF1 = 0, 1, 2, 3, 4, 5, 6
        psT = self.psum_bf(PT).rearrange('p (k t) -> p k t', t=128)[:, 0:8, :]
        psQ = self.psum_bf(PQ).rearrange('p (k t) -> p k t', t=128)[:, 0:6, :]
        gin_b = P['gin'].unsqueeze(2).to_broadcast([128, 8, 128])
        gqk = P['gqk']
        ident_b = self.ident_b
        fstate = {'fidx': 0, 'kci': 0}
        sts = list(st_list if st_list is not None else range(NST))
        tiles = [(st, j) for st in sts for j in range(4)]

        def stageX(st, j):
            hb = st % 2
            tt_ = st * 4 + j
            xb = tt_ % 2
            b.bg(1)
            b.dma('sp', XB[xb], xsrc[tt_ * 128:(tt_ + 1) * 128, :], [], [rXB[xb]])
            b.act(XN[xb], XB[xb], AF.Square, [rXB[xb]], [rXN[xb], rss[xb]], accum_out=ss[xb])
            b.act(sd[xb], ss[xb], AF.Sqrt, [rss[xb]], [rsd[xb]], scale=1.0 / D, bias=EPS)
            sc.op('dve', lambda e, o=rstd[xb], i=sd[xb]: e.reciprocal(o, i), [rsd[xb]], [rrs[xb]])
            b.act(XN[xb], XB[xb], AF.Copy, [rXB[xb], rrs[xb]], [rXN[xb]], scale=rstd[xb])
            for k in range(8):
                b.tr(psT[:, k, :], XN[xb][:, k * 128:(k + 1) * 128], ident_b, [rXN[xb], self.r_ident], [pr[PT]])
            b.tt('dve', HT[hb][:, :, j * 128:(j + 1) * 128], psT, gin_b, ALU.mult, [pr[PT], P['r']], [rHT[hb][j]])

        def stageYmm(st, j):
            hb = st % 2
            for bank, c0, c1, rw in ((PA, 0, 512, [rW[0]]), (PB, 512, 1024, rW[1:5]), (PC, 1024, 1048, [rW[5]])):
                for k in range(8):
                    b.mm(ps[bank][:, 0:c1 - c0], HT[hb][:, k, j * 128:(j + 1) * 128], W[:, k, c0:c1],
                         k == 0, k == 7, [rHT[hb][j]] + rw, [pr[bank]])

        def stageYpost(st, j):
            tt_ = st * 4 + j
            T1, rT1 = T1s[tt_ % 2], rT1s[tt_ % 2]
            b.act(SQ[:, 0:8, :], ps[PA][:, 0:512].rearrange('p (h d) -> p h d', d=64), AF.Square, [pr[PA]], [rSQ])
            b.act(SQ[:, 8:12, :], ps[PB][:, 0:256].rearrange('p (h d) -> p h d', d=64), AF.Square, [pr[PB]], [rSQ])
            sc.op('dve', lambda e: e.tensor_reduce(ss12, SQ, AX.X, ALU.add), [rSQ], [r12a])
            b.act(sd12, ss12, AF.Sqrt, [r12a], [r12b], scale=1.0 / 64, bias=EPS)
            sc.op('dve', lambda e: e.reciprocal(rs12, sd12), [r12b], [r12c])
            b.tt('dve', T1[:, 0:8, :], ps[PA][:, 0:512].rearrange('p (h d) -> p h d', d=64),
                 rs12[:, 0:8].unsqueeze(2).to_broadcast([128, 8, 64]), ALU.mult, [pr[PA], r12c], [rT1])
            b.tt('dve', T1[:, 8:12, :], ps[PB][:, 0:256].rearrange('p (h d) -> p h d', d=64),
                 rs12[:, 8:12].unsqueeze(2).to_broadcast([128, 4, 64]), ALU.mult, [pr[PB], r12c], [rT1])
            b.cp('dve', self.VsA[:, tt_, :, 0:64], ps[PB][:, 256:384].rearrange('p (h d) -> p h d', d=64),
                 [pr[PB]], [self.r_vs[tt_]])
            b.cp('dve', self.VwA[:, tt_, :, 0:64], ps[PB][:, 384:512].rearrange('p (h d) -> p h d', d=64),
                 [pr[PB]], [self.r_vw[tt_]])
            b.cp('act', self.gates[:, tt_, :], ps[PC][:, 0:24], [pr[PC]], [self.r_g[tt_]])

        def stageYpostB(st, j):
            tt_ = st * 4 + j
            QB, rQB = QBs[tt_ % 2], rQBs[tt_ % 2]
            T1, rT1 = T1s[tt_ % 2], rT1s[tt_ % 2]
            T2, rT2 = T1, rT1
            b.tt('pool', T2, T1, gqk, ALU.mult, [rT1, P['r']], [rT2])
            cosb = self.cosT[:, tt_:tt_ + 1, :].to_broadcast([128, 12, 8])
            sinb = self.sinT[:, tt_:tt_ + 1, :].to_broadcast([128, 12, 8])
            x1 = T2[:, :, 0:8]
            x2 = T2[:, :, 8:16]
            b.tt('dve', RA[:, 0], x1, cosb, ALU.mult, [rT2, self.r_rope], [rRA[0]])
            b.tt('pool', RA[:, 1], x2, sinb, ALU.mult, [rT2, self.r_rope], [rRA[1]])
            b.tt('dve', RA[:, 2], x2, cosb, ALU.mult, [rT2, self.r_rope], [rRA[2]])
            b.tt('pool', RA[:, 3], x1, sinb, ALU.mult, [rT2, self.r_rope], [rRA[3]])
            qdst = QB[:, 0:8, :].rearrange('p (pr hf) d -> p hf pr d', hf=2)

            def split(apx):
                return apx[:, 0:8].rearrange('p (hf pr) d -> p hf pr d', hf=2), apx[:, 8:12]
            r1q, r1k = split(RA[:, 0])
            s1q, s1k = split(RA[:, 1])
            r2q, r2k = split(RA[:, 2])
            s2q, s2k = split(RA[:, 3])
            b.tt('dve', qdst[:, :, :, 0:8], r1q, s1q, ALU.subtract, [rRA[0], rRA[1]], [rQB])
            b.tt('dve', QB[:, 8:12, 0:8], r1k, s1k, ALU.subtract, [rRA[0], rRA[1]], [rQB])
            b.tt('dve', qdst[:, :, :, 8:16], r2q, s2q, ALU.add, [rRA[2], rRA[3]], [rQB])
            b.tt('dve', QB[:, 8:12, 8:16], r2k, s2k, ALU.add, [rRA[2], rRA[3]], [rQB])
            t2q, t2k = split(T2)
            b.cp('pool', qdst[:, :, :, 16:64], t2q[:, :, :, 16:64], [rT2], [rQB])
            b.cp('pool', QB[:, 8:12, 16:64], t2k[:, :, 16:64], [rT2], [rQB])

        def stageYpost2(st, j):
            tt_ = st * 4 + j
            QB, rQB = QBs[tt_ % 2], rQBs[tt_ % 2]
            QBf = QB.rearrange('p h d -> p (h d)')
            for c in range(6):
                b.tr(psQ[:, c, :], QBf[:, c * 128:(c + 1) * 128], ident_b, [rQB, self.r_ident], [pr[PQ]])
            b.cp('act', self.QT[:, :, tt_ * 128:(tt_ + 1) * 128], psQ[:, 0:4, :], [pr[PQ]], [self.r_q[tt_]])
            b.cp('act', self.KE[0][0:64, tt_ * 128:(tt_ + 1) * 128], psQ[0:64, 4, :], [pr[PQ]], [self.r_ks[tt_]])
            b.cp('act', self.KE[1][64:128, tt_ * 128:(tt_ + 1) * 128], psQ[64:128, 4, :], [pr[PQ]],
                 [self.r_ks[tt_]])
            b.cp('act', self.KwT[:, tt_ * 128:(tt_ + 1) * 128], psQ[:, 5, :], [pr[PQ]], [self.r_kw[tt_]])

        def f_order():
            order = [('kc', 1048, rW[6]), ('vc', 1176, rW[7])]
            for ct in range(4):
                order.append(('g%d' % ct, 1304 + 512 + ct * 128, rW[8]))
            for ct in range(4):
                order.append(('a%d' % ct, 1304 + ct * 128, rW[8]))
            return order
        FGROUPS = (f_order()[0:4], f_order()[4:7], f_order()[7:10], [])

        def stageF(st, grp):
            hb = st % 2
            for name, c0, rw in FGROUPS[grp]:
                bank = PF0 if fstate['fidx'] % 2 == 0 else PF1
                fstate['fidx'] += 1
                for k in range(8):
                    b.mm(ps[bank][:, :], W[:, k, c0:c0 + 128], HT[hb][:, k, :], k == 0, k == 7,
                         rHT[hb] + [rw], [pr[bank]])
                if name in ('kc', 'vc'):
                    kb = fstate['kci'] % 2
                    fstate['kci'] += 1
                    b.cp('act', KCt[kb], ps[bank][:, :], [pr[bank]], [rKCt[kb]])
                    dstd = self.kc_scr if name == 'kc' else self.vc_scr
                    b.dma('sp', dstd[:, st * 512:(st + 1) * 512], KCt[kb], [rKCt[kb]], [Res()])
                elif name[0] == 'g':
                    ct = int(name[1])
                    b.act(SIG[ct], ps[bank][:, :], AF.Sigmoid, [pr[bank]], [rSIG[ct]])
                else:
                    ct = int(name[1])
                    sb = ct % 2
                    b.tt('dve', HGt[sb], ps[bank][:, :], SIG[ct], ALU.mult, [pr[bank], rSIG[ct]], [rHGt[sb]])
                    b.dma('sp', hg[ct][:, 30 + st * 512:30 + (st + 1) * 512], HGt[sb], [rHGt[sb]], [Res()])

        stageX(*tiles[0])
        for i, (st, j) in enumerate(tiles):
            if i + 1 < len(tiles):
                stageX(*tiles[i + 1])
            stageYmm(st, j)
            si = sts.index(st)
            if si > 0:
                stageF(sts[si - 1], j)
            if i > 1:
                stageYpost2(*tiles[i - 2])
            stageYpost(st, j)
            if i > 0:
                stageYpostB(*tiles[i - 1])
        stageYpostB(*tiles[-1])
        if len(tiles) > 1:
            stageYpost2(*tiles[-2])
        stageYpost2(*tiles[-1])
        for g in range(4):
            stageF(sts[-1], g)
        rg_all = Res()
        b.act(self.gates, self.gates, AF.Sigmoid, self.r_g, [rg_all])
        for t in range(NT):
            self.r_g[t] = rg_all
        self.markA = mark


INPUT_SPECS = [
    ('x', [S, D], F32), ('pos_t', [128, 32], I32), ('posc_t', [128, 2], I32),
    ('gin_t', [2, 128, 8], F32), ('gffn_t', [2, 128, 8], F32),
    ('w_in', [2, D, WIN], F32), ('w_out', [2, D, D], F32),
    ('q_norm_g', [2, 64], F32), ('k_norm_g', [2, 3, 64], F32),
    ('cmp_pos_k_t', [2, 128, 32], F32), ('cmp_w1_k', [2, 2048, 128], F32), ('cmp_w2_k', [2, 128, 64], F32),
    ('cmp_pos_v_t', [2, 128, 32], F32), ('cmp_w1_v', [2, 2048, 128], F32), ('cmp_w2_v', [2, 128, 64], F32),
    ('conv_w_t', [2, 128, 4, 31], F32), ('conv_b_t', [2, 128, 4], F32),
    ('conv_lng_t', [2, 128, 4], F32), ('conv_lnb_t', [2, 128, 4], F32),
    ('ffn_w_gate', [D, DFF], F32), ('ffn_w_up', [D, DFF], F32), ('ffn_w_down', [DFF, D], F32),
    ('moe_router', [D, NE], F32), ('moe_w_gate', [NE, D, DFF], F32), ('moe_w_up', [NE, D, DFF], F32),
    ('moe_w_down', [NE, DFF, D], F32),
]


def build_program(dbg=None):
    from contextlib import ExitStack
    nc = bass.Bass('TRN2', target_bir_lowering=False)
    dbg = dbg or {}
    with ExitStack() as stack:
        stack.enter_context(nc.allow_low_precision('bf16 matmul operands, fp32 accumulation'))
        stack.enter_context(nc.allow_non_contiguous_dma('small parameter loads'))
        B = Builder(nc, stack, dbg)
        use = dbg.get('inputs')
        inp = {}
        for name, shape, dt in INPUT_SPECS:
            if use is None or name in use:
                inp[name] = B.din(name, shape, dt)
        out = B.dout('out', [S, D], F32)
        B.hg_scr = [B.dscr('hg%d' % c, [128, S + 30], BF16) for c in range(4)]
        B.conv_scr = [B.dscr('cv%d' % c, [128, S], BF16) for c in range(4)]
        B.kc_scr = B.dscr('kc_scr', [128, S], BF16)
        B.vc_scr = B.dscr('vc_scr', [128, S], BF16)
        B.phase0(inp)
        B.build_selmap()
        if dbg.get('stage', 'full') == 'full':
            nl = dbg.get('layers', 2)
            B.final_out = out
            sparse = dbg.get('sparse', True)
            if sparse:
                B.setup_weight_conversion_sparse(inp)
            else:
                B.setup_weight_conversion(inp)
            xmid = [B.dscr('xmid%d' % i, [S, D], F32) for i in range(2)]
            xl1 = B.dscr('xl1', [S, D], F32)
            for l in range(nl):
                A = B.arena
                base = A.off
                xin = inp['x'] if l == 0 else xl1
                xout = out if l == nl - 1 else xl1
                P = B.load_layer_small(inp, l)
                markP = A.off
                B.phaseA(inp, l, xin, P)
                B.sc.barrier()
                A.off = B.markA
                B.phase_cmp(inp, l, P)
                A.off = B.markKc
                B.phase_conv(inp, l)
                B.phaseB(inp, l, xin, xmid[l])
                A.off = markP
                if l == 1:
                    B.bg(10000)
                    B.sc.barrier()
                if l == 1 and sparse:
                    B.phaseC_sparse(inp, l, xmid[l], xout, P)
                else:
                    B.phaseC(inp, l, xmid[l], xout, l == 1, P)
                A.off = base
        if dbg.get('stage') == 'B':
            P = B.load_layer_small(inp, 0)
            B.phaseA(inp, 0, inp['x'], P)
            B.sc.barrier()
            B.arena.off = B.markA
            B.phase_cmp(inp, 0, P)
            B.arena.off = B.markKc
            B.phase_conv(inp, 0)
            B.final_out = out
            dbg_attn = B.dout('dbg_attn', [S, 512], BF16)
            B.phaseB(inp, 0, inp['x'], out, qt_list=dbg.get('qt_list'), dbg_attn=dbg_attn)
        if dbg.get('stage') == 'conv':
            P = B.load_layer_small(inp, 0)
            B.phaseA(inp, 0, inp['x'], P)
            B.sc.barrier()
            B.arena.off = B.markKc
            B.phase_conv(inp, 0)
            cvo = B.dout('dbg_cv', [4, 128, S], BF16)
            for c in range(4):
                B.sc.dma('sp', lambda e, c=c: e.dma_start(out=cvo[c], in_=B.conv_scr[c]), [], [Res()], final=True)
        if dbg.get('stage') == 'cmp':
            P = B.load_layer_small(inp, 0)
            B.phaseA(inp, 0, inp['x'], P)
            B.sc.barrier()
            B.arena.off = B.markA
            B.phase_cmp(inp, 0, P)
            B.dump('KcmpT', B.KcmpT, [], [128, 256], BF16)
            B.dump('VcA', B.VcA, [], [128, 2, 2, 128], BF16)
        if dbg.get('stage') == 'A':
            P = B.load_layer_small(inp, 0)
            B.phaseA(inp, 0, inp['x'], P, st_list=dbg.get('st_list'))
            B.sc.barrier()
            B.dump('QT', B.QT, [], [128, 4, S], BF16)
            B.dump('KE0', B.KE[0], [], [128, S], BF16)
            B.dump('KE1', B.KE[1], [], [128, S], BF16)
            B.dump('KwT', B.KwT, [], [128, S], BF16)
            B.dump('VsA', B.VsA, [], [128, NT, 2, 65], BF16)
            B.dump('gates', B.gates, [], [128, NT, 24], F32)
            B.dump('cosT', B.cosT, [], [128, 34, 8], F32)
            B.dump('sinT', B.sinT, [], [128, 34, 8], F32)
            hgo = B.dout('dbg_hg', [4, 128, S + 30], BF16)
            B.dbg_out['dbg_hg'] = None
            for c in range(4):
                B.sc.dma('sp', lambda e, c=c: e.dma_start(out=hgo[c], in_=B.hg_scr[c]), [], [Res()], final=True)
        B.sc.emit(nc, stack)
    return nc, B


def host_inputs(inputs, bidx):
    pos = np.asarray(inputs['positions'][bidx])
    cmp_end = np.arange(255) * 16 + 31
    posc = np.zeros(256, np.int32)
    posc[:255] = pos[cmp_end]
    d = {
        'x': np.ascontiguousarray(inputs['x'][bidx]),
        'pos_t': np.ascontiguousarray(pos.reshape(32, 128).T),
        'posc_t': np.ascontiguousarray(posc.reshape(2, 128).T),
        'gin_t': np.ascontiguousarray(np.asarray(inputs['attn_norm_g']).reshape(2, 8, 128).transpose(0, 2, 1)),
        'gffn_t': np.ascontiguousarray(np.asarray(inputs['ffn_norm_g']).reshape(2, 8, 128).transpose(0, 2, 1)),
        'conv_w_t': np.ascontiguousarray(np.asarray(inputs['conv_w']).reshape(2, 31, 4, 128).transpose(0, 3, 2, 1)),
        'conv_b_t': np.ascontiguousarray(np.asarray(inputs['conv_b']).reshape(2, 4, 128).transpose(0, 2, 1)),
        'conv_lng_t': np.ascontiguousarray(np.asarray(inputs['conv_ln_g']).reshape(2, 4, 128).transpose(0, 2, 1)),
        'conv_lnb_t': np.ascontiguousarray(np.asarray(inputs['conv_ln_b']).reshape(2, 4, 128).transpose(0, 2, 1)),
        'ffn_w_gate': np.asarray(inputs['ffn_w_gate'])[0], 'ffn_w_up': np.asarray(inputs['ffn_w_up'])[0],
        'ffn_w_down': np.asarray(inputs['ffn_w_down'])[0],
        'moe_router': np.asarray(inputs['moe_router'])[0], 'moe_w_gate': np.asarray(inputs['moe_w_gate'])[0],
        'moe_w_up': np.asarray(inputs['moe_w_up'])[0], 'moe_w_down': np.asarray(inputs['moe_w_down'])[0],
    }
    for nm in ('cmp_pos_k', 'cmp_pos_v'):
        t = np.asarray(inputs[nm]).transpose(0, 2, 1)
        d[nm + '_t'] = np.ascontiguousarray(np.concatenate([t, t], axis=1))
    for k in ('w_in', 'w_out', 'q_norm_g', 'k_norm_g', 'cmp_w1_k', 'cmp_w2_k', 'cmp_w1_v', 'cmp_w2_v'):
        d[k] = np.asarray(inputs[k])
    return d


def _phase_cmp(self, inp, l, P):
    b, A, sc = self, self.arena, self.sc
    ps, pr = self.psb, self.psr
    self.r_kcmp = Res()
    self.r_vca = Res()
    mark = A.off
    for nt in range(2):
        for kh in range(2):
            b.cp('pool', self.VcA[:, nt, kh, 64:128], self.selmap[:, nt, :], [self.r_selmap], [self.r_vca])
    ident_b = self.ident_b
    sc.serialize_dma = bool(self.dbg.get('ser'))
    KVT = {}
    for which in ('k', 'v'):
        KVT[which] = A.alloc([S], BF16)
        rk = Res()
        b.dma('sp', KVT[which], self.kc_scr if which == 'k' else self.vc_scr, [], [rk])
        KVT[which + 'r'] = [rk]
    for which in ('k', 'v'):
        if self.dbg.get('cmp_stop', 99) <= 0:
            continue
        src = KVT[which]
        rsrc = KVT[which + 'r']
        W1 = A.alloc([32, 128], BF16)
        rW1 = Res()
        w1src = inp['cmp_w1_' + which][l].rearrange('(l d) h -> d l h', d=64)
        skip = self.dbg.get('cmp_skip', ())
        if 'w1' not in skip:
            b.dma('pool', W1[0:64], w1src, [], [rW1], nd=2048)
            b.dma('pool', W1[64:128], w1src, [], [rW1], nd=2048)
        posT = A.alloc([32], BF16)
        if 'pos' not in skip:
            b.dma('pool', posT, inp['cmp_pos_%s_t' % which][l], [], [rW1])
        W2 = A.alloc([64], BF16)
        if 'w2' not in skip:
            b.dma('pool', W2, inp['cmp_w2_' + which][l], [], [rW1])
        PH, PBI, PO, PTR = 0, 1, 2, 3
        PHS = (0, 4)
        stop = self.dbg.get('cmp_stop', 99)
        if stop <= 1:
            continue
        for kh in range(2):
            p0, p1 = kh * 64, (kh + 1) * 64
            for li in range(32):
                b.mm(ps[PHS[kh]][:, 0:255], W1[p0:p1, li, :], src[p0:p1, li:li + 4065:16],
                     li == 0, li == 31, [rW1] + rsrc, [pr[PHS[kh]]])
        for li in range(32):
            b.mm(ps[PBI][:, 0:1], W1[0:64, li, :], posT[0:64, li:li + 1], li == 0, li == 31, [rW1], [pr[PBI]])
        if stop <= 2:
            continue
        bias = A.alloc([1], F32)
        rb = Res()
        b.cp('dve', bias, ps[PBI][:, 0:1], [pr[PBI]], [rb])
        X = A.alloc([512], F32)
        X2 = A.alloc([512], F32)
        X3 = A.alloc([512], F32)
        HID = A.alloc([512], BF16)
        rX, rX2, rX3, rH = Res(), Res(), Res(), Res()
        for kh in range(2):
            b.ts('dve', X[:, kh * 256:(kh + 1) * 256], ps[PHS[kh]][:, 0:256], bias, None, ALU.add, None,
                 [pr[PHS[kh]], rb], [rX])
        b.tt('dve', X2, X, X, ALU.mult, [rX], [rX2])
        b.ts('dve', X3, X2, 0.044715, 1.0, ALU.mult, ALU.add, [rX2], [rX3])
        b.tt('dve', X2, X3, X, ALU.mult, [rX3, rX], [rX2])
        b.act(X3, X2, AF.Tanh, [rX2], [rX3], scale=math.sqrt(2.0 / math.pi))
        b.ts('dve', X2, X3, 1.0, 0.5, ALU.add, ALU.mult, [rX3], [rX2])
        b.tt('dve', HID, X2, X, ALU.mult, [rX2, rX], [rH])
        if stop <= 3:
            continue
        pso = ps[PO][:, 0:256].rearrange('p (a k d) -> p a k d', a=2, k=2)
        for nt in range(2):
            nn = 128 if nt == 0 else 127
            for kh in range(2):
                b.mm(pso[0:nn, nt, kh, :], HID[:, kh * 256 + nt * 128:kh * 256 + nt * 128 + nn], W2, True, True,
                     [rH, rW1], [pr[PO]])
        if stop <= 4:
            continue
        if which == 'v':
            for nt in range(2):
                nn = 128 if nt == 0 else 127
                b.cp('dve', self.VcA[0:nn, nt, :, 0:64], pso[0:nn, nt, :, :], [pr[PO]], [self.r_vca])
        else:
            SQ = A.alloc([4, 64], F32)
            s4 = A.alloc([4], F32)
            d4 = A.alloc([4], F32)
            r4 = A.alloc([4], F32)
            T1 = A.alloc([4, 64], F32)
            T2 = A.alloc([4, 64], F32)
            RA = A.alloc([4, 4, 8], F32)
            KB = A.alloc([2, 2, 64], BF16)
            rq, rs4, rd4, rr4, rT1, rT2, rRA, rKB = (Res() for _ in range(8))
            psf = ps[PO][:, 0:256].rearrange('p (h d) -> p h d', d=64)
            b.memset('pool', SQ, 1.0, [rq])
            b.memset('pool', T1, 0.0, [rT1])
            for nt in range(2):
                nn = 128 if nt == 0 else 127
                h0 = nt * 2
                b.act(SQ[0:nn, h0:h0 + 2, :], psf[0:nn, h0:h0 + 2, :], AF.Square, [pr[PO]], [rq])
            sc.op('dve', lambda e: e.tensor_reduce(s4, SQ, AX.X, ALU.add), [rq], [rs4])
            b.act(d4, s4, AF.Sqrt, [rs4], [rd4], scale=1.0 / 64, bias=EPS)
            sc.op('dve', lambda e: e.reciprocal(r4, d4), [rd4], [rr4])
            for nt in range(2):
                nn = 128 if nt == 0 else 127
                h0 = nt * 2
                b.tt('dve', T1[0:nn, h0:h0 + 2, :], psf[0:nn, h0:h0 + 2, :],
                     r4[0:nn, h0:h0 + 2].unsqueeze(2).to_broadcast([nn, 2, 64]), ALU.mult, [pr[PO], rr4], [rT1])
            b.tt('dve', T2, T1, P['gk'][:, 0:1, :].to_broadcast([128, 4, 64]), ALU.mult, [rT1, P['r']], [rT2])
            T2v = T2.rearrange('p (a k) d -> p a k d', a=2)
            RAv = RA.rearrange('p r (a k) d -> p r a k d', a=2)
            KBv = KB
            cosb = self.cosT[:, 32:34, :].unsqueeze(2).to_broadcast([128, 2, 2, 8])
            sinb = self.sinT[:, 32:34, :].unsqueeze(2).to_broadcast([128, 2, 2, 8])
            x1 = T2v[:, :, :, 0:8]
            x2 = T2v[:, :, :, 8:16]
            b.tt('dve', RAv[:, 0], x1, cosb, ALU.mult, [rT2, self.r_rope], [rRA])
            b.tt('dve', RAv[:, 1], x2, sinb, ALU.mult, [rT2, self.r_rope], [rRA])
            b.tt('dve', RAv[:, 2], x2, cosb, ALU.mult, [rT2, self.r_rope], [rRA])
            b.tt('dve', RAv[:, 3], x1, sinb, ALU.mult, [rT2, self.r_rope], [rRA])
            b.tt('dve', KBv[:, :, :, 0:8], RAv[:, 0], RAv[:, 1], ALU.subtract, [rRA], [rKB])
            b.tt('dve', KBv[:, :, :, 8:16], RAv[:, 2], RAv[:, 3], ALU.add, [rRA], [rKB])
            b.cp('dve', KBv[:, :, :, 16:64], T2v[:, :, :, 16:64], [rT2], [rKB])
            psq = self.psum_bf(PTR).rearrange('p (k t) -> p k t', t=128)
            for nt in range(2):
                b.tr(psq[:, nt, :], KB[:, nt].rearrange('p k d -> p (k d)'), ident_b, [rKB, self.r_ident], [pr[PTR]])
            b.cp('dve', self.KcmpT.rearrange('p (a n) -> p a n', a=2), psq[:, 0:2, :], [pr[PTR]], [self.r_kcmp])
    sc.barrier()
    A.off = mark


Builder.phase_cmp = _phase_cmp


def _build_selmap(self):
    b, A, sc = self, self.arena, self.sc
    self.selmap = A.alloc([2, 64], F32)
    self.r_selmap = Res()
    mark = A.off
    ones = A.alloc([64], F32)
    half = A.alloc([64], F32)
    t1 = A.alloc([2, 64], F32)
    t2 = A.alloc([2, 64], F32)
    t3 = A.alloc([2, 64], F32)
    r0, r1, r2, r3 = Res(), Res(), Res(), Res()
    b.memset('pool', ones, 1.0, [r0])
    b.memset('pool', half, 0.5, [r0])
    for nt in range(2):
        base = 128 * nt
        o1, o2, o3 = t1[:, nt, :], t2[:, nt, :], t3[:, nt, :]
        sc.op('pool', lambda e, o=o1, base=base: e.affine_select(out=o, in_=ones, pattern=[[-4, 64]],
              compare_op=ALU.is_ge, fill=0.0 if REGS is None else REGS['zero'], base=base, channel_multiplier=1), [r0], [r1])
        sc.op('pool', lambda e, o=o1, base=base: e.affine_select(out=o, in_=o, pattern=[[4, 64]],
              compare_op=ALU.is_ge, fill=0.0 if REGS is None else REGS['zero'], base=3 - base, channel_multiplier=-1), [r1], [r1])
        sc.op('pool', lambda e, o=o2, base=base: e.affine_select(out=o, in_=half, pattern=[[-4, 64]],
              compare_op=ALU.is_equal, fill=0.0 if REGS is None else REGS['zero'], base=base - 3, channel_multiplier=1), [r0], [r2])
        sc.op('pool', lambda e, o=o3, base=base: e.affine_select(out=o, in_=half, pattern=[[-4, 64]],
              compare_op=ALU.is_equal, fill=0.0 if REGS is None else REGS['zero'], base=base + 1, channel_multiplier=1), [r0], [r3])
    b.tt('pool', t1, t1, t2, ALU.subtract, [r1, r2], [r1])
    b.tt('pool', self.selmap, t1, t3, ALU.add, [r1, r3], [self.r_selmap])
    sc.barrier()
    A.off = mark


Builder.build_selmap = _build_selmap


def _phase_conv(self, inp, l):
    b, A, sc = self, self.arena, self.sc
    ps, pr = self.psb, self.psr
    mark = A.off
    cw = A.alloc([4, 31], F32)
    cb = A.alloc([4], F32)
    lg = A.alloc([4], F32)
    lb = A.alloc([4], F32)
    rp = Res()
    b.dma('sp', cw, inp['conv_w_t'][l], [], [rp])
    b.dma('sp', cb, inp['conv_b_t'][l], [], [rp])
    b.dma('sp', lg, inp['conv_lng_t'][l], [], [rp])
    b.dma('sp', lb, inp['conv_lnb_t'][l], [], [rp])
    DG = A.alloc([4, 31, 128], BF16)
    rDG = [Res() for _ in range(4)]
    i = 0
    for ct in range(4):
        for j in range(31):
            eng = 'dve'
            i += 1
            b.ts(eng, DG[:, ct, j, :], self.ident_b, cw[:, ct, j:j + 1], None, ALU.mult, None,
                 [self.r_ident, rp], [rDG[ct]])
    HG = A.alloc([4, S + 30], BF16)
    rHG = [Res() for _ in range(4)]
    for ct in range(4):
        b.dma('sp', HG[:, ct, :], self.hg_scr[ct], [], [rHG[ct]])
    onesm = A.alloc([128], F32)
    rones = Res()
    b.memset('pool', onesm, 1.0 / 512.0, [rones])
    YS = [A.alloc([512], F32) for _ in range(4)]
    YQ = [A.alloc([512], F32) for _ in range(2)] * 2
    rYS = [Res() for _ in range(4)]
    rYQ = [Res() for _ in range(2)] * 2
    MEAN = A.alloc([512], F32)
    MSQ = A.alloc([512], F32)
    VAR = A.alloc([512], F32)
    RSTD = A.alloc([512], F32)
    rM, rMS, rV, rR = Res(), Res(), Res(), Res()
    Z = [VAR] * 2
    rZ = [rV] * 2
    CT = [A.alloc([512], BF16)] * 2
    rCT = [Res()] * 2
    zi = 0
    for tc in range(NST):
        for ct in range(4):
            for j in range(31):
                b.mm(ps[ct][:, :], DG[:, ct, j, :], HG[:, ct, tc * 512 + j:tc * 512 + j + 512], j == 0, j == 30,
                     [rDG[ct], rHG[ct]], [pr[ct]])
        for ct in range(4):
            b.act(YS[ct], ps[ct][:, :], AF.Identity, [pr[ct], rp], [rYS[ct]], bias=cb[:, ct:ct + 1])
            b.act(YQ[ct], ps[ct][:, :], AF.Square, [pr[ct], rp], [rYQ[ct]], bias=cb[:, ct:ct + 1])
            b.mm(ps[5][:, :], onesm, YQ[ct], ct == 0, ct == 3, [rones, rYQ[ct]], [pr[5]])
        for ct in range(4):
            b.mm(ps[4][:, :], onesm, YS[ct], ct == 0, ct == 3, [rones, rYS[ct]], [pr[4]])
        b.cp('act', MEAN, ps[4][:, :], [pr[4]], [rM])
        b.tt('dve', MSQ, ps[4][:, :], MEAN, ALU.mult, [pr[4], rM], [rMS])
        b.tt('dve', VAR, ps[5][:, :], MSQ, ALU.subtract, [pr[5], rMS], [rV])
        b.act(MSQ, VAR, AF.Sqrt, [rV], [rMS], bias=EPS)
        sc.op('dve', lambda e: e.reciprocal(RSTD, MSQ), [rMS], [rR])
        for ct in range(4):
            zb = zi % 2
            zi += 1
            b.tt('dve', Z[zb], YS[ct], MEAN, ALU.subtract, [rYS[ct], rM], [rZ[zb]])
            b.tt('dve', Z[zb], Z[zb], RSTD, ALU.mult, [rZ[zb], rR], [rZ[zb]])
            b.act(CT[zb], Z[zb], AF.Silu, [rZ[zb], rp], [rCT[zb]], scale=lg[:, ct:ct + 1], bias=lb[:, ct:ct + 1])
            b.dma('sp', self.conv_scr[ct][:, tc * 512:(tc + 1) * 512], CT[zb], [rCT[zb]], [Res()])
    sc.barrier()
    A.off = mark


Builder.phase_conv = _phase_conv


def _phaseB(self, inp, l, xsrc, xdst, qt_list=None, dbg_attn=None):
    b, A, sc = self, self.arena, self.sc
    ps, pr = self.psb, self.psr
    mark = A.off
    ident_b = self.ident_b
    Wout = A.alloc([8, D], BF16)
    rWo = Res()
    wsrc = inp['w_out'][l].rearrange('(m p) d -> p m d', p=128)
    for h in range(2):
        b.dma('pool', Wout[:, :, h * 512:(h + 1) * 512], wsrc[:, :, h * 512:(h + 1) * 512], [], [rWo], nd=1024)
    NB = 2
    R = [[A.alloc([4, 128], BF16) for _ in range(2)] for _ in range(NB)]
    RZ = [[A.alloc([4, 128], BF16) for _ in range(2)] for _ in range(NB)]
    rRq = [[Res() for _ in range(2)] for _ in range(NB)]
    rRm = [[Res() for _ in range(2)] for _ in range(NB)]
    rRZ = [[Res() for _ in range(2)] for _ in range(NB)]
    for i in range(NB):
        b.memset('pool', RZ[i][0][64:128], 0.0, [rRZ[i][0]])
        b.memset('pool', RZ[i][1][0:64], 0.0, [rRZ[i][1]])
    NP = 6
    PT_ = [A.alloc([512], BF16) for _ in range(NP)]
    rPT = [Res() for _ in range(NP)]
    cmask = [[A.alloc([4, 128], BF16) for _ in range(2)] for _ in range(NB)]
    rcm = [[Res() for _ in range(2)] for _ in range(NB)]
    XT = [A.alloc([D], F32) for _ in range(3)]
    rXT = [Res() for _ in range(3)]
    CV = [A.alloc([4, 128], BF16) for _ in range(3)]
    rCV = [Res() for _ in range(3)]
    ATT = A.alloc([512], BF16)
    rATT = Res()
    ATT_T = A.alloc([4, 128], BF16)
    rATT_T = Res()
    MM_ = A.alloc([128], BF16)
    rMM = Res()
    ACC = [[A.alloc([4, 64], F32) for _ in range(2)] for _ in range(NB)]
    r_acc = [[Res() for _ in range(2)] for _ in range(NB)]
    rsumC = [[A.alloc([4], F32) for _ in range(2)] for _ in range(NB)]
    rinvC = [[A.alloc([4], F32) for _ in range(2)] for _ in range(NB)]
    r_rsumC = [[Res() for _ in range(2)] for _ in range(NB)]
    r_rinvC = [[Res() for _ in range(2)] for _ in range(NB)]
    rsum2 = [A.alloc([2, 4], F32) for _ in range(2)]
    rinv = [A.alloc([3, 4], F32) for _ in range(2)]
    coef = [A.alloc([3, 4], F32) for _ in range(2)]
    r_rsum2 = [Res() for _ in range(2)]
    r_rinv = [Res() for _ in range(2)]
    r_coef = [Res() for _ in range(2)]
    IMP = [A.alloc([64], F32) for _ in range(2)]
    IMPM = [A.alloc([64], F32) for _ in range(2)]
    IMP2 = [A.alloc([64], F32) for _ in range(2)]
    M8 = [A.alloc([16], F32) for _ in range(2)]
    SELC = [A.alloc([64], F32) for _ in range(2)]
    FRC = [A.alloc([64], F32) for _ in range(2)]
    r_imp = [Res() for _ in range(2)]
    r_impm = [Res() for _ in range(2)]
    r_imp2 = [Res() for _ in range(2)]
    r_m8 = [Res() for _ in range(2)]
    r_selc = [Res() for _ in range(2)]
    r_frc = [Res() for _ in range(2)]
    TMP = [A.alloc([4, 64], F32) for _ in range(2)]
    r_tmp = [Res() for _ in range(2)]
    ST = (0, 1)
    OC, OS, OW, TRP, OUT0, OUT1 = 2, 3, 4, 5, 6, 7
    psOC = ps[OC][:, 0:512].rearrange('p (g c) -> p g c', g=4)
    psOST = ps[OS][0:65, :]
    psOWT = ps[OW][0:65, :]
    psOS = ps[OUT0][:, 0:260].rearrange('p (g c) -> p g c', g=4)
    psOW = ps[OUT1][:, 0:260].rearrange('p (g c) -> p g c', g=4)
    OTs = [A.alloc([512], F32) for _ in range(2)]
    rOTs = [Res() for _ in range(2)]
    psTR = self.psum_bf(TRP).rearrange('p (k t) -> p k t', t=128)
    gates_v = self.gates.rearrange('p t (h r) -> p t h r', r=3)
    state = {'sti': 0, 'pti': 0, 'pend': None}

    def flush():
        if state['pend'] is not None:
            state['pend']()
            state['pend'] = None

    def tile_step(lhsT, rhs, rl, rr, nn, post_mask, rmask, acc_view, vrhs, rv, first, width, accbank,
                  transposed=False):
        sb = ST[state['sti'] % 2]
        state['sti'] += 1
        pb = state['pti'] % NP
        state['pti'] += 1
        if post_mask is None:
            b.mm(ps[sb][0:nn, :], lhsT, rhs, True, True, rl + rr, [pr[sb]])
        else:
            b.mm(ps[sb][0:nn, :], lhsT, rhs, True, False, rl + rr, [pr[sb]])
            b.mm(ps[sb][0:nn, :], ident_b[0:nn, 0:nn], post_mask[0:nn].rearrange('p g q -> p (g q)'), False, True,
                 [self.r_ident] + rmask, [pr[sb]])
        b.act(PT_[pb][0:nn, :], ps[sb][0:nn, :], AF.Exp, [pr[sb]], [rPT[pb]])
        flush()

        def pv():
            if transposed:
                b.mm(acc_view, vrhs, PT_[pb][0:nn, :], first, False, [rPT[pb]] + rv, [pr[accbank]])
                return
            for g in range(4):
                b.mm(acc_view[:, g, 0:width], PT_[pb][0:nn, g * 128:(g + 1) * 128], vrhs, first and g == 0, False,
                     [rPT[pb]] + rv, [pr[accbank]])
        state['pend'] = pv

    def s1pre(qi, qt):
        t0 = qt * 128
        rb_ = qi % NB
        xb = qi % 3
        tsl = slice(t0, t0 + 128)
        b.bg(2)
        b.dma('sp', XT[xb], xsrc[t0:t0 + 128, :], [], [rXT[xb]])
        for ct in range(4):
            b.dma('sp', CV[xb][:, ct, :], self.conv_scr[ct][:, tsl], [], [rCV[xb]])
        b.cp('pool', R[rb_][0][0:64], self.QT[0:64, :, tsl], [self.r_q[qt]], [rRq[rb_][0]])
        b.cp('pool', R[rb_][1][64:128], self.QT[64:128, :, tsl], [self.r_q[qt]], [rRq[rb_][1]])
        b.cp('pool', RZ[rb_][0][0:64], self.QT[0:64, :, tsl], [self.r_q[qt]], [rRZ[rb_][0]])
        b.cp('pool', RZ[rb_][1][64:128], self.QT[64:128, :, tsl], [self.r_q[qt]], [rRZ[rb_][1]])

    def s1a(qi, qt, kh):
        t0 = qt * 128
        rb_ = qi % NB
        nmax = 8 * qt + 6
        ntiles = [0] if nmax < 128 else [0, 1]
        if True:
            rz = RZ[rb_][kh].rearrange('p g q -> p (g q)')
            for ni, nt_ in enumerate(ntiles):
                nn = 128 if nt_ == 0 else 127
                nbase = 128 * nt_
                full = (16 * (nbase + nn - 1) + 31) <= t0
                pm, rmk = None, []
                if not full:
                    cm_ = cmask[rb_][nt_]
                    if kh == 0:
                        zb4 = self.zeros_b4
                        sc.op('pool', lambda e, cm_=cm_, base=t0 - 16 * nbase - 31, zb4=zb4: e.affine_select(
                            out=cm_, in_=zb4, pattern=[[0, 4], [1, 128]], compare_op=ALU.is_ge,
                            fill=NEG if REGS is None else REGS['neg'], base=base,
                            channel_multiplier=-16), [self.r_masks], [rcm[rb_][nt_]])
                    pm, rmk = cm_, [rcm[rb_][nt_]]
                tile_step(self.KcmpT[:, nbase:nbase + nn], rz, [self.r_kcmp], [rRZ[rb_][kh]], nn, pm, rmk,
                          psOC, self.VcA[0:nn, nt_, kh, :], [self.r_vca], ni == 0, 128, OC)
            flush()
            sc.op('dve', lambda e, o=rsumC[rb_][kh], i=psOC[:, :, 64:128]: e.tensor_reduce(o, i, AX.X, ALU.add),
                  [pr[OC]], [r_rsumC[rb_][kh]])
            b.ts('dve', rinvC[rb_][kh], rsumC[rb_][kh], 1e-30, None, ALU.max, None, [r_rsumC[rb_][kh]],
                 [r_rinvC[rb_][kh]])
            sc.op('dve', lambda e, o=rinvC[rb_][kh]: e.reciprocal(o, o), [r_rinvC[rb_][kh]], [r_rinvC[rb_][kh]])
            b.cp('act', ACC[rb_][kh], psOC[:, :, 0:64], [pr[OC]], [r_acc[rb_][kh]])

    def s1topk(qi, qt, kh):
        rb_ = qi % NB
        if True:
            mdst = MM_[:, 64:128] if kh == 0 else MM_[:, 0:64]
            if qt >= 8:
                b.ts('dve', IMP[kh], psOC[:, 0, 64:128], rinvC[rb_][kh][:, 0:1], None, ALU.mult, None,
                     [pr[OC], r_rinvC[rb_][kh]], [r_imp[kh]])
                for g in range(1, 4):
                    b.stt(IMP[kh], psOC[:, g, 64:128], rinvC[rb_][kh][:, g:g + 1], IMP[kh], ALU.mult, ALU.add,
                          [pr[OC], r_rinvC[rb_][kh], r_imp[kh]], [r_imp[kh]])
                for half in range(2):
                    cur = 2 * qt + half
                    hs = slice(half * 64, half * 64 + 64)
                    sc.op('pool', lambda e, o=IMPM[kh][hs, :], i=IMP[kh][hs, :], base=cur - 2: e.affine_select(
                        out=o, in_=i, pattern=[[-1, 64]], compare_op=ALU.is_ge,
                        fill=NEG if REGS is None else REGS['neg'], base=base,
                        channel_multiplier=0), [r_imp[kh]], [r_impm[kh]])
                b.memset('pool', IMPM[kh][:, 0:1], NEG, [r_impm[kh]])
                sc.op('dve', lambda e, o=M8[kh][:, 0:8], i=IMPM[kh]: e.max(o, i), [r_impm[kh]], [r_m8[kh]])
                sc.op('dve', lambda e, o=IMP2[kh], a=M8[kh][:, 0:8], v=IMPM[kh]: e.match_replace(o, a, v, NEG),
                      [r_impm[kh], r_m8[kh]], [r_imp2[kh]])
                sc.op('dve', lambda e, o=M8[kh][:, 8:16], i=IMP2[kh]: e.max(o, i), [r_imp2[kh]], [r_m8[kh]])
                b.ts('dve', SELC[kh], IMPM[kh], M8[kh][:, 12:13], None, ALU.is_ge, None, [r_impm[kh], r_m8[kh]],
                     [r_selc[kh]])
                b.memset('pool', FRC[kh], 0.0, [r_frc[kh]])
                b.memset('pool', FRC[kh][:, 0:1], 1.0, [r_frc[kh]])
                b.memset('pool', FRC[kh][0:64, 2 * qt - 1:2 * qt + 1], 1.0, [r_frc[kh]])
                b.memset('pool', FRC[kh][64:128, 2 * qt:2 * qt + 2], 1.0, [r_frc[kh]])
                b.tt('dve', mdst, SELC[kh], FRC[kh], ALU.max, [r_selc[kh], r_frc[kh]], [rMM])
            else:
                b.memset('pool', mdst, 0.0, [rMM])
                b.memset('pool', mdst[0:64, 0:2 * qt + 1], 1.0, [rMM])
                b.memset('pool', mdst[64:128, 0:2 * qt + 2], 1.0, [rMM])
    def s1b(qi, qt):
        rb_ = qi % NB
        b.tr(psTR[:, 0, :], MM_, ident_b, [rMM, self.r_ident], [pr[TRP]])
        b.ts('dve', R[rb_][0][64:128], psTR[64:128, 0:1, :].to_broadcast([64, 4, 128]), -1.0, -NEG, ALU.add,
             ALU.mult, [pr[TRP]], [rRm[rb_][0]])
        b.ts('dve', R[rb_][1][0:64], psTR[0:64, 0:1, :].to_broadcast([64, 4, 128]), -1.0, -NEG, ALU.add,
             ALU.mult, [pr[TRP]], [rRm[rb_][1]])

    def s2win(qi, qt, kh):
        rb_ = qi % NB
        if True:
            rz = RZ[rb_][kh].rearrange('p g q -> p (g q)')
            kts = list(range(max(0, qt - 4), qt + 1))
            for i, kt in enumerate(kts):
                if kt == qt:
                    pm, rmk = self.causalN, [self.r_masks]
                elif kt == qt - 4:
                    pm, rmk = self.lowN, [self.r_masks]
                else:
                    pm, rmk = None, []
                tile_step(self.KwT[:, kt * 128:(kt + 1) * 128], rz, [self.r_kw[kt]], [rRZ[rb_][kh]], 128, pm,
                          rmk, psOWT, self.VwA[:, kt, kh, :], [self.r_vw[kt]], i == 0, 65, OW, transposed=True)

    def s2sel(qi, qt, kh):
        rb_ = qi % NB
        if True:
            rr = R[rb_][kh].rearrange('p g q -> p (g q)')
            for kt in range(qt + 1):
                pm, rmk = (self.causalN, [self.r_masks]) if kt == qt else (None, [])
                tile_step(self.KE[kh][:, kt * 128:(kt + 1) * 128], rr, [self.r_ks[kt], self.r_E],
                          [rRq[rb_][kh], rRm[rb_][kh]], 128, pm, rmk, psOST, self.VsA[:, kt, kh, :],
                          [self.r_vs[kt]], kt == 0, 65, OS, transposed=True)
            flush()
            b.cp('act', OTs[0][0:65, :], psOWT, [pr[OW]], [rOTs[0]])
            b.cp('dve', OTs[1][0:65, :], psOST, [pr[OS]], [rOTs[1]])
            for src_i, dstv, dbank in ((0, psOW, OUT1), (1, psOS, OUT0)):
                for g in range(4):
                    b.tr(dstv[:, g, :], OTs[src_i][0:65, g * 128:(g + 1) * 128], self.ident_f[0:65, 0:65],
                         [rOTs[src_i], self.r_identf], [pr[dbank]])
            b.cp('dve', rsum2[kh][:, 0, :], psOS[:, :, 64], [pr[OUT0]], [r_rsum2[kh]])
            b.cp('dve', rsum2[kh][:, 1, :], psOW[:, :, 64], [pr[OUT1]], [r_rsum2[kh]])
            sc.op('dve', lambda e, o=rinv[kh][:, 1:3, :], i=rsum2[kh]: e.reciprocal(o, i), [r_rsum2[kh]],
                  [r_rinv[kh]])
            b.cp('dve', rinv[kh][:, 0, :], rinvC[rb_][kh], [r_rinvC[rb_][kh]], [r_rinv[kh]])
            gv = gates_v[:, qt, kh * 4:(kh + 1) * 4, :].rearrange('p h r -> p r h')
            b.tt('dve', coef[kh], rinv[kh], gv, ALU.mult, [r_rinv[kh], self.r_g[qt]], [r_coef[kh]])
            acc = ACC[rb_][kh]
            racc = r_acc[rb_][kh]
            b.tt('dve', acc, acc, coef[kh][:, 0, :].unsqueeze(2).to_broadcast([128, 4, 64]), ALU.mult,
                 [racc, r_coef[kh]], [racc])
            b.tt('dve', TMP[kh], psOS[:, :, 0:64], coef[kh][:, 1, :].unsqueeze(2).to_broadcast([128, 4, 64]),
                 ALU.mult, [pr[OUT0], r_coef[kh]], [r_tmp[kh]])
            b.tt('dve', acc, acc, TMP[kh], ALU.add, [racc, r_tmp[kh]], [racc])
            b.tt('dve', TMP[kh], psOW[:, :, 0:64], coef[kh][:, 2, :].unsqueeze(2).to_broadcast([128, 4, 64]),
                 ALU.mult, [pr[OUT1], r_coef[kh]], [r_tmp[kh]])
            b.tt('dve', ATT[:, kh * 256:(kh + 1) * 256].rearrange('p (g d) -> p g d', g=4), acc, TMP[kh],
                 ALU.add, [racc, r_tmp[kh]], [rATT])
    def s2tail(qi, qt):
        t0 = qt * 128
        xb = qi % 3
        if dbg_attn is not None:
            b.dma('sp', dbg_attn[t0:t0 + 128, :], ATT, [rATT], [Res()], final=True)
        for c in range(4):
            b.tr(psTR[:, 1 + c, :], ATT[:, c * 128:(c + 1) * 128], ident_b, [rATT, self.r_ident], [pr[TRP]])
        b.cp('act', ATT_T, psTR[:, 1:5, :], [pr[TRP]], [rATT_T])
        for h, bank in enumerate((OUT0, OUT1)):
            for m in range(8):
                lhs = ATT_T[:, m, :] if m < 4 else CV[xb][:, m - 4, :]
                rl = [rATT_T] if m < 4 else [rCV[xb]]
                b.mm(ps[bank][:, :], lhs, Wout[:, m, h * 512:(h + 1) * 512], m == 0, m == 7, rl + [rWo], [pr[bank]])
            b.tt('dve', XT[xb][:, h * 512:(h + 1) * 512], XT[xb][:, h * 512:(h + 1) * 512], ps[bank][:, :], ALU.add,
                 [rXT[xb], pr[bank]], [rXT[xb]])
        b.dma('sp', xdst[t0:t0 + 128, :], XT[xb], [rXT[xb]], [Res()], final=(xdst is self.final_out))

    qts = list(qt_list if qt_list is not None else range(NT))
    s1pre(0, qts[0])
    for kh in range(2):
        s1a(0, qts[0], kh)
        s1topk(0, qts[0], kh)
    s1b(0, qts[0])
    for qi, qt in enumerate(qts):
        nxt = qi + 1 < len(qts)
        if nxt:
            s1pre(qi + 1, qts[qi + 1])
        for kh in range(2):
            if nxt:
                s1a(qi + 1, qts[qi + 1], kh)
            s2win(qi, qt, kh)
            if kh == 0 and qi > 0:
                s2tail(qi - 1, qts[qi - 1])
            if nxt:
                s1topk(qi + 1, qts[qi + 1], kh)
            s2sel(qi, qt, kh)
        if nxt:
            s1b(qi + 1, qts[qi + 1])
    s2tail(len(qts) - 1, qts[-1])
    sc.barrier()
    A.off = mark


Builder.phaseB = _phaseB


def _setup_weight_conversion(self, inp):
    self.wb = {}
    self.bg_tasks = []
    self.r_wb = {}
    specs = [('ffn', None)] + [('moe', e) for e in range(NE)]
    for kind, e in specs:
        key = 'd' if kind == 'ffn' else e
        for nm, shape in (('gate', [D, DFF]), ('up', [D, DFF]), ('down', [DFF, D])):
            src = inp['%s_w_%s' % (kind, nm)] if kind == 'ffn' else inp['moe_w_%s' % nm][e]
            dst = self.dscr('wb_%s_%s' % (key, nm), shape, BF16)
            self.wb[(key, nm)] = dst
            r = Res()
            self.r_wb[(key, nm)] = r
            rows = shape[0]
            step = 256 if shape[1] > 2048 else 1024
            for r0 in range(0, rows, step):
                r1 = min(rows, r0 + step)
                ndesc = (r1 - r0) * (2 if shape[1] > 2048 else 1)

                def task(dst=dst, src=src, r0=r0, r1=r1, r=r, ndesc=ndesc):
                    self.dma('pool', dst[r0:r1, :], src[r0:r1, :], [], [Res()], nd=ndesc, max_dma_last_dim=4096)
                self.bg_tasks.append(task)


def _bg(self, n=1):
    for _ in range(n):
        if self.bg_tasks:
            self.bg_tasks.pop(0)()


Builder.setup_weight_conversion = _setup_weight_conversion
Builder.bg = _bg


def _phaseC(self, inp, l, xsrc, xdst, moe, P, st_list=None):
    b, A, sc = self, self.arena, self.sc
    ps, pr = self.psb, self.psr
    mark = A.off
    ident_b, ident_f = self.ident_b, self.ident_f
    XF = [A.alloc([D], F32) for _ in range(4)]
    rXF = [Res() for _ in range(4)]
    XN = A.alloc([D], F32 if moe else BF16)
    rXN = Res()
    H2T = A.alloc([8, 512], BF16)
    rH2T = [Res() for _ in range(4)]
    ss = A.alloc([1], F32)
    sd = A.alloc([1], F32)
    rstd = A.alloc([1], F32)
    rss, rsd, rrs = Res(), Res(), Res()
    ACTT = A.alloc([NFT, 512], BF16)
    rACT = [Res() for _ in range(NFT)]
    WD = A.alloc([NFT, D], BF16)
    rWD = Res()
    NW = 2
    WG = [A.alloc([8, 512], BF16) for _ in range(NW)]
    WU = [A.alloc([8, 512], BF16) for _ in range(NW)]
    rWG = [Res() for _ in range(NW)]
    rWU = [Res() for _ in range(NW)]
    SL = [A.alloc([512], F32) for _ in range(2)]
    rSL = [Res() for _ in range(2)]
    if moe:
        H32 = A.alloc([8, 128], F32)
        rH32 = Res()
        RT = A.alloc([8, NE], F32)
        rRT = Res()
        b.dma('sp', RT, inp['moe_router'].rearrange('(k p) e -> p k e', p=128), [], [rRT])
        LG = A.alloc([NE], F32)
        M8 = A.alloc([8], F32)
        PP = A.alloc([2], F32)
        G0 = A.alloc([NE], F32)
        GATE = A.alloc([4, NE], F32)
        rLG, rM8, rPP, rG0 = Res(), Res(), Res(), Res()
        rGATE = [Res() for _ in range(4)]
    T0, T1 = 0, 1
    GU = (2, 3, 4, 5)
    DN = (6, 7)
    gui = 0
    dni = 0
    wi = 0
    gffn_b = P['gffn'].unsqueeze(2).to_broadcast([128, 8, 128])
    experts = list(range(NE)) if moe else ['d']
    if not moe:
        XP = [A.alloc([D], F32) for _ in range(2)]
        rXP = [Res() for _ in range(2)]
        XN2 = [XN, A.alloc([D], BF16)]
        rXN2 = [rXN, Res()]
        sts = list(st_list if st_list is not None else range(NST))
        psT = self.psum_bf(T0).rearrange('p (k t) -> p k t', t=128)[:, 0:8, :]

        def norm(st, j):
            tt_ = st * 4 + j
            xb = tt_ % 2
            b.dma('sp', XP[xb], xsrc[tt_ * 128:(tt_ + 1) * 128, :], [], [rXP[xb]])
            b.act(XN2[xb], XP[xb], AF.Square, [rXP[xb]], [rXN2[xb], rss], accum_out=ss)
            b.act(sd, ss, AF.Sqrt, [rss], [rsd], scale=1.0 / D, bias=EPS)
            sc.op('dve', lambda e: e.reciprocal(rstd, sd), [rsd], [rrs])
            b.act(XN2[xb], XP[xb], AF.Copy, [rXP[xb], rrs], [rXN2[xb]], scale=rstd)

        def trans(st, j):
            xb = (st * 4 + j) % 2
            for k in range(8):
                b.tr(psT[:, k, :], XN2[xb][:, k * 128:(k + 1) * 128], ident_b, [rXN2[xb], self.r_ident], [pr[T0]])
            b.tt('dve', H2T[:, :, j * 128:(j + 1) * 128], psT, gffn_b, ALU.mult, [pr[T0], P['r']], [rH2T[j]])

        for j in range(4):
            norm(sts[0], j)
            trans(sts[0], j)
        wg_d, wu_d, wd_d = self.wb[('d', 'gate')], self.wb[('d', 'up')], self.wb[('d', 'down')]
        wgv = wg_d.rearrange('(k p) f -> p k f', p=128)
        wuv = wu_d.rearrange('(k p) f -> p k f', p=128)
        for si, st in enumerate(sts):
            nxt = sts[si + 1] if si + 1 < len(sts) else None
            for j in range(4):
                tt_ = st * 4 + j
                b.dma('sp', XF[j], xsrc[tt_ * 128:(tt_ + 1) * 128, :], [], [rXF[j]])
            for c in range(7):
                wb_ = wi % NW
                wi += 1
                b.dma('sp', WG[wb_], wgv[:, :, c * 512:(c + 1) * 512], [], [rWG[wb_]], nd=1024)
                b.dma('sp', WU[wb_], wuv[:, :, c * 512:(c + 1) * 512], [], [rWU[wb_]], nd=1024)
                if c == 0:
                    b.dma('sp', WD, wd_d.rearrange('(ft p) d -> p ft d', p=128), [], [rWD], nd=3584)
                for fi in range(4):
                    ft = c * 4 + fi
                    gb, ub = GU[(gui * 2) % 4], GU[(gui * 2 + 1) % 4]
                    sl = gui % 2
                    gui += 1
                    for k in range(8):
                        b.mm(ps[gb][:, :], WG[wb_][:, k, fi * 128:(fi + 1) * 128], H2T[:, k, :], k == 0, k == 7,
                             [rWG[wb_]] + rH2T, [pr[gb]])
                    for k in range(8):
                        b.mm(ps[ub][:, :], WU[wb_][:, k, fi * 128:(fi + 1) * 128], H2T[:, k, :], k == 0, k == 7,
                             [rWU[wb_]] + rH2T, [pr[ub]])
                    b.act(SL[sl], ps[gb][:, :], AF.Silu, [pr[gb]], [rSL[sl]])
                    b.tt('dve', ACTT[:, ft, :], ps[ub][:, :], SL[sl], ALU.mult, [pr[ub], rSL[sl]], [rACT[ft]])
            for j in range(4):
                if nxt is not None:
                    norm(nxt, j)
                for h in range(2):
                    db = DN[dni % 2]
                    dni += 1
                    for ft in range(NFT):
                        b.mm(ps[db][:, :], ACTT[:, ft, j * 128:(j + 1) * 128], WD[:, ft, h * 512:(h + 1) * 512],
                             ft == 0, ft == NFT - 1, [rACT[ft], rWD], [pr[db]])
                    xs = XF[j][:, h * 512:(h + 1) * 512]
                    b.tt('dve', xs, xs, ps[db][:, :], ALU.add, [pr[db], rXF[j]], [rXF[j]])
                if nxt is not None:
                    trans(nxt, j)
                tt_ = st * 4 + j
                b.dma('sp', xdst[tt_ * 128:(tt_ + 1) * 128, :], XF[j], [rXF[j]], [Res()],
                      final=(xdst is self.final_out))
        sc.barrier()
        A.off = mark
        return
    for st in (st_list if st_list is not None else range(NST)):
        for j in range(4):
            tt_ = st * 4 + j
            b.dma('sp', XF[j], xsrc[tt_ * 128:(tt_ + 1) * 128, :], [], [rXF[j]])
            b.act(XN, XF[j], AF.Square, [rXF[j]], [rXN, rss], accum_out=ss)
            b.act(sd, ss, AF.Sqrt, [rss], [rsd], scale=1.0 / D, bias=EPS)
            sc.op('dve', lambda e: e.reciprocal(rstd, sd), [rsd], [rrs])
            b.act(XN, XF[j], AF.Copy, [rXF[j], rrs], [rXN], scale=rstd)
            if not moe:
                psT = self.psum_bf(T0).rearrange('p (k t) -> p k t', t=128)[:, 0:8, :]
                for k in range(8):
                    b.tr(psT[:, k, :], XN[:, k * 128:(k + 1) * 128], ident_b, [rXN, self.r_ident], [pr[T0]])
                b.tt('dve', H2T[:, :, j * 128:(j + 1) * 128], psT, gffn_b, ALU.mult, [pr[T0], P['r']], [rH2T[j]])
            else:
                for k in range(8):
                    bank = T0 if k < 4 else T1
                    b.tr(ps[bank][:, (k % 4) * 128:(k % 4 + 1) * 128], XN[:, k * 128:(k + 1) * 128], ident_f,
                         [rXN, self.r_identf], [pr[bank]])
                for hk, bank in ((0, T0), (1, T1)):
                    b.tt('dve', H32[:, hk * 4:(hk + 1) * 4, :], ps[bank][:, :].rearrange('p (k t) -> p k t', t=128),
                         P['gffn'][:, hk * 4:(hk + 1) * 4].unsqueeze(2).to_broadcast([128, 4, 128]), ALU.mult,
                         [pr[bank], P['r']], [rH32])
                b.cp('act', H2T[:, :, j * 128:(j + 1) * 128], H32, [rH32], [rH2T[j]])
                for k in range(8):
                    b.mm(ps[T0][:, 0:NE], H32[:, k, :], RT[:, k, :], k == 0, k == 7, [rH32, rRT], [pr[T0]])
                b.cp('dve', LG, ps[T0][:, 0:NE], [pr[T0]], [rLG])
                sc.op('dve', lambda e: e.max(M8, LG), [rLG], [rM8])
                b.tt('dve', PP[:, 1:2], M8[:, 1:2], M8[:, 0:1], ALU.subtract, [rM8], [rPP])
                b.act(PP[:, 1:2], PP[:, 1:2], AF.Sigmoid, [rPP], [rPP])
                b.ts('dve', PP[:, 0:1], PP[:, 1:2], -1.0, 1.0, ALU.mult, ALU.add, [rPP], [rPP])
                b.ts('dve', G0, LG, M8[:, 0:1], PP[:, 0:1], ALU.is_equal, ALU.mult, [rLG, rM8, rPP], [rG0])
                b.ts('dve', GATE[:, j, :], LG, M8[:, 1:2], PP[:, 1:2], ALU.is_equal, ALU.mult, [rLG, rM8, rPP],
                     [rGATE[j]])
                b.tt('dve', GATE[:, j, :], GATE[:, j, :], G0, ALU.add, [rGATE[j], rG0], [rGATE[j]])
        for ex in experts:
            wg_d, wu_d, wd_d = self.wb[(ex, 'gate')], self.wb[(ex, 'up')], self.wb[(ex, 'down')]
            wgv = wg_d.rearrange('(k p) f -> p k f', p=128)
            wuv = wu_d.rearrange('(k p) f -> p k f', p=128)
            for c in range(7):
                wb_ = wi % NW
                wi += 1
                b.dma('sp', WG[wb_], wgv[:, :, c * 512:(c + 1) * 512], [], [rWG[wb_]], nd=1024)
                b.dma('sp', WU[wb_], wuv[:, :, c * 512:(c + 1) * 512], [], [rWU[wb_]], nd=1024)
                if c == 0:
                    b.dma('sp', WD, wd_d.rearrange('(ft p) d -> p ft d', p=128), [], [rWD], nd=3584)
                for fi in range(4):
                    ft = c * 4 + fi
                    gb, ub = GU[(gui * 2) % 4], GU[(gui * 2 + 1) % 4]
                    sl = gui % 2
                    gui += 1
                    for k in range(8):
                        b.mm(ps[gb][:, :], WG[wb_][:, k, fi * 128:(fi + 1) * 128], H2T[:, k, :], k == 0, k == 7,
                             [rWG[wb_]] + rH2T, [pr[gb]])
                    for k in range(8):
                        b.mm(ps[ub][:, :], WU[wb_][:, k, fi * 128:(fi + 1) * 128], H2T[:, k, :], k == 0, k == 7,
                             [rWU[wb_]] + rH2T, [pr[ub]])
                    b.act(SL[sl], ps[gb][:, :], AF.Silu, [pr[gb]], [rSL[sl]])
                    b.tt('dve', ACTT[:, ft, :], ps[ub][:, :], SL[sl], ALU.mult, [pr[ub], rSL[sl]], [rACT[ft]])
            for j in range(4):
                for h in range(2):
                    db = DN[dni % 2]
                    dni += 1
                    for ft in range(NFT):
                        b.mm(ps[db][:, :], ACTT[:, ft, j * 128:(j + 1) * 128], WD[:, ft, h * 512:(h + 1) * 512],
                             ft == 0, ft == NFT - 1, [rACT[ft], rWD], [pr[db]])
                    xs = XF[j][:, h * 512:(h + 1) * 512]
                    if moe:
                        b.stt(xs, ps[db][:, :], GATE[:, j, ex:ex + 1], xs, ALU.mult, ALU.add,
                              [pr[db], rGATE[j], rXF[j]], [rXF[j]])
                    else:
                        b.tt('dve', xs, xs, ps[db][:, :], ALU.add, [pr[db], rXF[j]], [rXF[j]])
        for j in range(4):
            tt_ = st * 4 + j
            b.dma('sp', xdst[tt_ * 128:(tt_ + 1) * 128, :], XF[j], [rXF[j]], [Res()],
                  final=(xdst is self.final_out))
    sc.barrier()
    A.off = mark


Builder.phaseC = _phaseC


_CACHE = {}


def kernel(**inputs):
    n = 8
    if 'nc' not in _CACHE:
        _CACHE['nc'] = build_program()[0]
    nc = _CACHE['nc']
    shared = host_inputs(inputs, 0)
    in_maps = []
    for c in range(n):
        m = dict(shared)
        pos = np.asarray(inputs['positions'][c])
        posc = np.zeros(256, np.int32)
        posc[:255] = pos[np.arange(255) * 16 + 31]
        m['x'] = np.ascontiguousarray(inputs['x'][c])
        m['pos_t'] = np.ascontiguousarray(pos.reshape(32, 128).T)
        m['posc_t'] = np.ascontiguousarray(posc.reshape(2, 128).T)
        in_maps.append(m)
    res = run_bass_kernel_spmd(nc, in_maps, core_ids=list(range(n)))
    return np.stack([np.asarray(res.results[c]['out']) for c in range(n)], axis=0).astype(np.float32)


BLK = 512
NBLK = 2 * S // BLK + NE - 1
NSLOT = NBLK * BLK


def _setup_weight_conversion_sparse(self, inp):
    self.wb = {}
    self.bg_tasks = []
    for nm, shape in (('gate', [D, DFF]), ('up', [D, DFF]), ('down', [DFF, D])):
        src = inp['ffn_w_%s' % nm]
        dst = self.dscr('wb_d_%s' % nm, shape, BF16)
        self.wb[('d', nm)] = dst
        rows = shape[0]
        step = 256 if shape[1] > 2048 else 1024
        for r0 in range(0, rows, step):
            r1 = min(rows, r0 + step)
            ndesc = (r1 - r0) * (2 if shape[1] > 2048 else 1)

            def task(dst=dst, src=src, r0=r0, r1=r1, ndesc=ndesc):
                self.dma('pool', dst[r0:r1, :], src[r0:r1, :], [], [Res()], nd=ndesc, max_dma_last_dim=4096)
            self.bg_tasks.append(task)
    self.wgS = self.dscr('wgS', [7 * NE * 128, 8 * 512], BF16)
    self.wuS = self.dscr('wuS', [7 * NE * 128, 8 * 512], BF16)
    self.wdS = [self.dscr('wdS%d' % h, [NE * 128, 14 * D], BF16) for h in range(2)]
    for e in range(NE):
        for nm, dstT in (('gate', self.wgS), ('up', self.wuS)):
            srcv = inp['moe_w_%s' % nm][e].rearrange('(k p) f -> p k f', p=128)
            for c in range(7):
                r0 = c * 1024 + e * 128
                dstv = dstT[r0:r0 + 128, :].rearrange('p (k f) -> p k f', k=8)

                def task(dstv=dstv, srcv=srcv, c=c):
                    self.dma('pool', dstv, srcv[:, :, c * 512:(c + 1) * 512], [], [Res()], nd=1024)
                self.bg_tasks.append(task)
        srcd = inp['moe_w_down'][e].rearrange('(ft p) d -> p ft d', p=128)
        for h in range(2):
            dstd = self.wdS[h][e * 128:(e + 1) * 128, :].rearrange('p (ft d) -> p ft d', ft=14)

            def task(dstd=dstd, srcd=srcd, h=h):
                self.dma('pool', dstd, srcd[:, h * 14:(h + 1) * 14, :], [], [Res()], nd=1792)
            self.bg_tasks.append(task)


Builder.setup_weight_conversion_sparse = _setup_weight_conversion_sparse


def _phaseC_sparse(self, inp, l, xsrc, xdst, P):
    b, A, sc = self, self.arena, self.sc
    ps, pr = self.psb, self.psr
    mark = A.off
    ident_b, ident_f = self.ident_b, self.ident_f
    Xs = self.dscr('moe_xs', [NSLOT, D], BF16)
    Ys = self.dscr('moe_ys', [NSLOT, D], F32)
    DEST = [A.alloc([NT], I32) for _ in range(2)]
    PALL = A.alloc([NT, 2], F32)
    IDXW = A.alloc([NBLK, 7], I32)
    IDXD = A.alloc([NBLK], I32)
    rDEST, rPALL, rIDX = Res(), Res(), Res()
    mark1 = A.off
    XNB = A.alloc([NT, D], BF16)
    rXNB = [Res() for _ in range(NT)]
    XF = [A.alloc([D], F32) for _ in range(2)]
    rXF = [Res() for _ in range(2)]
    XN = A.alloc([D], F32)
    rXN = Res()
    ss = A.alloc([1], F32)
    sd = A.alloc([1], F32)
    rstd = A.alloc([1], F32)
    rss, rsd, rrs = Res(), Res(), Res()
    H32 = A.alloc([8, 128], F32)
    rH32 = Res()
    RT = A.alloc([8, NE], F32)
    rRT = Res()
    b.dma('sp', RT, inp['moe_router'].rearrange('(k p) e -> p k e', p=128), [], [rRT])
    LG = A.alloc([NE], F32)
    M8 = A.alloc([8], F32)
    rLG, rM8 = Res(), Res()
    OH = [A.alloc([NT, NE], F32) for _ in range(2)]
    rOH = Res()
    T0, T1 = 0, 1
    XNf = [XN, A.alloc([D], F32)]
    rXNf = [rXN, Res()]
    ss2 = [ss, A.alloc([1], F32)]
    sd2 = [sd, A.alloc([1], F32)]
    rstd2 = [rstd, A.alloc([1], F32)]
    rss2, rsd2, rrs2 = [rss, Res()], [rsd, Res()], [rrs, Res()]
    DD = A.alloc([NT], F32)
    rDD = Res()

    def pX(tt_):
        xb = tt_ % 2
        b.dma('sp', XF[xb], xsrc[tt_ * 128:(tt_ + 1) * 128, :], [], [rXF[xb]])
        b.act(XNf[xb], XF[xb], AF.Square, [rXF[xb]], [rXNf[xb], rss2[xb]], accum_out=ss2[xb])
        b.act(sd2[xb], ss2[xb], AF.Sqrt, [rss2[xb]], [rsd2[xb]], scale=1.0 / D, bias=EPS)
        sc.op('dve', lambda e, o=rstd2[xb], i=sd2[xb]: e.reciprocal(o, i), [rsd2[xb]], [rrs2[xb]])
        b.act(XNf[xb], XF[xb], AF.Copy, [rXF[xb], rrs2[xb]], [rXNf[xb]], scale=rstd2[xb])
        b.act(XNB[:, tt_, :], XF[xb], AF.Copy, [rXF[xb], rrs2[xb]], [rXNB[tt_]], scale=rstd2[xb])

    def pR(tt_):
        xb = tt_ % 2
        for k in range(8):
            bank = T0 if k < 4 else T1
            b.tr(ps[bank][:, (k % 4) * 128:(k % 4 + 1) * 128], XNf[xb][:, k * 128:(k + 1) * 128], ident_f,
                 [rXNf[xb], self.r_identf], [pr[bank]])
        for hk, bank in ((0, T0), (1, T1)):
            b.tt('dve', H32[:, hk * 4:(hk + 1) * 4, :], ps[bank][:, :].rearrange('p (k t) -> p k t', t=128),
                 P['gffn'][:, hk * 4:(hk + 1) * 4].unsqueeze(2).to_broadcast([128, 4, 128]), ALU.mult,
                 [pr[bank], P['r']], [rH32])
        for k in range(8):
            b.mm(ps[T0][:, 0:NE], H32[:, k, :], RT[:, k, :], k == 0, k == 7, [rH32, rRT], [pr[T0]])
        b.cp('dve', LG, ps[T0][:, 0:NE], [pr[T0]], [rLG])
        sc.op('dve', lambda e: e.max(M8, LG), [rLG], [rM8])
        b.tt('dve', DD[:, tt_:tt_ + 1], M8[:, 1:2], M8[:, 0:1], ALU.subtract, [rM8], [rDD])
        b.ts('dve', OH[0][:, tt_, :], LG, M8[:, 0:1], None, ALU.is_equal, None, [rLG, rM8], [rOH])
        b.ts('dve', OH[1][:, tt_, :], LG, M8[:, 1:2], None, ALU.is_equal, None, [rLG, rM8], [rOH])

    pX(0)
    for tt_ in range(NT):
        if tt_ + 1 < NT:
            pX(tt_ + 1)
        pR(tt_)
    b.act(PALL[:, :, 1], DD, AF.Sigmoid, [rDD], [rPALL])
    b.ts('dve', PALL[:, :, 0], PALL[:, :, 1], -1.0, 1.0, ALU.mult, ALU.add, [rPALL], [rPALL])
    LS = A.alloc([128], F32)
    ONESF = A.alloc([128], F32)
    rc = Res()
    b.memset('pool', ONESF, 1.0, [rc])
    sc.op('pool', lambda e: e.affine_select(out=LS, in_=ONESF, pattern=[[1, 128]], compare_op=ALU.is_ge,
                                            fill=0.0 if REGS is None else REGS['zero'], base=-1,
                                            channel_multiplier=-1), [rc], [rc])
    THR = A.alloc([8, NE], F32)
    BIDX = A.alloc([NBLK, NE], F32)
    PIDX = A.alloc([1], F32)
    sc.op('pool', lambda e: e.iota(THR, [[BLK, 8], [0, NE]], base=0, channel_multiplier=0,
                                   allow_small_or_imprecise_dtypes=True), [], [rc])
    sc.op('pool', lambda e: e.iota(BIDX, [[1, NBLK], [0, NE]], base=0, channel_multiplier=0,
                                   allow_small_or_imprecise_dtypes=True), [], [rc])
    sc.op('pool', lambda e: e.iota(PIDX, [[0, 1]], base=0, channel_multiplier=1,
                                   allow_small_or_imprecise_dtypes=True), [], [rc])
    MSK = A.alloc([NT, NE], F32)
    SA = A.alloc([NT, NE], F32)
    SB = A.alloc([NT, NE], F32)
    rM, rSA, rSB = Res(), Res(), Res()
    b.tt('dve', MSK, OH[0], OH[1], ALU.add, [rOH], [rM])
    CP = A.alloc([NE], F32)
    rCP = Res()
    sc.op('dve', lambda e: e.tensor_reduce(CP, MSK.rearrange('p t e -> p e t'), AX.X, ALU.add), [rM], [rCP])
    b.mm(ps[T1][:, 0:NE], LS, CP, True, True, [rc, rCP], [pr[T1]])
    b.mm(ps[T1][:, NE:2 * NE], ONESF, CP, True, True, [rc, rCP], [pr[T1]])
    BT = A.alloc([2 * NE], F32)
    rBT = Res()
    b.cp('dve', BT, ps[T1][:, 0:2 * NE], [pr[T1]], [rBT])
    b.cp('dve', SA, MSK, [rM], [rSA])
    cur, rcur, oth, roth = SA, rSA, SB, rSB
    for s_ in (1, 2, 4, 8, 16):
        b.cp('dve', oth[:, 0:s_, :], cur[:, 0:s_, :], [rcur], [roth])
        b.tt('dve', oth[:, s_:, :], cur[:, s_:, :], cur[:, 0:NT - s_, :], ALU.add, [rcur], [roth])
        cur, rcur, oth, roth = oth, roth, cur, rcur
    b.tt('dve', oth, cur, MSK, ALU.subtract, [rcur, rM], [roth])
    b.tt('dve', cur, oth, BT[:, 0:NE].unsqueeze(1).to_broadcast([128, NT, NE]), ALU.add, [roth, rBT], [rcur])
    RANK, rRANK = cur, rcur
    SCR, rSCR = oth, roth
    NBM = A.alloc([8, NE], F32)
    NBv = A.alloc([NE], F32)
    PB = A.alloc([NE], F32)
    PB2 = A.alloc([NE], F32)
    PEND = A.alloc([NE], F32)
    rN = Res()
    b.tt('dve', NBM, BT[:, NE:2 * NE].unsqueeze(1).to_broadcast([128, 8, NE]), THR, ALU.is_gt, [rBT, rc], [rN])
    sc.op('dve', lambda e: e.tensor_reduce(NBv, NBM.rearrange('p m e -> p e m'), AX.X, ALU.add), [rN], [rN])
    b.cp('dve', PB, NBv, [rN], [rN])
    src_, dst_ = PB, PB2
    for s_ in (1, 2, 4):
        b.cp('dve', dst_[:, 0:s_], src_[:, 0:s_], [rN], [rN])
        b.tt('dve', dst_[:, s_:], src_[:, s_:], src_[:, 0:NE - s_], ALU.add, [rN], [rN])
        src_, dst_ = dst_, src_
    b.cp('dve', PEND, src_, [rN], [rN])
    b.tt('dve', dst_, src_, NBv, ALU.subtract, [rN], [rN])
    PBX = dst_
    b.ts('dve', src_, PBX, float(BLK), None, ALU.mult, None, [rN], [rN])
    PS_ = src_
    b.tt('dve', SCR, RANK, PS_.unsqueeze(1).to_broadcast([128, NT, NE]), ALU.add, [rRANK, rN], [rSCR])
    DF = A.alloc([NT], F32)
    rDF = Res()
    for k in range(2):
        b.tt('dve', RANK, SCR, OH[k], ALU.mult, [rSCR, rOH], [rRANK])
        sc.op('dve', lambda e: e.tensor_reduce(DF, RANK, AX.X, ALU.add), [rRANK], [rDF])
        b.cp('dve', DEST[k], DF, [rDF], [rDEST])
    CMPB = A.alloc([NBLK, NE], F32)
    BE = A.alloc([NBLK], F32)
    BE2 = A.alloc([NBLK], F32)
    rB = Res()
    b.tt('dve', CMPB, PEND.unsqueeze(1).to_broadcast([128, NBLK, NE]), BIDX, ALU.is_le, [rN, rc], [rB])
    sc.op('dve', lambda e: e.tensor_reduce(BE, CMPB, AX.X, ALU.add), [rB], [rB])
    b.ts('dve', BE2, BE, float(NE - 1), 128.0, ALU.min, ALU.mult, [rB], [rB])
    b.ts('dve', BE, BE2, PIDX[:, 0:1], None, ALU.add, None, [rB, rc], [rB])
    b.cp('dve', IDXD, BE, [rB], [rIDX])
    for c in range(7):
        b.ts('dve', BE2, BE, float(c * 1024), None, ALU.add, None, [rB], [rB])
        b.cp('dve', IDXW[:, :, c], BE2, [rB], [rIDX])
    for tt_ in range(NT):
        for k in range(2):
            sc.dma('pool', lambda e, tt_=tt_, k=k: e.indirect_dma_start(
                out=Xs[:, :], out_offset=bass.IndirectOffsetOnAxis(ap=DEST[k][:, tt_:tt_ + 1], axis=0),
                in_=XNB[:, tt_, :], in_offset=None), [rXNB[tt_], rDEST], [Res()], nd=128)
    if self.dbg.get('moe_dump'):
        self.dump('dest0', DEST[0], [rDEST], [128, NT], I32)
        self.dump('dest1', DEST[1], [rDEST], [128, NT], I32)
        self.dump('pall', PALL, [rPALL], [128, NT, 2], F32)
        self.dump('idxd', IDXD, [rIDX], [128, NBLK], I32)
        self.dump('idxw', IDXW, [rIDX], [128, NBLK, 7], I32)
    sc.barrier()
    A.off = mark1
    XS = [A.alloc([D], BF16) for _ in range(4)]
    rXS = [Res() for _ in range(4)]
    H2T = A.alloc([8, 512], BF16)
    rH2T = [Res() for _ in range(4)]
    ACTT = A.alloc([NFT, 512], BF16)
    rACT = [Res() for _ in range(NFT)]
    WD = A.alloc([NFT, D], BF16)
    rWD = Res()
    NW = 2
    WG = [A.alloc([8, 512], BF16) for _ in range(NW)]
    WU = [A.alloc([8, 512], BF16) for _ in range(NW)]
    rWG = [Res() for _ in range(NW)]
    rWU = [Res() for _ in range(NW)]
    SL = [A.alloc([512], F32) for _ in range(2)]
    rSL = [Res() for _ in range(2)]
    YB = [A.alloc([D], F32) for _ in range(2)]
    rYB = [Res() for _ in range(2)]
    rWDh = [Res() for _ in range(2)]
    GU = (2, 3, 4, 5)
    DN = (6, 7)
    gui = dni = wi = yi = 0
    gffn_b = P['gffn'].unsqueeze(2).to_broadcast([128, 8, 128])
    psT = self.psum_bf(T0).rearrange('p (k t) -> p k t', t=128)[:, 0:8, :]
    XS2 = [XS, [A.alloc([D], BF16) for _ in range(4)]]
    rXS2 = [rXS, [Res() for _ in range(4)]]

    def load_xs(blk):
        for j in range(4):
            r0 = blk * BLK + j * 128
            b.dma('sp', XS2[blk % 2][j], Xs[r0:r0 + 128, :], [], [rXS2[blk % 2][j]])

    def trans_xs(blk, j):
        xs_, rxs_ = XS2[blk % 2][j], rXS2[blk % 2][j]
        for k in range(8):
            b.tr(psT[:, k, :], xs_[:, k * 128:(k + 1) * 128], ident_b, [rxs_, self.r_ident], [pr[T0]])
        b.tt('dve', H2T[:, :, j * 128:(j + 1) * 128], psT, gffn_b, ALU.mult, [pr[T0], P['r']], [rH2T[j]])

    load_xs(0)
    for j in range(4):
        trans_xs(0, j)
    for blk in range(NBLK):
        if blk + 1 < NBLK:
            load_xs(blk + 1)
        def gather_chunk(blk, c):
            wb2 = (blk * 7 + c) % NW
            sc.dma('pool', lambda e, wb2=wb2, blk=blk, c=c: e.indirect_dma_start(
                out=WG[wb2].rearrange('p k f -> p (k f)'), out_offset=None, in_=self.wgS[:, :],
                in_offset=bass.IndirectOffsetOnAxis(ap=IDXW[:, blk, c:c + 1], axis=0)), [rIDX], [rWG[wb2]], nd=512)
            sc.dma('pool', lambda e, wb2=wb2, blk=blk, c=c: e.indirect_dma_start(
                out=WU[wb2].rearrange('p k f -> p (k f)'), out_offset=None, in_=self.wuS[:, :],
                in_offset=bass.IndirectOffsetOnAxis(ap=IDXW[:, blk, c:c + 1], axis=0)), [rIDX], [rWU[wb2]], nd=512)

        def gather_wd(blk, h):
            sc.dma('pool', lambda e, blk=blk, h=h: e.indirect_dma_start(
                out=WD[:, h * 14:(h + 1) * 14, :].rearrange('p ft d -> p (ft d)'), out_offset=None,
                in_=self.wdS[h][:, :], in_offset=bass.IndirectOffsetOnAxis(ap=IDXD[:, blk:blk + 1], axis=0)),
                [rIDX], [rWDh[h]], nd=1024)

        if blk == 0:
            gather_chunk(0, 0)
            gather_chunk(0, 1)
        gather_wd(blk, 0)
        for c in range(7):
            wb_ = (blk * 7 + c) % NW
            if c == 2:
                gather_wd(blk, 1)
            for fi in range(4):
                ft = c * 4 + fi
                gb, ub = GU[(gui * 2) % 4], GU[(gui * 2 + 1) % 4]
                sl = gui % 2
                gui += 1
                for k in range(8):
                    b.mm(ps[gb][:, :], WG[wb_][:, k, fi * 128:(fi + 1) * 128], H2T[:, k, :], k == 0, k == 7,
                         [rWG[wb_]] + rH2T, [pr[gb]])
                for k in range(8):
                    b.mm(ps[ub][:, :], WU[wb_][:, k, fi * 128:(fi + 1) * 128], H2T[:, k, :], k == 0, k == 7,
                         [rWU[wb_]] + rH2T, [pr[ub]])
                b.act(SL[sl], ps[gb][:, :], AF.Silu, [pr[gb]], [rSL[sl]])
                b.tt('dve', ACTT[:, ft, :], ps[ub][:, :], SL[sl], ALU.mult, [pr[ub], rSL[sl]], [rACT[ft]])
            nc_, nb_ = c + 2, blk
            if nc_ >= 7:
                nc_, nb_ = nc_ - 7, blk + 1
            if nb_ < NBLK:
                gather_chunk(nb_, nc_)
        for j in range(4):
            yb = yi % 2
            yi += 1
            for h in range(2):
                db = DN[dni % 2]
                dni += 1
                for ft in range(NFT):
                    b.mm(ps[db][:, :], ACTT[:, ft, j * 128:(j + 1) * 128], WD[:, ft, h * 512:(h + 1) * 512],
                         ft == 0, ft == NFT - 1, [rACT[ft], rWDh[ft // 14]], [pr[db]])
                if h == 0:
                    b.cp('act', YB[yb][:, 0:512], ps[db][:, :], [pr[db]], [rYB[yb]])
                else:
                    b.cp('dve', YB[yb][:, 512:1024], ps[db][:, :], [pr[db]], [rYB[yb]])
            r0 = blk * BLK + j * 128
            b.dma('sp', Ys[r0:r0 + 128, :], YB[yb], [rYB[yb]], [Res()])
            if blk + 1 < NBLK:
                trans_xs(blk + 1, j)
    sc.barrier()
    A.off = mark1
    NC3 = 6
    XC = [A.alloc([D], F32) for _ in range(NC3)]
    Y0 = [A.alloc([D], F32) for _ in range(NC3)]
    Y1 = [A.alloc([D], F32) for _ in range(NC3)]
    rXC = [Res() for _ in range(NC3)]
    rY0 = [Res() for _ in range(NC3)]
    rY1 = [Res() for _ in range(NC3)]
    for tt_ in range(NT):
        xb = tt_ % NC3
        b.dma('sp', XC[xb], xsrc[tt_ * 128:(tt_ + 1) * 128, :], [], [rXC[xb]])
        for k, (Yk, rYk) in enumerate(((Y0, rY0), (Y1, rY1))):
            sc.dma('pool', lambda e, Yk=Yk, k=k, tt_=tt_, xb=xb: e.indirect_dma_start(
                out=Yk[xb][:, :], out_offset=None, in_=Ys[:, :],
                in_offset=bass.IndirectOffsetOnAxis(ap=DEST[k][:, tt_:tt_ + 1], axis=0)), [rDEST], [rYk[xb]], nd=128)
        b.stt(XC[xb], Y0[xb], PALL[:, tt_, 0:1], XC[xb], ALU.mult, ALU.add, [rY0[xb], rPALL, rXC[xb]], [rXC[xb]])
        b.stt(XC[xb], Y1[xb], PALL[:, tt_, 1:2], XC[xb], ALU.mult, ALU.add, [rY1[xb], rPALL, rXC[xb]], [rXC[xb]])
        b.dma('sp', xdst[tt_ * 128:(tt_ + 1) * 128, :], XC[xb], [rXC[xb]], [Res()],
              final=(xdst is self.final_out))
    sc.barrier()
    A.off = mark


Builder.phaseC_sparse = _phaseC_sparse
```

```python
import math
import numpy as np
import concourse.bass as bass
import concourse.mybir as mybir
from concourse.bass_utils import run_bass_kernel_spmd

F32 = mybir.dt.float32
BF16 = mybir.dt.bfloat16
I32 = mybir.dt.int32
AF = mybir.ActivationFunctionType
ALU = mybir.AluOpType
AX = mybir.AxisListType

S = 4096
D = 1024
NT = S // 128
NST = S // 512
DFF = 3584
NFT = DFF // 128
NE = 8
EPS = 1e-6
NEG = -30000.0
ENGS = ('pe', 'act', 'dve', 'pool', 'sp')
REGS = None


class Res:
    __slots__ = ('name', 'w', 'rs', 'excl')

    def __init__(self, name='', excl=False):
        self.name = name
        self.w = None
        self.rs = {}
        self.excl = excl


class Sched:
    NEAR = 4

    def __init__(self, n_dma_sems=32):
        self.streams = {e: [] for e in ENGS}
        self.cnt = {e: 0 for e in ENGS}
        self.seen = {e: {} for e in ENGS}
        self.dcnt = [0] * n_dma_sems
        self.dnext = 0
        self.final = []
        self.serialize_dma = False
        self.inflight = {}

    def op(self, eng, fn, reads=(), writes=()):
        idx = self.cnt[eng] + 1
        me = ('e', eng)
        deps = {}
        same_raw = 0
        for r in reads:
            if r.w is not None:
                k, v = r.w
                if k == me:
                    if v > same_raw:
                        same_raw = v
                elif deps.get(k, 0) < v:
                    deps[k] = v
            if r.excl:
                for k, v in r.rs.items():
                    if k != me and deps.get(k, 0) < v:
                        deps[k] = v
        for r in writes:
            if r.w is not None:
                k, v = r.w
                if k != me and deps.get(k, 0) < v:
                    deps[k] = v
            for k, v in r.rs.items():
                if k != me and deps.get(k, 0) < v:
                    deps[k] = v
        waits = []
        if same_raw and idx - same_raw <= self.NEAR:
            waits.append((me, same_raw))
        sn = self.seen[eng]
        for k, v in deps.items():
            if sn.get(k, 0) < v:
                sn[k] = v
                waits.append((k, v))
        self.streams[eng].append((waits, fn, me))
        self.cnt[eng] = idx
        tok = (me, idx)
        for r in reads:
            if r.rs.get(me, 0) < idx:
                r.rs[me] = idx
        for r in writes:
            r.w = tok
            r.rs = {}
        return tok

    def dma(self, q, fn, reads=(), writes=(), final=False, nd=128):
        k = self.dnext
        self.dnext = (self.dnext + 1) % len(self.dcnt)
        v = self.dcnt[k] + 16
        dk = ('d', k)
        deps = {}
        if self.dcnt[k] > 0:
            deps[dk] = self.dcnt[k]
        fl = self.inflight.setdefault(q, [])
        cap_n = 6 if q == 'pool' else 12
        cap_d = 3000 if q == 'pool' else 1 << 30
        while fl and (len(fl) >= cap_n or sum(x[2] for x in fl) + nd > cap_d):
            ok, ov, _ = fl.pop(0)
            if deps.get(ok, 0) < ov:
                deps[ok] = ov
        fl.append((dk, v, nd))
        for r in reads:
            if r.w is not None:
                k2, v2 = r.w
                if deps.get(k2, 0) < v2:
                    deps[k2] = v2
        for r in writes:
            if r.w is not None:
                k2, v2 = r.w
                if deps.get(k2, 0) < v2:
                    deps[k2] = v2
            for k2, v2 in r.rs.items():
                if deps.get(k2, 0) < v2:
                    deps[k2] = v2
        sn = self.seen[q]
        waits = []
        for k2, v2 in deps.items():
            if sn.get(k2, 0) < v2:
                sn[k2] = v2
                waits.append((k2, v2))
        self.streams[q].append((waits, fn, dk))
        self.dcnt[k] = v
        tok = (dk, v)
        for r in reads:
            if r.rs.get(dk, 0) < v:
                r.rs[dk] = v
        for r in writes:
            r.w = tok
            r.rs = {}
        if final:
            self.final.append(tok)
        if self.serialize_dma:
            sn[dk] = v
            self.streams[q].append(([(dk, v)], None, None))
        return tok

    def barrier(self):
        snap_e = dict(self.cnt)
        snap_d = list(self.dcnt)
        for e in ENGS:
            waits = []
            sn = self.seen[e]
            for e2 in ENGS:
                k = ('e', e2)
                if e2 != e and snap_e[e2] > 0 and sn.get(k, 0) < snap_e[e2]:
                    sn[k] = snap_e[e2]
                    waits.append((k, snap_e[e2]))
            for i, v in enumerate(snap_d):
                k = ('d', i)
                if v > 0 and sn.get(k, 0) < v:
                    sn[k] = v
                    waits.append((k, v))
            self.streams[e].append((waits, None, None))

    def emit(self, nc, stack):
        esem = {e: stack.enter_context(nc.semaphore('es_' + e)) for e in ENGS}
        dsem = [stack.enter_context(nc.semaphore('ds_%d' % i)) for i in range(len(self.dcnt))]

        def semof(k):
            return esem[k[1]] if k[0] == 'e' else dsem[k[1]]

        fin = {}
        for k, v in self.final:
            if fin.get(k, 0) < v:
                fin[k] = v

        def make(name):
            def body(e):
                if name == 'pool':
                    global REGS
                    REGS = {'zero': e.to_reg(0.0), 'neg': e.to_reg(NEG)}
                for waits, fn, sig in self.streams[name]:
                    for (k, v) in waits:
                        e.wait_ge(semof(k), v)
                    if fn is None:
                        continue
                    ins = fn(e)
                    if sig[0] == 'e':
                        ins.then_inc(esem[sig[1]], 1)
                    else:
                        ins.then_inc(dsem[sig[1]], 16)
                if name == 'sp':
                    for k, v in fin.items():
                        e.wait_ge(semof(k), v)
            return body

        with nc.Block() as block:
            block.tensor(make('pe'))
            block.scalar(make('act'))
            block.vector(make('dve'))
            block.gpsimd(make('pool'))
            block.sync(make('sp'))


def _dtsize(dt):
    return {F32: 4, BF16: 2, I32: 4}[dt]


class Arena:
    def __init__(self, ap_u8, size):
        self.ap = ap_u8
        self.size = size
        self.off = 0

    def alloc(self, free_shape, dt):
        n = 1
        for s in free_shape:
            n *= s
        nb = n * _dtsize(dt)
        nb_al = (nb + 63) // 64 * 64
        assert self.off + nb_al <= self.size, ("SBUF arena overflow", self.off, nb_al, self.size)
        a = self.ap[:, self.off:self.off + nb].bitcast(dt)
        self.off += nb_al
        if len(free_shape) > 1:
            names = ' '.join('a%d' % i for i in range(len(free_shape)))
            kw = {'a%d' % i: free_shape[i] for i in range(1, len(free_shape))}
            a = a.rearrange('p (%s) -> p %s' % (names, names), **kw)
        return a


def _ROPE_INV_FREQ():
    return [float(np.float32(500000.0) ** np.float32(-2.0 * i / 16.0)) for i in range(8)]


W_BLOCKS = [
    (0, 0, 512),
    (512, 768, 128),
    (640, 1024, 128),
    (768, 896, 128),
    (896, 1152, 128),
    (1024, 1280, 24),
    (1048, 512, 128),
    (1176, 640, 128),
    (1304, 1304, 1024),
]
WIN = 2328


class Builder:
    def __init__(self, nc, stack, dbg=None):
        self.bg_tasks = []
        self.final_out = None
        self.nc = nc
        self.stack = stack
        self.sc = Sched()
        self.dbg = dbg or {}
        self.dbg_out = {}
        arena_t = stack.enter_context(nc.sbuf_tensor('arena', [128, 175 * 1024], mybir.dt.uint8))
        self.arena = Arena(arena_t, 175 * 1024)
        self.psb = []
        self.psr = []
        for i in range(8):
            t = stack.enter_context(nc.psum_tensor('psb%d' % i, [128, 512], F32))
            self.psb.append(t)
            self.psr.append(Res('ps%d' % i, excl=True))

    def din(self, name, shape, dt=F32):
        return self.nc.dram_tensor(name, list(shape), dt, kind='ExternalInput').ap()

    def dout(self, name, shape, dt=F32):
        return self.nc.dram_tensor(name, list(shape), dt, kind='ExternalOutput').ap()

    def dscr(self, name, shape, dt=F32):
        return self.nc.dram_tensor(name, list(shape), dt).ap()

    def dump(self, name, ap, res_list, shape, dt=F32):
        o = self.dout('dbg_' + name, shape, dt)
        self.dbg_out['dbg_' + name] = (shape, dt)
        self.sc.dma('sp', lambda e, o=o, ap=ap: e.dma_start(out=o, in_=ap), reads=res_list, writes=[Res()],
                    final=True)

    def mm(self, out, lhsT, rhs, start, stop, reads, writes):
        self.sc.op('pe', lambda e: e.matmul(out, lhsT, rhs, start=start, stop=stop), reads, writes)

    def tr(self, out, in_, ident, reads, writes):
        self.sc.op('pe', lambda e: e.transpose(out, in_, ident), reads, writes)

    def act(self, out, in_, func, reads, writes, **kw):
        self.sc.op('act', lambda e: e.activation(out, in_, func, **kw), reads, writes)

    def ts(self, eng, out, in0, s1, s2, op0, op1, reads, writes):
        if op1 is None:
            self.sc.op(eng, lambda e: e.tensor_scalar(out, in0, s1, None, op0), reads, writes)
        else:
            self.sc.op(eng, lambda e: e.tensor_scalar(out, in0, s1, s2, op0, op1), reads, writes)

    def tt(self, eng, out, in0, in1, op, reads, writes):
        self.sc.op(eng, lambda e: e.tensor_tensor(out, in0, in1, op), reads, writes)

    def stt(self, out, in0, scalar, in1, op0, op1, reads, writes):
        self.sc.op('dve', lambda e: e.scalar_tensor_tensor(out, in0, scalar, in1, op0, op1), reads, writes)

    def cp(self, eng, out, in_, reads, writes):
        if eng == 'act':
            self.sc.op('act', lambda e: e.copy(out, in_), reads, writes)
        else:
            self.sc.op(eng, lambda e: e.tensor_copy(out, in_), reads, writes)

    def memset(self, eng, ap, val, writes):
        self.sc.op(eng, lambda e: e.memset(ap, val), (), writes)

    def dma(self, q, out, in_, reads, writes, final=False, nd=128, **kw):
        return self.sc.dma(q, lambda e: e.dma_start(out=out, in_=in_, **kw), reads, writes, final=final, nd=nd)

    def psum_bf(self, i):
        return self.psb[i][:, :].bitcast(BF16)

    def phase0(self, inp):
        b, A = self, self.arena
        self.ones_f = A.alloc([128], F32)
        self.ident_f = A.alloc([128], F32)
        self.ident_b = A.alloc([128], BF16)
        r1, r2, r3 = Res(), Res(), Res()
        b.memset('pool', self.ones_f, 1.0, [r1])
        ones_f, ident_f = self.ones_f, self.ident_f
        b.sc.op('pool', lambda e: e.affine_select(out=ident_f, in_=ones_f, pattern=[[-1, 128]],
                                                  compare_op=ALU.is_equal, fill=0.0 if REGS is None else REGS['zero'], base=0,
                                                  channel_multiplier=1), [r1], [r2])
        b.cp('pool', self.ident_b, self.ident_f, [r2], [r3])
        self.r_ident = r3
        self.r_identf = r2

        self.causal01 = A.alloc([128], BF16)
        self.low01 = A.alloc([128], BF16)
        self.ones_b = A.alloc([128], BF16)
        self.r_masks = Res()
        b.memset('pool', self.ones_b, 1.0, [self.r_masks])
        ones_b, causal01, low01 = self.ones_b, self.causal01, self.low01
        self.sc.op('pool', lambda e: e.affine_select(out=causal01, in_=ones_b, pattern=[[1, 128]],
                   compare_op=ALU.is_ge, fill=0.0 if REGS is None else REGS['zero'], base=0, channel_multiplier=-1), [self.r_masks], [self.r_masks])
        self.sc.op('pool', lambda e: e.affine_select(out=low01, in_=ones_b, pattern=[[-1, 128]],
                   compare_op=ALU.is_ge, fill=0.0 if REGS is None else REGS['zero'], base=-1, channel_multiplier=1), [self.r_masks], [self.r_masks])
        self.zeros_b4 = A.alloc([4, 128], BF16)
        self.causalN = A.alloc([4, 128], BF16)
        self.lowN = A.alloc([4, 128], BF16)
        b.memset('pool', self.zeros_b4, 0.0, [self.r_masks])
        zeros_b4, causalN, lowN = self.zeros_b4, self.causalN, self.lowN
        self.sc.op('pool', lambda e: e.affine_select(out=causalN, in_=zeros_b4, pattern=[[0, 4], [1, 128]],
                   compare_op=ALU.is_ge, fill=NEG if REGS is None else REGS['neg'], base=0,
                   channel_multiplier=-1), [self.r_masks], [self.r_masks])
        self.sc.op('pool', lambda e: e.affine_select(out=lowN, in_=zeros_b4, pattern=[[0, 4], [-1, 128]],
                   compare_op=ALU.is_ge, fill=NEG if REGS is None else REGS['neg'], base=-1,
                   channel_multiplier=1), [self.r_masks], [self.r_masks])
        self.sinT = A.alloc([34, 8], F32)
        self.cosT = A.alloc([34, 8], F32)
        mark0 = A.off
        pos_i = A.alloc([34], I32)
        rp = Res()
        b.dma('sp', pos_i[:, 0:32], inp['pos_t'], [], [rp])
        b.dma('sp', pos_i[:, 32:34], inp['posc_t'], [], [rp])
        pos_f = A.alloc([34], F32)
        rpf = Res()
        b.cp('dve', pos_f, pos_i, [rp], [rpf])
        invf = A.alloc([8], F32)
        rif = Res()
        for i, v in enumerate(_ROPE_INV_FREQ()):
            b.memset('pool', invf[:, i:i + 1], v, [rif])
        ang = A.alloc([34, 8], F32)
        angc = A.alloc([34, 8], F32)
        ra, rac = Res(), Res()
        b.tt('dve', ang, pos_f.unsqueeze(2).to_broadcast([128, 34, 8]),
             invf.unsqueeze(1).to_broadcast([128, 34, 8]), ALU.mult, [rpf, rif], [ra])
        b.ts('dve', angc, ang, math.pi / 2, None, ALU.add, None, [ra], [rac])
        self.r_rope = Res()
        MAGIC = 12582912.0
        TWO_PI = 2.0 * math.pi
        for src, rsrc, dst in ((ang, ra, self.sinT), (angc, rac, self.cosT)):
            u = A.alloc([34, 8], F32)
            k2 = A.alloc([34, 8], F32)
            ru, rk = Res(), Res()
            b.ts('dve', u, src, 1.0 / TWO_PI, MAGIC, ALU.mult, ALU.add, [rsrc], [ru])
            b.ts('dve', k2, u, -MAGIC, TWO_PI, ALU.add, ALU.mult, [ru], [rk])
            b.tt('dve', u, src, k2, ALU.subtract, [rsrc, rk], [ru])
            b.ts('dve', k2, u, -3.1415, 3.1415, ALU.max, ALU.min, [ru], [rk])
            b.act(dst, k2, AF.Sin, [rk], [self.r_rope])
        b.sc.barrier()
        A.off = mark0

    def load_layer_small(self, inp, l):
        b, A = self, self.arena
        P = {}
        r = Res()
        P['r'] = r
        P['gin'] = A.alloc([8], F32)
        b.dma('sp', P['gin'], inp['gin_t'][l], [], [r])
        P['gffn'] = A.alloc([8], F32)
        b.dma('sp', P['gffn'], inp['gffn_t'][l], [], [r])
        gq = A.alloc([64], F32)
        gk = A.alloc([3, 64], F32)
        r0 = Res()
        b.dma('sp', gq, inp['q_norm_g'][l].partition_broadcast(128), [], [r0])
        b.dma('sp', gk, inp['k_norm_g'][l].rearrange('a b -> (a b)').partition_broadcast(128)
              .rearrange('p (a b) -> p a b', b=64), [], [r0])
        P['gk'] = gk
        gqk = A.alloc([12, 64], F32)
        b.ts('dve', gqk[:, 0:8, :], gq.unsqueeze(1).to_broadcast([128, 8, 64]), 0.125, None, ALU.mult, None,
             [r0], [r])
        b.cp('dve', gqk[:, 8:10, :], gk[:, 1:2, :].to_broadcast([128, 2, 64]), [r0], [r])
        b.cp('dve', gqk[:, 10:12, :], gk[:, 2:3, :].to_broadcast([128, 2, 64]), [r0], [r])
        P['gqk'] = gqk
        return P

    def phaseA(self, inp, l, xsrc, P, st_list=None):
        b, A, sc = self, self.arena, self.sc
        self.KE = [A.alloc([S], BF16) for _ in range(2)]
        self.r_E = Res()
        for kh in range(2):
            reg = self.KE[kh][64:128, :] if kh == 0 else self.KE[kh][0:64, :]
            b.memset('pool', reg, 1.0, [self.r_E])
            sc_ = self.sc
            sc_.op('pool', lambda e, reg=reg: e.affine_select(out=reg, in_=reg, pattern=[[1, S]],
                   compare_op=ALU.is_ge, fill=0.0 if REGS is None else REGS['zero'], base=0, channel_multiplier=-64), [self.r_E], [self.r_E])
            sc_.op('pool', lambda e, reg=reg: e.affine_select(out=reg, in_=reg, pattern=[[-1, S]],
                   compare_op=ALU.is_ge, fill=0.0 if REGS is None else REGS['zero'], base=63, channel_multiplier=64), [self.r_E], [self.r_E])
        self.QT = A.alloc([4, S], BF16)
        self.KwT = A.alloc([S], BF16)
        self.VsA = A.alloc([NT, 2, 65], BF16)
        self.VwA = A.alloc([NT, 2, 65], BF16)
        self.gates = A.alloc([NT, 24], F32)
        self.KcmpT = A.alloc([256], BF16)
        self.VcA = A.alloc([2, 2, 128], BF16)
        self.markKc = A.off
        self.r_q = [Res() for _ in range(NT)]
        self.r_ks = [Res() for _ in range(NT)]
        self.r_kw = [Res() for _ in range(NT)]
        self.r_vs = [Res() for _ in range(NT)]
        self.r_vw = [Res() for _ in range(NT)]
        self.r_g = [Res() for _ in range(NT)]
        self.r_kc = [Res() for _ in range(NST)]
        self.r_vc = [Res() for _ in range(NST)]
        rones = Res()
        b.memset('pool', self.VsA[:, :, :, 64:65], 1.0, [rones])
        b.memset('pool', self.VwA[:, :, :, 64:65], 1.0, [rones])
        for t in range(NT):
            self.r_vs[t].w = rones.w
            self.r_vw[t].w = rones.w
        mark = A.off
        W = A.alloc([8, WIN], BF16)
        rW = [Res() for _ in W_BLOCKS]
        wsrc = inp['w_in'][l].rearrange('(k p) c -> p k c', p=128)
        for i, (do, so, n) in enumerate(W_BLOCKS):
            b.dma('pool', W[:, :, do:do + n], wsrc[:, :, so:so + n], [], [rW[i]], nd=1024)
        XB = [A.alloc([D], F32) for _ in range(2)]
        rXB = [Res() for _ in range(2)]
        XN = [A.alloc([D], BF16) for _ in range(2)]
        rXN = [Res() for _ in range(2)]
        HT = [A.alloc([8, 512], BF16) for _ in range(2)]
        rHT = [[Res() for _ in range(4)] for _ in range(2)]
        KCt = [A.alloc([512], BF16) for _ in range(2)]
        rKCt = [Res() for _ in range(2)]
        ss = [A.alloc([1], F32) for _ in range(2)]
        sd = [A.alloc([1], F32) for _ in range(2)]
        rstd = [A.alloc([1], F32) for _ in range(2)]
        rss = [Res() for _ in range(2)]
        rsd = [Res() for _ in range(2)]
        rrs = [Res() for _ in range(2)]

        SQ = A.alloc([12, 64], F32)
        rSQ = Res()
        ss12 = A.alloc([12], F32)
        sd12 = A.alloc([12], F32)
        rs12 = A.alloc([12], F32)
        r12a, r12b, r12c = Res(), Res(), Res()
        T1s = [A.alloc([12, 64], F32) for _ in range(2)]
        rT1s = [Res() for _ in range(2)]
        RA = A.alloc([4, 12, 8], F32)
        rRA = [Res() for _ in range(4)]
        QBs = [A.alloc([12, 64], BF16) for _ in range(2)]
        rQBs = [Res() for _ in range(2)]
        SIG = [A.alloc([512], BF16) for _ in range(4)]
        rSIG = [Res() for _ in range(4)]
        HGt = [A.alloc([512], BF16) for _ in range(2)]
        rHGt = [Res() for _ in range(2)]
        zt = A.alloc([32], BF16)
        rz = Res()
        b.memset('pool', zt, 0.0, [rz])
        hg = self.hg_scr
        for ct in range(4):
            b.dma('sp', hg[ct][:, 0:30], zt[:, 0:30], [rz], [Res()])
        ps, pr = self.psb, self.psr
        PT, PA, PB, PC, PQ, PF0, PF1 = 0, 1, 2, 3, 4, 5, 6
        psT = self.psum_bf(PT).rearrange('p (k t) -> p k t', t=128)[:, 0:8, :]
        psQ = self.psum_bf(PQ).rearrange('p (k t) -> p k t', t=128)[:, 0:6, :]
        gin_b = P['gin'].unsqueeze(2).to_broadcast([128, 8, 128])
        gqk = P['gqk']
        ident_b = self.ident_b
        fstate = {'fidx': 0, 'kci': 0}
        sts = list(st_list if st_list is not None else range(NST))
        tiles = [(st, j) for st in sts for j in range(4)]

        def stageX(st, j):
            hb = st % 2
            tt_ = st * 4 + j
            xb = tt_ % 2
            b.bg(1)
            b.dma('sp', XB[xb], xsrc[tt_ * 128:(tt_ + 1) * 128, :], [], [rXB[xb]])
            b.act(XN[xb], XB[xb], AF.Square, [rXB[xb]], [rXN[xb], rss[xb]], accum_out=ss[xb])
            b.act(sd[xb], ss[xb], AF.Sqrt, [rss[xb]], [rsd[xb]], scale=1.0 / D, bias=EPS)
            sc.op('dve', lambda e, o=rstd[xb], i=sd[xb]: e.reciprocal(o, i), [rsd[xb]], [rrs[xb]])
            b.act(XN[xb], XB[xb], AF.Copy, [rXB[xb], rrs[xb]], [rXN[xb]], scale=rstd[xb])
            for k in range(8):
                b.tr(psT[:, k, :], XN[xb][:, k * 128:(k + 1) * 128], ident_b, [rXN[xb], self.r_ident], [pr[PT]])
            b.tt('dve', HT[hb][:, :, j * 128:(j + 1) * 128], psT, gin_b, ALU.mult, [pr[PT], P['r']], [rHT[hb][j]])

        def stageYmm(st, j):
            hb = st % 2
            for bank, c0, c1, rw in ((PA, 0, 512, [rW[0]]), (PB, 512, 1024, rW[1:5]), (PC, 1024, 1048, [rW[5]])):
                for k in range(8):
                    b.mm(ps[bank][:, 0:c1 - c0], HT[hb][:, k, j * 128:(j + 1) * 128], W[:, k, c0:c1],
                         k == 0, k == 7, [rHT[hb][j]] + rw, [pr[bank]])

        def stageYpost(st, j):
            tt_ = st * 4 + j
            T1, rT1 = T1s[tt_ % 2], rT1s[tt_ % 2]
            b.act(SQ[:, 0:8, :], ps[PA][:, 0:512].rearrange('p (h d) -> p h d', d=64), AF.Square, [pr[PA]], [rSQ])
            b.act(SQ[:, 8:12, :], ps[PB][:, 0:256].rearrange('p (h d) -> p h d', d=64), AF.Square, [pr[PB]], [rSQ])
            sc.op('dve', lambda e: e.tensor_reduce(ss12, SQ, AX.X, ALU.add), [rSQ], [r12a])
            b.act(sd12, ss12, AF.Sqrt, [r12a], [r12b], scale=1.0 / 64, bias=EPS)
            sc.op('dve', lambda e: e.reciprocal(rs12, sd12), [r12b], [r12c])
            b.tt('dve', T1[:, 0:8, :], ps[PA][:, 0:512].rearrange('p (h d) -> p h d', d=64),
                 rs12[:, 0:8].unsqueeze(2).to_broadcast([128, 8, 64]), ALU.mult, [pr[PA], r12c], [rT1])
            b.tt('dve', T1[:, 8:12, :], ps[PB][:, 0:256].rearrange('p (h d) -> p h d', d=64),
                 rs12[:, 8:12].unsqueeze(2).to_broadcast([128, 4, 64]), ALU.mult, [pr[PB], r12c], [rT1])
            b.cp('dve', self.VsA[:, tt_, :, 0:64], ps[PB][:, 256:384].rearrange('p (h d) -> p h d', d=64),
                 [pr[PB]], [self.r_vs[tt_]])
            b.cp('dve', self.VwA[:, tt_, :, 0:64], ps[PB][:, 384:512].rearrange('p (h d) -> p h d', d=64),
                 [pr[PB]], [self.r_vw[tt_]])
            b.cp('act', self.gates[:, tt_, :], ps[PC][:, 0:24], [pr[PC]], [self.r_g[tt_]])

        def stageYpostB(st, j):
            tt_ = st * 4 + j
            QB, rQB = QBs[tt_ % 2], rQBs[tt_ % 2]
            T1, rT1 = T1s[tt_ % 2], rT1s[tt_ % 2]
            T2, rT2 = T1, rT1
            b.tt('pool', T2, T1, gqk, ALU.mult, [rT1, P['r']], [rT2])
            cosb = self.cosT[:, tt_:tt_ + 1, :].to_broadcast([128, 12, 8])
            sinb = self.sinT[:, tt_:tt_ + 1, :].to_broadcast([128, 12, 8])
            x1 = T2[:, :, 0:8]
            x2 = T2[:, :, 8:16]
            b.tt('dve', RA[:, 0], x1, cosb, ALU.mult, [rT2, self.r_rope], [rRA[0]])
            b.tt('pool', RA[:, 1], x2, sinb, ALU.mult, [rT2, self.r_rope], [rRA[1]])
            b.tt('dve', RA[:, 2], x2, cosb, ALU.mult, [rT2, self.r_rope], [rRA[2]])
            b.tt('pool', RA[:, 3], x1, sinb, ALU.mult, [rT2, self.r_rope], [rRA[3]])
            qdst = QB[:, 0:8, :].rearrange('p (pr hf) d -> p hf pr d', hf=2)

            def split(apx):
                return apx[:, 0:8].rearrange('p (hf pr) d -> p hf pr d', hf=2), apx[:, 8:12]
            r1q, r1k = split(RA[:, 0])
            s1q, s1k = split(RA[:, 1])
            r2q, r2k = split(RA[:, 2])
            s2q, s2k = split(RA[:, 3])
            b.tt('dve', qdst[:, :, :, 0:8], r1q, s1q, ALU.subtract, [rRA[0], rRA[1]], [rQB])
            b.tt('dve', QB[:, 8:12, 0:8], r1k, s1k, ALU.subtract, [rRA[0], rRA[1]], [rQB])
            b.tt('dve', qdst[:, :, :, 8:16], r2q, s2q, ALU.add, [rRA[2], rRA[3]], [rQB])
            b.tt('dve', QB[:, 8:12, 8:16], r2k, s2k, ALU.add, [rRA[2], rRA[3]], [rQB])
            t2q, t2k = split(T2)
            b.cp('pool', qdst[:, :, :, 16:64], t2q[:, :, :, 16:64], [rT2], [rQB])
            b.cp('pool', QB[:, 8:12, 16:64], t2k[:, :, 16:64], [rT2], [rQB])

        def stageYpost2(st, j):
            tt_ = st * 4 + j
            QB, rQB = QBs[tt_ % 2], rQBs[tt_ % 2]
            QBf = QB.rearrange('p h d -> p (h d)')
            for c in range(6):
                b.tr(psQ[:, c, :], QBf[:, c * 128:(c + 1) * 128], ident_b, [rQB, self.r_ident], [pr[PQ]])
            b.cp('act', self.QT[:, :, tt_ * 128:(tt_ + 1) * 128], psQ[:, 0:4, :], [pr[PQ]], [self.r_q[tt_]])
            b.cp('act', self.KE[0][0:64, tt_ * 128:(tt_ + 1) * 128], psQ[0:64, 4, :], [pr[PQ]], [self.r_ks[tt_]])
            b.cp('act', self.KE[1][64:128, tt_ * 128:(tt_ + 1) * 128], psQ[64:128, 4, :], [pr[PQ]],
                 [self.r_ks[tt_]])
            b.cp('act', self.KwT[:, tt_ * 128:(tt_ + 1) * 128], psQ[:, 5, :], [pr[PQ]], [self.r_kw[tt_]])

        def f_order():
            order = [('kc', 1048, rW[6]), ('vc', 1176, rW[7])]
            for ct in range(4):
                order.append(('g%d' % ct, 1304 + 512 + ct * 128, rW[8]))
            for ct in range(4):
                order.append(('a%d' % ct, 1304 + ct * 128, rW[8]))
            return order
        FGROUPS = (f_order()[0:4], f_order()[4:7], f_order()[7:10], [])

        def stageF(st, grp):
            hb = st % 2
            for name, c0, rw in FGROUPS[grp]:
                bank = PF0 if fstate['fidx'] % 2 == 0 else PF1
                fstate['fidx'] += 1
                for k in range(8):
                    b.mm(ps[bank][:, :], W[:, k, c0:c0 + 128], HT[hb][:, k, :], k == 0, k == 7,
                         rHT[hb] + [rw], [pr[bank]])
                if name in ('kc', 'vc'):
                    kb = fstate['kci'] % 2
                    fstate['kci'] += 1
                    b.cp('act', KCt[kb], ps[bank][:, :], [pr[bank]], [rKCt[kb]])
                    dstd = self.kc_scr if name == 'kc' else self.vc_scr
                    b.dma('sp', dstd[:, st * 512:(st + 1) * 512], KCt[kb], [rKCt[kb]], [Res()])
                elif name[0] == 'g':
                    ct = int(name[1])
                    b.act(SIG[ct], ps[bank][:, :], AF.Sigmoid, [pr[bank]], [rSIG[ct]])
                else:
                    ct = int(name[1])
                    sb = ct % 2
                    b.tt('dve', HGt[sb], ps[bank][:, :], SIG[ct], ALU.mult, [pr[bank], rSIG[ct]], [rHGt[sb]])
                    b.dma('sp', hg[ct][:, 30 + st * 512:30 + (st + 1) * 512], HGt[sb], [rHGt[sb]], [Res()])

        stageX(*tiles[0])
        for i, (st, j) in enumerate(tiles):
            if i + 1 < len(tiles):
                stageX(*tiles[i + 1])
            stageYmm(st, j)
            si = sts.index(st)
            if si > 0:
                stageF(sts[si - 1], j)
            if i > 1:
                stageYpost2(*tiles[i - 2])
            stageYpost(st, j)
            if i > 0:
                stageYpostB(*tiles[i - 1])
        stageYpostB(*tiles[-1])
        if len(tiles) > 1:
            stageYpost2(*tiles[-2])
        stageYpost2(*tiles[-1])
        for g in range(4):
            stageF(sts[-1], g)
        rg_all = Res()
        b.act(self.gates, self.gates, AF.Sigmoid, self.r_g, [rg_all])
        for t in range(NT):
            self.r_g[t] = rg_all
        self.markA = mark


INPUT_SPECS = [
    ('x', [S, D], F32), ('pos_t', [128, 32], I32), ('posc_t', [128, 2], I32),
    ('gin_t', [2, 128, 8], F32), ('gffn_t', [2, 128, 8], F32),
    ('w_in', [2, D, WIN], F32), ('w_out', [2, D, D], F32),
    ('q_norm_g', [2, 64], F32), ('k_norm_g', [2, 3, 64], F32),
    ('cmp_pos_k_t', [2, 128, 32], F32), ('cmp_w1_k', [2, 2048, 128], F32), ('cmp_w2_k', [2, 128, 64], F32),
    ('cmp_pos_v_t', [2, 128, 32], F32), ('cmp_w1_v', [2, 2048, 128], F32), ('cmp_w2_v', [2, 128, 64], F32),
    ('conv_w_t', [2, 128, 4, 31], F32), ('conv_b_t', [2, 128, 4], F32),
    ('conv_lng_t', [2, 128, 4], F32), ('conv_lnb_t', [2, 128, 4], F32),
    ('ffn_w_gate', [D, DFF], F32), ('ffn_w_up', [D, DFF], F32), ('ffn_w_down', [DFF, D], F32),
    ('moe_router', [D, NE], F32), ('moe_w_gate', [NE, D, DFF], F32), ('moe_w_up', [NE, D, DFF], F32),
    ('moe_w_down', [NE, DFF, D], F32),
]


def build_program(dbg=None):
    from contextlib import ExitStack
    nc = bass.Bass('TRN2', target_bir_lowering=False)
    dbg = dbg or {}
    with ExitStack() as stack:
        stack.enter_context(nc.allow_low_precision('bf16 matmul operands, fp32 accumulation'))
        stack.enter_context(nc.allow_non_contiguous_dma('small parameter loads'))
        B = Builder(nc, stack, dbg)
        use = dbg.get('inputs')
        inp = {}
        for name, shape, dt in INPUT_SPECS:
            if use is None or name in use:
                inp[name] = B.din(name, shape, dt)
        out = B.dout('out', [S, D], F32)
        B.hg_scr = [B.dscr('hg%d' % c, [128, S + 30], BF16) for c in range(4)]
        B.conv_scr = [B.dscr('cv%d' % c, [128, S], BF16) for c in range(4)]
        B.kc_scr = B.dscr('kc_scr', [128, S], BF16)
        B.vc_scr = B.dscr('vc_scr', [128, S], BF16)
        B.phase0(inp)
        B.build_selmap()
        if dbg.get('stage', 'full') == 'full':
            nl = dbg.get('layers', 2)
            B.final_out = out
            sparse = dbg.get('sparse', True)
            if sparse:
                B.setup_weight_conversion_sparse(inp)
            else:
                B.setup_weight_conversion(inp)
            xmid = [B.dscr('xmid%d' % i, [S, D], F32) for i in range(2)]
            xl1 = B.dscr('xl1', [S, D], F32)
            for l in range(nl):
                A = B.arena
                base = A.off
                xin = inp['x'] if l == 0 else xl1
                xout = out if l == nl - 1 else xl1
                P = B.load_layer_small(inp, l)
                markP = A.off
                B.phaseA(inp, l, xin, P)
                B.sc.barrier()
                A.off = B.markA
                B.phase_cmp(inp, l, P)
                A.off = B.markKc
                B.phase_conv(inp, l)
                B.phaseB(inp, l, xin, xmid[l])
                A.off = markP
                if l == 1:
                    B.bg(10000)
                    B.sc.barrier()
                if l == 1 and sparse:
                    B.phaseC_sparse(inp, l, xmid[l], xout, P)
                else:
                    B.phaseC(inp, l, xmid[l], xout, l == 1, P)
                A.off = base
        if dbg.get('stage') == 'B':
            P = B.load_layer_small(inp, 0)
            B.phaseA(inp, 0, inp['x'], P)
            B.sc.barrier()
            B.arena.off = B.markA
            B.phase_cmp(inp, 0, P)
            B.arena.off = B.markKc
            B.phase_conv(inp, 0)
            B.final_out = out
            dbg_attn = B.dout('dbg_attn', [S, 512], BF16)
            B.phaseB(inp, 0, inp['x'], out, qt_list=dbg.get('qt_list'), dbg_attn=dbg_attn)
        if dbg.get('stage') == 'conv':
            P = B.load_layer_small(inp, 0)
            B.phaseA(inp, 0, inp['x'], P)
            B.sc.barrier()
            B.arena.off = B.markKc
            B.phase_conv(inp, 0)
            cvo = B.dout('dbg_cv', [4, 128, S], BF16)
            for c in range(4):
                B.sc.dma('sp', lambda e, c=c: e.dma_start(out=cvo[c], in_=B.conv_scr[c]), [], [Res()], final=True)
        if dbg.get('stage') == 'cmp':
            P = B.load_layer_small(inp, 0)
            B.phaseA(inp, 0, inp['x'], P)
            B.sc.barrier()
            B.arena.off = B.markA
            B.phase_cmp(inp, 0, P)
            B.dump('KcmpT', B.KcmpT, [], [128, 256], BF16)
            B.dump('VcA', B.VcA, [], [128, 2, 2, 128], BF16)
        if dbg.get('stage') == 'A':
            P = B.load_layer_small(inp, 0)
            B.phaseA(inp, 0, inp['x'], P, st_list=dbg.get('st_list'))
            B.sc.barrier()
            B.dump('QT', B.QT, [], [128, 4, S], BF16)
            B.dump('KE0', B.KE[0], [], [128, S], BF16)
            B.dump('KE1', B.KE[1], [], [128, S], BF16)
            B.dump('KwT', B.KwT, [], [128, S], BF16)
            B.dump('VsA', B.VsA, [], [128, NT, 2, 65], BF16)
            B.dump('gates', B.gates, [], [128, NT, 24], F32)
            B.dump('cosT', B.cosT, [], [128, 34, 8], F32)
            B.dump('sinT', B.sinT, [], [128, 34, 8], F32)
            hgo = B.dout('dbg_hg', [4, 128, S + 30], BF16)
            B.dbg_out['dbg_hg'] = None
            for c in range(4):
                B.sc.dma('sp', lambda e, c=c: e.dma_start(out=hgo[c], in_=B.hg_scr[c]), [], [Res()], final=True)
        B.sc.emit(nc, stack)
    return nc, B


def host_inputs(inputs, bidx):
    pos = np.asarray(inputs['positions'][bidx])
    cmp_end = np.arange(255) * 16 + 31
    posc = np.zeros(256, np.int32)
    posc[:255] = pos[cmp_end]
    d = {
        'x': np.ascontiguousarray(inputs['x'][bidx]),
        'pos_t': np.ascontiguousarray(pos.reshape(32, 128).T),
        'posc_t': np.ascontiguousarray(posc.reshape(2, 128).T),
        'gin_t': np.ascontiguousarray(np.asarray(inputs['attn_norm_g']).reshape(2, 8, 128).transpose(0, 2, 1)),
        'gffn_t': np.ascontiguousarray(np.asarray(inputs['ffn_norm_g']).reshape(2, 8, 128).transpose(0, 2, 1)),
        'conv_w_t': np.ascontiguousarray(np.asarray(inputs['conv_w']).reshape(2, 31, 4, 128).transpose(0, 3, 2, 1)),
        'conv_b_t': np.ascontiguousarray(np.asarray(inputs['conv_b']).reshape(2, 4, 128).transpose(0, 2, 1)),
        'conv_lng_t': np.ascontiguousarray(np.asarray(inputs['conv_ln_g']).reshape(2, 4, 128).transpose(0, 2, 1)),
        'conv_lnb_t': np.ascontiguousarray(np.asarray(inputs['conv_ln_b']).reshape(2, 4, 128).transpose(0, 2, 1)),
        'ffn_w_gate': np.asarray(inputs['ffn_w_gate'])[0], 'ffn_w_up': np.asarray(inputs['ffn_w_up'])[0],
        'ffn_w_down': np.asarray(inputs['ffn_w_down'])[0],
        'moe_router': np.asarray(inputs['moe_router'])[0], 'moe_w_gate': np.asarray(inputs['moe_w_gate'])[0],
        'moe_w_up': np.asarray(inputs['moe_w_up'])[0], 'moe_w_down': np.asarray(inputs['moe_w_down'])[0],
    }
    for nm in ('cmp_pos_k', 'cmp_pos_v'):
        t = np.asarray(inputs[nm]).transpose(0, 2, 1)
        d[nm + '_t'] = np.ascontiguousarray(np.concatenate([t, t], axis=1))
    for k in ('w_in', 'w_out', 'q_norm_g', 'k_norm_g', 'cmp_w1_k', 'cmp_w2_k', 'cmp_w1_v', 'cmp_w2_v'):
        d[k] = np.asarray(inputs[k])
    return d


def _phase_cmp(self, inp, l, P):
    b, A, sc = self, self.arena, self.sc
    ps, pr = self.psb, self.psr
    self.r_kcmp = Res()
    self.r_vca = Res()
    mark = A.off
    for nt in range(2):
        for kh in range(2):
            b.cp('pool', self.VcA[:, nt, kh, 64:128], self.selmap[:, nt, :], [self.r_selmap], [self.r_vca])
    ident_b = self.ident_b
    sc.serialize_dma = bool(self.dbg.get('ser'))
    KVT = {}
    for which in ('k', 'v'):
        KVT[which] = A.alloc([S], BF16)
        rk = Res()
        b.dma('sp', KVT[which], self.kc_scr if which == 'k' else self.vc_scr, [], [rk])
        KVT[which + 'r'] = [rk]
    for which in ('k', 'v'):
        if self.dbg.get('cmp_stop', 99) <= 0:
            continue
        src = KVT[which]
        rsrc = KVT[which + 'r']
        W1 = A.alloc([32, 128], BF16)
        rW1 = Res()
        w1src = inp['cmp_w1_' + which][l].rearrange('(l d) h -> d l h', d=64)
        skip = self.dbg.get('cmp_skip', ())
        if 'w1' not in skip:
            b.dma('pool', W1[0:64], w1src, [], [rW1], nd=2048)
            b.dma('pool', W1[64:128], w1src, [], [rW1], nd=2048)
        posT = A.alloc([32], BF16)
        if 'pos' not in skip:
            b.dma('pool', posT, inp['cmp_pos_%s_t' % which][l], [], [rW1])
        W2 = A.alloc([64], BF16)
        if 'w2' not in skip:
            b.dma('pool', W2, inp['cmp_w2_' + which][l], [], [rW1])
        PH, PBI, PO, PTR = 0, 1, 2, 3
        PHS = (0, 4)
        stop = self.dbg.get('cmp_stop', 99)
        if stop <= 1:
            continue
        for kh in range(2):
            p0, p1 = kh * 64, (kh + 1) * 64
            for li in range(32):
                b.mm(ps[PHS[kh]][:, 0:255], W1[p0:p1, li, :], src[p0:p1, li:li + 4065:16],
                     li == 0, li == 31, [rW1] + rsrc, [pr[PHS[kh]]])
        for li in range(32):
            b.mm(ps[PBI][:, 0:1], W1[0:64, li, :], posT[0:64, li:li + 1], li == 0, li == 31, [rW1], [pr[PBI]])
        if stop <= 2:
            continue
        bias = A.alloc([1], F32)
        rb = Res()
        b.cp('dve', bias, ps[PBI][:, 0:1], [pr[PBI]], [rb])
        X = A.alloc([512], F32)
        X2 = A.alloc([512], F32)
        X3 = A.alloc([512], F32)
        HID = A.alloc([512], BF16)
        rX, rX2, rX3, rH = Res(), Res(), Res(), Res()
        for kh in range(2):
            b.ts('dve', X[:, kh * 256:(kh + 1) * 256], ps[PHS[kh]][:, 0:256], bias, None, ALU.add, None,
                 [pr[PHS[kh]], rb], [rX])
        b.tt('dve', X2, X, X, ALU.mult, [rX], [rX2])
        b.ts('dve', X3, X2, 0.044715, 1.0, ALU.mult, ALU.add, [rX2], [rX3])
        b.tt('dve', X2, X3, X, ALU.mult, [rX3, rX], [rX2])
        b.act(X3, X2, AF.Tanh, [rX2], [rX3], scale=math.sqrt(2.0 / math.pi))
        b.ts('dve', X2, X3, 1.0, 0.5, ALU.add, ALU.mult, [rX3], [rX2])
        b.tt('dve', HID, X2, X, ALU.mult, [rX2, rX], [rH])
        if stop <= 3:
            continue
        pso = ps[PO][:, 0:256].rearrange('p (a k d) -> p a k d', a=2, k=2)
        for nt in range(2):
            nn = 128 if nt == 0 else 127
            for kh in range(2):
                b.mm(pso[0:nn, nt, kh, :], HID[:, kh * 256 + nt * 128:kh * 256 + nt * 128 + nn], W2, True, True,
                     [rH, rW1], [pr[PO]])
        if stop <= 4:
            continue
        if which == 'v':
            for nt in range(2):
                nn = 128 if nt == 0 else 127
                b.cp('dve', self.VcA[0:nn, nt, :, 0:64], pso[0:nn, nt, :, :], [pr[PO]], [self.r_vca])
        else:
            SQ = A.alloc([4, 64], F32)
            s4 = A.alloc([4], F32)
            d4 = A.alloc([4], F32)
            r4 = A.alloc([4], F32)
            T1 = A.alloc([4, 64], F32)
            T2 = A.alloc([4, 64], F32)
            RA = A.alloc([4, 4, 8], F32)
            KB = A.alloc([2, 2, 64], BF16)
            rq, rs4, rd4, rr4, rT1, rT2, rRA, rKB = (Res() for _ in range(8))
            psf = ps[PO][:, 0:256].rearrange('p (h d) -> p h d', d=64)
            b.memset('pool', SQ, 1.0, [rq])
            b.memset('pool', T1, 0.0, [rT1])
            for nt in range(2):
                nn = 128 if nt == 0 else 127
                h0 = nt * 2
                b.act(SQ[0:nn, h0:h0 + 2, :], psf[0:nn, h0:h0 + 2, :], AF.Square, [pr[PO]], [rq])
            sc.op('dve', lambda e: e.tensor_reduce(s4, SQ, AX.X, ALU.add), [rq], [rs4])
            b.act(d4, s4, AF.Sqrt, [rs4], [rd4], scale=1.0 / 64, bias=EPS)
            sc.op('dve', lambda e: e.reciprocal(r4, d4), [rd4], [rr4])
            for nt in range(2):
                nn = 128 if nt == 0 else 127
                h0 = nt * 2
                b.tt('dve', T1[0:nn, h0:h0 + 2, :], psf[0:nn, h0:h0 + 2, :],
                     r4[0:nn, h0:h0 + 2].unsqueeze(2).to_broadcast([nn, 2, 64]), ALU.mult, [pr[PO], rr4], [rT1])
            b.tt('dve', T2, T1, P['gk'][:, 0:1, :].to_broadcast([128, 4, 64]), ALU.mult, [rT1, P['r']], [rT2])
            T2v = T2.rearrange('p (a k) d -> p a k d', a=2)
            RAv = RA.rearrange('p r (a k) d -> p r a k d', a=2)
            KBv = KB
            cosb = self.cosT[:, 32:34, :].unsqueeze(2).to_broadcast([128, 2, 2, 8])
            sinb = self.sinT[:, 32:34, :].unsqueeze(2).to_broadcast([128, 2, 2, 8])
            x1 = T2v[:, :, :, 0:8]
            x2 = T2v[:, :, :, 8:16]
            b.tt('dve', RAv[:, 0], x1, cosb, ALU.mult, [rT2, self.r_rope], [rRA])
            b.tt('dve', RAv[:, 1], x2, sinb, ALU.mult, [rT2, self.r_rope], [rRA])
            b.tt('dve', RAv[:, 2], x2, cosb, ALU.mult, [rT2, self.r_rope], [rRA])
            b.tt('dve', RAv[:, 3], x1, sinb, ALU.mult, [rT2, self.r_rope], [rRA])
            b.tt('dve', KBv[:, :, :, 0:8], RAv[:, 0], RAv[:, 1], ALU.subtract, [rRA], [rKB])
            b.tt('dve', KBv[:, :, :, 8:16], RAv[:, 2], RAv[:, 3], ALU.add, [rRA], [rKB])
            b.cp('dve', KBv[:, :, :, 16:64], T2v[:, :, :, 16:64], [rT2], [rKB])
            psq = self.psum_bf(PTR).rearrange('p (k t) -> p k t', t=128)
            for nt in range(2):
                b.tr(psq[:, nt, :], KB[:, nt].rearrange('p k d -> p (k d)'), ident_b, [rKB, self.r_ident], [pr[PTR]])
            b.cp('dve', self.KcmpT.rearrange('p (a n) -> p a n', a=2), psq[:, 0:2, :], [pr[PTR]], [self.r_kcmp])
    sc.barrier()
    A.off = mark


Builder.phase_cmp = _phase_cmp


def _build_selmap(self):
    b, A, sc = self, self.arena, self.sc
    self.selmap = A.alloc([2, 64], F32)
    self.r_selmap = Res()
    mark = A.off
    ones = A.alloc([64], F32)
    half = A.alloc([64], F32)
    t1 = A.alloc([2, 64], F32)
    t2 = A.alloc([2, 64], F32)
    t3 = A.alloc([2, 64], F32)
    r0, r1, r2, r3 = Res(), Res(), Res(), Res()
    b.memset('pool', ones, 1.0, [r0])
    b.memset('pool', half, 0.5, [r0])
    for nt in range(2):
        base = 128 * nt
        o1, o2, o3 = t1[:, nt, :], t2[:, nt, :], t3[:, nt, :]
        sc.op('pool', lambda e, o=o1, base=base: e.affine_select(out=o, in_=ones, pattern=[[-4, 64]],
              compare_op=ALU.is_ge, fill=0.0 if REGS is None else REGS['zero'], base=base, channel_multiplier=1), [r0], [r1])
        sc.op('pool', lambda e, o=o1, base=base: e.affine_select(out=o, in_=o, pattern=[[4, 64]],
              compare_op=ALU.is_ge, fill=0.0 if REGS is None else REGS['zero'], base=3 - base, channel_multiplier=-1), [r1], [r1])
        sc.op('pool', lambda e, o=o2, base=base: e.affine_select(out=o, in_=half, pattern=[[-4, 64]],
              compare_op=ALU.is_equal, fill=0.0 if REGS is None else REGS['zero'], base=base - 3, channel_multiplier=1), [r0], [r2])
        sc.op('pool', lambda e, o=o3, base=base: e.affine_select(out=o, in_=half, pattern=[[-4, 64]],
              compare_op=ALU.is_equal, fill=0.0 if REGS is None else REGS['zero'], base=base + 1, channel_multiplier=1), [r0], [r3])
    b.tt('pool', t1, t1, t2, ALU.subtract, [r1, r2], [r1])
    b.tt('pool', self.selmap, t1, t3, ALU.add, [r1, r3], [self.r_selmap])
    sc.barrier()
    A.off = mark


Builder.build_selmap = _build_selmap


def _phase_conv(self, inp, l):
    b, A, sc = self, self.arena, self.sc
    ps, pr = self.psb, self.psr
    mark = A.off
    cw = A.alloc([4, 31], F32)
    cb = A.alloc([4], F32)
    lg = A.alloc([4], F32)
    lb = A.alloc([4], F32)
    rp = Res()
    b.dma('sp', cw, inp['conv_w_t'][l], [], [rp])
    b.dma('sp', cb, inp['conv_b_t'][l], [], [rp])
    b.dma('sp', lg, inp['conv_lng_t'][l], [], [rp])
    b.dma('sp', lb, inp['conv_lnb_t'][l], [], [rp])
    DG = A.alloc([4, 31, 128], BF16)
    rDG = [Res() for _ in range(4)]
    i = 0
    for ct in range(4):
        for j in range(31):
            eng = 'dve'
            i += 1
            b.ts(eng, DG[:, ct, j, :], self.ident_b, cw[:, ct, j:j + 1], None, ALU.mult, None,
                 [self.r_ident, rp], [rDG[ct]])
    HG = A.alloc([4, S + 30], BF16)
    rHG = [Res() for _ in range(4)]
    for ct in range(4):
        b.dma('sp', HG[:, ct, :], self.hg_scr[ct], [], [rHG[ct]])
    onesm = A.alloc([128], F32)
    rones = Res()
    b.memset('pool', onesm, 1.0 / 512.0, [rones])
    YS = [A.alloc([512], F32) for _ in range(4)]
    YQ = [A.alloc([512], F32) for _ in range(2)] * 2
    rYS = [Res() for _ in range(4)]
    rYQ = [Res() for _ in range(2)] * 2
    MEAN = A.alloc([512], F32)
    MSQ = A.alloc([512], F32)
    VAR = A.alloc([512], F32)
    RSTD = A.alloc([512], F32)
    rM, rMS, rV, rR = Res(), Res(), Res(), Res()
    Z = [VAR] * 2
    rZ = [rV] * 2
    CT = [A.alloc([512], BF16)] * 2
    rCT = [Res()] * 2
    zi = 0
    for tc in range(NST):
        for ct in range(4):
            for j in range(31):
                b.mm(ps[ct][:, :], DG[:, ct, j, :], HG[:, ct, tc * 512 + j:tc * 512 + j + 512], j == 0, j == 30,
                     [rDG[ct], rHG[ct]], [pr[ct]])
        for ct in range(4):
            b.act(YS[ct], ps[ct][:, :], AF.Identity, [pr[ct], rp], [rYS[ct]], bias=cb[:, ct:ct + 1])
            b.act(YQ[ct], ps[ct][:, :], AF.Square, [pr[ct], rp], [rYQ[ct]], bias=cb[:, ct:ct + 1])
            b.mm(ps[5][:, :], onesm, YQ[ct], ct == 0, ct == 3, [rones, rYQ[ct]], [pr[5]])
        for ct in range(4):
            b.mm(ps[4][:, :], onesm, YS[ct], ct == 0, ct == 3, [rones, rYS[ct]], [pr[4]])
        b.cp('act', MEAN, ps[4][:, :], [pr[4]], [rM])
        b.tt('dve', MSQ, ps[4][:, :], MEAN, ALU.mult, [pr[4], rM], [rMS])
        b.tt('dve', VAR, ps[5][:, :], MSQ, ALU.subtract, [pr[5], rMS], [rV])
        b.act(MSQ, VAR, AF.Sqrt, [rV], [rMS], bias=EPS)
        sc.op('dve', lambda e: e.reciprocal(RSTD, MSQ), [rMS], [rR])
        for ct in range(4):
            zb = zi % 2
            zi += 1
            b.tt('dve', Z[zb], YS[ct], MEAN, ALU.subtract, [rYS[ct], rM], [rZ[zb]])
            b.tt('dve', Z[zb], Z[zb], RSTD, ALU.mult, [rZ[zb], rR], [rZ[zb]])
            b.act(CT[zb], Z[zb], AF.Silu, [rZ[zb], rp], [rCT[zb]], scale=lg[:, ct:ct + 1], bias=lb[:, ct:ct + 1])
            b.dma('sp', self.conv_scr[ct][:, tc * 512:(tc + 1) * 512], CT[zb], [rCT[zb]], [Res()])
    sc.barrier()
    A.off = mark


Builder.phase_conv = _phase_conv


def _phaseB(self, inp, l, xsrc, xdst, qt_list=None, dbg_attn=None):
    b, A, sc = self, self.arena, self.sc
    ps, pr = self.psb, self.psr
    mark = A.off
    ident_b = self.ident_b
    Wout = A.alloc([8, D], BF16)
    rWo = Res()
    wsrc = inp['w_out'][l].rearrange('(m p) d -> p m d', p=128)
    for h in range(2):
        b.dma('pool', Wout[:, :, h * 512:(h + 1) * 512], wsrc[:, :, h * 512:(h + 1) * 512], [], [rWo], nd=1024)
    NB = 2
    R = [[A.alloc([4, 128], BF16) for _ in range(2)] for _ in range(NB)]
    RZ = [[A.alloc([4, 128], BF16) for _ in range(2)] for _ in range(NB)]
    rRq = [[Res() for _ in range(2)] for _ in range(NB)]
    rRm = [[Res() for _ in range(2)] for _ in range(NB)]
    rRZ = [[Res() for _ in range(2)] for _ in range(NB)]
    for i in range(NB):
        b.memset('pool', RZ[i][0][64:128], 0.0, [rRZ[i][0]])
        b.memset('pool', RZ[i][1][0:64], 0.0, [rRZ[i][1]])
    NP = 6
    PT_ = [A.alloc([512], BF16) for _ in range(NP)]
    rPT = [Res() for _ in range(NP)]
    cmask = [[A.alloc([4, 128], BF16) for _ in range(2)] for _ in range(NB)]
    rcm = [[Res() for _ in range(2)] for _ in range(NB)]
    XT = [A.alloc([D], F32) for _ in range(3)]
    rXT = [Res() for _ in range(3)]
    CV = [A.alloc([4, 128], BF16) for _ in range(3)]
    rCV = [Res() for _ in range(3)]
    ATT = A.alloc([512], BF16)
    rATT = Res()
    ATT_T = A.alloc([4, 128], BF16)
    rATT_T = Res()
    MM_ = A.alloc([128], BF16)
    rMM = Res()
    ACC = [[A.alloc([4, 64], F32) for _ in range(2)] for _ in range(NB)]
    r_acc = [[Res() for _ in range(2)] for _ in range(NB)]
    rsumC = [[A.alloc([4], F32) for _ in range(2)] for _ in range(NB)]
    rinvC = [[A.alloc([4], F32) for _ in range(2)] for _ in range(NB)]
    r_rsumC = [[Res() for _ in range(2)] for _ in range(NB)]
    r_rinvC = [[Res() for _ in range(2)] for _ in range(NB)]
    rsum2 = [A.alloc([2, 4], F32) for _ in range(2)]
    rinv = [A.alloc([3, 4], F32) for _ in range(2)]
    coef = [A.alloc([3, 4], F32) for _ in range(2)]
    r_rsum2 = [Res() for _ in range(2)]
    r_rinv = [Res() for _ in range(2)]
    r_coef = [Res() for _ in range(2)]
    IMP = [A.alloc([64], F32) for _ in range(2)]
    IMPM = [A.alloc([64], F32) for _ in range(2)]
    IMP2 = [A.alloc([64], F32) for _ in range(2)]
    M8 = [A.alloc([16], F32) for _ in range(2)]
    SELC = [A.alloc([64], F32) for _ in range(2)]
    FRC = [A.alloc([64], F32) for _ in range(2)]
    r_imp = [Res() for _ in range(2)]
    r_impm = [Res() for _ in range(2)]
    r_imp2 = [Res() for _ in range(2)]
    r_m8 = [Res() for _ in range(2)]
    r_selc = [Res() for _ in range(2)]
    r_frc = [Res() for _ in range(2)]
    TMP = [A.alloc([4, 64], F32) for _ in range(2)]
    r_tmp = [Res() for _ in range(2)]
    ST = (0, 1)
    OC, OS, OW, TRP, OUT0, OUT1 = 2, 3, 4, 5, 6, 7
    psOC = ps[OC][:, 0:512].rearrange('p (g c) -> p g c', g=4)
    psOST = ps[OS][0:65, :]
    psOWT = ps[OW][0:65, :]
    psOS = ps[OUT0][:, 0:260].rearrange('p (g c) -> p g c', g=4)
    psOW = ps[OUT1][:, 0:260].rearrange('p (g c) -> p g c', g=4)
    OTs = [A.alloc([512], F32) for _ in range(2)]
    rOTs = [Res() for _ in range(2)]
    psTR = self.psum_bf(TRP).rearrange('p (k t) -> p k t', t=128)
    gates_v = self.gates.rearrange('p t (h r) -> p t h r', r=3)
    state = {'sti': 0, 'pti': 0, 'pend': None}

    def flush():
        if state['pend'] is not None:
            state['pend']()
            state['pend'] = None

    def tile_step(lhsT, rhs, rl, rr, nn, post_mask, rmask, acc_view, vrhs, rv, first, width, accbank,
                  transposed=False):
        sb = ST[state['sti'] % 2]
        state['sti'] += 1
        pb = state['pti'] % NP
        state['pti'] += 1
        if post_mask is None:
            b.mm(ps[sb][0:nn, :], lhsT, rhs, True, True, rl + rr, [pr[sb]])
        else:
            b.mm(ps[sb][0:nn, :], lhsT, rhs, True, False, rl + rr, [pr[sb]])
            b.mm(ps[sb][0:nn, :], ident_b[0:nn, 0:nn], post_mask[0:nn].rearrange('p g q -> p (g q)'), False, True,
                 [self.r_ident] + rmask, [pr[sb]])
        b.act(PT_[pb][0:nn, :], ps[sb][0:nn, :], AF.Exp, [pr[sb]], [rPT[pb]])
        flush()

        def pv():
            if transposed:
                b.mm(acc_view, vrhs, PT_[pb][0:nn, :], first, False, [rPT[pb]] + rv, [pr[accbank]])
                return
            for g in range(4):
                b.mm(acc_view[:, g, 0:width], PT_[pb][0:nn, g * 128:(g + 1) * 128], vrhs, first and g == 0, False,
                     [rPT[pb]] + rv, [pr[accbank]])
        state['pend'] = pv

    def s1pre(qi, qt):
        t0 = qt * 128
        rb_ = qi % NB
        xb = qi % 3
        tsl = slice(t0, t0 + 128)
        b.bg(2)
        b.dma('sp', XT[xb], xsrc[t0:t0 + 128, :], [], [rXT[xb]])
        for ct in range(4):
            b.dma('sp', CV[xb][:, ct, :], self.conv_scr[ct][:, tsl], [], [rCV[xb]])
        b.cp('pool', R[rb_][0][0:64], self.QT[0:64, :, tsl], [self.r_q[qt]], [rRq[rb_][0]])
        b.cp('pool', R[rb_][1][64:128], self.QT[64:128, :, tsl], [self.r_q[qt]], [rRq[rb_][1]])
        b.cp('pool', RZ[rb_][0][0:64], self.QT[0:64, :, tsl], [self.r_q[qt]], [rRZ[rb_][0]])
        b.cp('pool', RZ[rb_][1][64:128], self.QT[64:128, :, tsl], [self.r_q[qt]], [rRZ[rb_][1]])

    def s1a(qi, qt, kh):
        t0 = qt * 128
        rb_ = qi % NB
        nmax = 8 * qt + 6
        ntiles = [0] if nmax < 128 else [0, 1]
        if True:
            rz = RZ[rb_][kh].rearrange('p g q -> p (g q)')
            for ni, nt_ in enumerate(ntiles):
                nn = 128 if nt_ == 0 else 127
                nbase = 128 * nt_
                full = (16 * (nbase + nn - 1) + 31) <= t0
                pm, rmk = None, []
                if not full:
                    cm_ = cmask[rb_][nt_]
                    if kh == 0:
                        zb4 = self.zeros_b4
                        sc.op('pool', lambda e, cm_=cm_, base=t0 - 16 * nbase - 31, zb4=zb4: e.affine_select(
                            out=cm_, in_=zb4, pattern=[[0, 4], [1, 128]], compare_op=ALU.is_ge,
                            fill=NEG if REGS is None else REGS['neg'], base=base,
                            channel_multiplier=-16), [self.r_masks], [rcm[rb_][nt_]])
                    pm, rmk = cm_, [rcm[rb_][nt_]]
                tile_step(self.KcmpT[:, nbase:nbase + nn], rz, [self.r_kcmp], [rRZ[rb_][kh]], nn, pm, rmk,
                          psOC, self.VcA[0:nn, nt_, kh, :], [self.r_vca], ni == 0, 128, OC)
            flush()
            sc.op('dve', lambda e, o=rsumC[rb_][kh], i=psOC[:, :, 64:128]: e.tensor_reduce(o, i, AX.X, ALU.add),
                  [pr[OC]], [r_rsumC[rb_][kh]])
            b.ts('dve', rinvC[rb_][kh], rsumC[rb_][kh], 1e-30, None, ALU.max, None, [r_rsumC[rb_][kh]],
                 [r_rinvC[rb_][kh]])
            sc.op('dve', lambda e, o=rinvC[rb_][kh]: e.reciprocal(o, o), [r_rinvC[rb_][kh]], [r_rinvC[rb_][kh]])
            b.cp('dve', ACC[rb_][kh], psOC[:, :, 0:64], [pr[OC]], [r_acc[rb_][kh]])

    def s1topk(qi, qt, kh):
        rb_ = qi % NB
        if True:
            mdst = MM_[:, 64:128] if kh == 0 else MM_[:, 0:64]
            if qt >= 8:
                b.ts('dve', IMP[kh], psOC[:, 0, 64:128], rinvC[rb_][kh][:, 0:1], None, ALU.mult, None,
                     [pr[OC], r_rinvC[rb_][kh]], [r_imp[kh]])
                for g in range(1, 4):
                    b.stt(IMP[kh], psOC[:, g, 64:128], rinvC[rb_][kh][:, g:g + 1], IMP[kh], ALU.mult, ALU.add,
                          [pr[OC], r_rinvC[rb_][kh], r_imp[kh]], [r_imp[kh]])
                for half in range(2):
                    cur = 2 * qt + half
                    hs = slice(half * 64, half * 64 + 64)
                    sc.op('pool', lambda e, o=IMPM[kh][hs, :], i=IMP[kh][hs, :], base=cur - 2: e.affine_select(
                        out=o, in_=i, pattern=[[-1, 64]], compare_op=ALU.is_ge,
                        fill=NEG if REGS is None else REGS['neg'], base=base,
                        channel_multiplier=0), [r_imp[kh]], [r_impm[kh]])
                b.memset('pool', IMPM[kh][:, 0:1], NEG, [r_impm[kh]])
                sc.op('dve', lambda e, o=M8[kh][:, 0:8], i=IMPM[kh]: e.max(o, i), [r_impm[kh]], [r_m8[kh]])
                sc.op('dve', lambda e, o=IMP2[kh], a=M8[kh][:, 0:8], v=IMPM[kh]: e.match_replace(o, a, v, NEG),
                      [r_impm[kh], r_m8[kh]], [r_imp2[kh]])
                sc.op('dve', lambda e, o=M8[kh][:, 8:16], i=IMP2[kh]: e.max(o, i), [r_imp2[kh]], [r_m8[kh]])
                b.ts('dve', SELC[kh], IMPM[kh], M8[kh][:, 12:13], None, ALU.is_ge, None, [r_impm[kh], r_m8[kh]],
                     [r_selc[kh]])
                b.memset('pool', FRC[kh], 0.0, [r_frc[kh]])
                b.memset('pool', FRC[kh][:, 0:1], 1.0, [r_frc[kh]])
                b.memset('pool', FRC[kh][0:64, 2 * qt - 1:2 * qt + 1], 1.0, [r_frc[kh]])
                b.memset('pool', FRC[kh][64:128, 2 * qt:2 * qt + 2], 1.0, [r_frc[kh]])
                b.tt('dve', mdst, SELC[kh], FRC[kh], ALU.max, [r_selc[kh], r_frc[kh]], [rMM])
            else:
                b.memset('pool', mdst, 0.0, [rMM])
                b.memset('pool', mdst[0:64, 0:2 * qt + 1], 1.0, [rMM])
                b.memset('pool', mdst[64:128, 0:2 * qt + 2], 1.0, [rMM])
    def s1b(qi, qt):
        rb_ = qi % NB
        b.tr(psTR[:, 0, :], MM_, ident_b, [rMM, self.r_ident], [pr[TRP]])
        b.ts('dve', R[rb_][0][64:128], psTR[64:128, 0:1, :].to_broadcast([64, 4, 128]), -1.0, -NEG, ALU.add,
             ALU.mult, [pr[TRP]], [rRm[rb_][0]])
        b.ts('dve', R[rb_][1][0:64], psTR[0:64, 0:1, :].to_broadcast([64, 4, 128]), -1.0, -NEG, ALU.add,
             ALU.mult, [pr[TRP]], [rRm[rb_][1]])

    def s2win(qi, qt, kh):
        rb_ = qi % NB
        if True:
            rz = RZ[rb_][kh].rearrange('p g q -> p (g q)')
            kts = list(range(max(0, qt - 4), qt + 1))
            for i, kt in enumerate(kts):
                if kt == qt:
                    pm, rmk = self.causalN, [self.r_masks]
                elif kt == qt - 4:
                    pm, rmk = self.lowN, [self.r_masks]
                else:
                    pm, rmk = None, []
                tile_step(self.KwT[:, kt * 128:(kt + 1) * 128], rz, [self.r_kw[kt]], [rRZ[rb_][kh]], 128, pm,
                          rmk, psOWT, self.VwA[:, kt, kh, :], [self.r_vw[kt]], i == 0, 65, OW, transposed=True)

    def s2sel(qi, qt, kh):
        rb_ = qi % NB
        if True:
            rr = R[rb_][kh].rearrange('p g q -> p (g q)')
            for kt in range(qt + 1):
                pm, rmk = (self.causalN, [self.r_masks]) if kt == qt else (None, [])
                tile_step(self.KE[kh][:, kt * 128:(kt + 1) * 128], rr, [self.r_ks[kt], self.r_E],
                          [rRq[rb_][kh], rRm[rb_][kh]], 128, pm, rmk, psOST, self.VsA[:, kt, kh, :],
                          [self.r_vs[kt]], kt == 0, 65, OS, transposed=True)
            flush()
            b.cp('act', OTs[0][0:65, :], psOWT, [pr[OW]], [rOTs[0]])
            b.cp('dve', OTs[1][0:65, :], psOST, [pr[OS]], [rOTs[1]])
            for src_i, dstv, dbank in ((0, psOW, OUT1), (1, psOS, OUT0)):
                for g in range(4):
                    b.tr(dstv[:, g, :], OTs[src_i][0:65, g * 128:(g + 1) * 128], self.ident_f[0:65, 0:65],
                         [rOTs[src_i], self.r_identf], [pr[dbank]])
            b.cp('dve', rsum2[kh][:, 0, :], psOS[:, :, 64], [pr[OUT0]], [r_rsum2[kh]])
            b.cp('dve', rsum2[kh][:, 1, :], psOW[:, :, 64], [pr[OUT1]], [r_rsum2[kh]])
            sc.op('dve', lambda e, o=rinv[kh][:, 1:3, :], i=rsum2[kh]: e.reciprocal(o, i), [r_rsum2[kh]],
                  [r_rinv[kh]])
            b.cp('dve', rinv[kh][:, 0, :], rinvC[rb_][kh], [r_rinvC[rb_][kh]], [r_rinv[kh]])
            gv = gates_v[:, qt, kh * 4:(kh + 1) * 4, :].rearrange('p h r -> p r h')
            b.tt('dve', coef[kh], rinv[kh], gv, ALU.mult, [r_rinv[kh], self.r_g[qt]], [r_coef[kh]])
            acc = ACC[rb_][kh]
            racc = r_acc[rb_][kh]
            b.tt('dve', acc, acc, coef[kh][:, 0, :].unsqueeze(2).to_broadcast([128, 4, 64]), ALU.mult,
                 [racc, r_coef[kh]], [racc])
            b.tt('dve', TMP[kh], psOS[:, :, 0:64], coef[kh][:, 1, :].unsqueeze(2).to_broadcast([128, 4, 64]),
                 ALU.mult, [pr[OUT0], r_coef[kh]], [r_tmp[kh]])
            b.tt('dve', acc, acc, TMP[kh], ALU.add, [racc, r_tmp[kh]], [racc])
            b.tt('dve', TMP[kh], psOW[:, :, 0:64], coef[kh][:, 2, :].unsqueeze(2).to_broadcast([128, 4, 64]),
                 ALU.mult, [pr[OUT1], r_coef[kh]], [r_tmp[kh]])
            b.tt('dve', ATT[:, kh * 256:(kh + 1) * 256].rearrange('p (g d) -> p g d', g=4), acc, TMP[kh],
                 ALU.add, [racc, r_tmp[kh]], [rATT])
    def s2tail(qi, qt):
        t0 = qt * 128
        xb = qi % 3
        if dbg_attn is not None:
            b.dma('sp', dbg_attn[t0:t0 + 128, :], ATT, [rATT], [Res()], final=True)
        for c in range(4):
            b.tr(psTR[:, 1 + c, :], ATT[:, c * 128:(c + 1) * 128], ident_b, [rATT, self.r_ident], [pr[TRP]])
        b.cp('dve', ATT_T, psTR[:, 1:5, :], [pr[TRP]], [rATT_T])
        for h, bank in enumerate((OUT0, OUT1)):
            for m in range(8):
                lhs = ATT_T[:, m, :] if m < 4 else CV[xb][:, m - 4, :]
                rl = [rATT_T] if m < 4 else [rCV[xb]]
                b.mm(ps[bank][:, :], lhs, Wout[:, m, h * 512:(h + 1) * 512], m == 0, m == 7, rl + [rWo], [pr[bank]])
            b.tt('dve', XT[xb][:, h * 512:(h + 1) * 512], XT[xb][:, h * 512:(h + 1) * 512], ps[bank][:, :], ALU.add,
                 [rXT[xb], pr[bank]], [rXT[xb]])
        b.dma('sp', xdst[t0:t0 + 128, :], XT[xb], [rXT[xb]], [Res()], final=(xdst is self.final_out))

    qts = list(qt_list if qt_list is not None else range(NT))
    s1pre(0, qts[0])
    for kh in range(2):
        s1a(0, qts[0], kh)
        s1topk(0, qts[0], kh)
    s1b(0, qts[0])
    for qi, qt in enumerate(qts):
        nxt = qi + 1 < len(qts)
        if nxt:
            s1pre(qi + 1, qts[qi + 1])
        for kh in range(2):
            if nxt:
                s1a(qi + 1, qts[qi + 1], kh)
            s2win(qi, qt, kh)
            if kh == 0 and qi > 0:
                s2tail(qi - 1, qts[qi - 1])
            if nxt:
                s1topk(qi + 1, qts[qi + 1], kh)
            s2sel(qi, qt, kh)
        if nxt:
            s1b(qi + 1, qts[qi + 1])
    s2tail(len(qts) - 1, qts[-1])
    sc.barrier()
    A.off = mark


Builder.phaseB = _phaseB


def _setup_weight_conversion(self, inp):
    self.wb = {}
    self.bg_tasks = []
    self.r_wb = {}
    specs = [('ffn', None)] + [('moe', e) for e in range(NE)]
    for kind, e in specs:
        key = 'd' if kind == 'ffn' else e
        for nm, shape in (('gate', [D, DFF]), ('up', [D, DFF]), ('down', [DFF, D])):
            src = inp['%s_w_%s' % (kind, nm)] if kind == 'ffn' else inp['moe_w_%s' % nm][e]
            dst = self.dscr('wb_%s_%s' % (key, nm), shape, BF16)
            self.wb[(key, nm)] = dst
            r = Res()
            self.r_wb[(key, nm)] = r
            rows = shape[0]
            step = 256 if shape[1] > 2048 else 1024
            for r0 in range(0, rows, step):
                r1 = min(rows, r0 + step)
                ndesc = (r1 - r0) * (2 if shape[1] > 2048 else 1)

                def task(dst=dst, src=src, r0=r0, r1=r1, r=r, ndesc=ndesc):
                    self.dma('pool', dst[r0:r1, :], src[r0:r1, :], [], [Res()], nd=ndesc, max_dma_last_dim=4096)
                self.bg_tasks.append(task)


def _bg(self, n=1):
    for _ in range(n):
        if self.bg_tasks:
            self.bg_tasks.pop(0)()


Builder.setup_weight_conversion = _setup_weight_conversion
Builder.bg = _bg


def _phaseC(self, inp, l, xsrc, xdst, moe, P, st_list=None):
    b, A, sc = self, self.arena, self.sc
    ps, pr = self.psb, self.psr
    mark = A.off
    ident_b, ident_f = self.ident_b, self.ident_f
    XF = [A.alloc([D], F32) for _ in range(4)]
    rXF = [Res() for _ in range(4)]
    XN = A.alloc([D], F32 if moe else BF16)
    rXN = Res()
    H2T = A.alloc([8, 512], BF16)
    rH2T = [Res() for _ in range(4)]
    ss = A.alloc([1], F32)
    sd = A.alloc([1], F32)
    rstd = A.alloc([1], F32)
    rss, rsd, rrs = Res(), Res(), Res()
    ACTT = A.alloc([NFT, 512], BF16)
    rACT = [Res() for _ in range(NFT)]
    WD = A.alloc([NFT, D], BF16)
    rWD = Res()
    NW = 2
    WG = [A.alloc([8, 512], BF16) for _ in range(NW)]
    WU = [A.alloc([8, 512], BF16) for _ in range(NW)]
    rWG = [Res() for _ in range(NW)]
    rWU = [Res() for _ in range(NW)]
    SL = [A.alloc([512], F32) for _ in range(2)]
    rSL = [Res() for _ in range(2)]
    if moe:
        H32 = A.alloc([8, 128], F32)
        rH32 = Res()
        RT = A.alloc([8, NE], F32)
        rRT = Res()
        b.dma('sp', RT, inp['moe_router'].rearrange('(k p) e -> p k e', p=128), [], [rRT])
        LG = A.alloc([NE], F32)
        M8 = A.alloc([8], F32)
        PP = A.alloc([2], F32)
        G0 = A.alloc([NE], F32)
        GATE = A.alloc([4, NE], F32)
        rLG, rM8, rPP, rG0 = Res(), Res(), Res(), Res()
        rGATE = [Res() for _ in range(4)]
    T0, T1 = 0, 1
    GU = (2, 3, 4, 5)
    DN = (6, 7)
    gui = 0
    dni = 0
    wi = 0
    gffn_b = P['gffn'].unsqueeze(2).to_broadcast([128, 8, 128])
    experts = list(range(NE)) if moe else ['d']
    if not moe:
        XP = [A.alloc([D], F32) for _ in range(2)]
        rXP = [Res() for _ in range(2)]
        XN2 = [XN, A.alloc([D], BF16)]
        rXN2 = [rXN, Res()]
        sts = list(st_list if st_list is not None else range(NST))
        psT = self.psum_bf(T0).rearrange('p (k t) -> p k t', t=128)[:, 0:8, :]

        def norm(st, j):
            tt_ = st * 4 + j
            xb = tt_ % 2
            b.dma('sp', XP[xb], xsrc[tt_ * 128:(tt_ + 1) * 128, :], [], [rXP[xb]])
            b.act(XN2[xb], XP[xb], AF.Square, [rXP[xb]], [rXN2[xb], rss], accum_out=ss)
            b.act(sd, ss, AF.Sqrt, [rss], [rsd], scale=1.0 / D, bias=EPS)
            sc.op('dve', lambda e: e.reciprocal(rstd, sd), [rsd], [rrs])
            b.act(XN2[xb], XP[xb], AF.Copy, [rXP[xb], rrs], [rXN2[xb]], scale=rstd)

        def trans(st, j):
            xb = (st * 4 + j) % 2
            for k in range(8):
                b.tr(psT[:, k, :], XN2[xb][:, k * 128:(k + 1) * 128], ident_b, [rXN2[xb], self.r_ident], [pr[T0]])
            b.tt('dve', H2T[:, :, j * 128:(j + 1) * 128], psT, gffn_b, ALU.mult, [pr[T0], P['r']], [rH2T[j]])

        for j in range(4):
            norm(sts[0], j)
            trans(sts[0], j)
        wg_d, wu_d, wd_d = self.wb[('d', 'gate')], self.wb[('d', 'up')], self.wb[('d', 'down')]
        wgv = wg_d.rearrange('(k p) f -> p k f', p=128)
        wuv = wu_d.rearrange('(k p) f -> p k f', p=128)
        for si, st in enumerate(sts):
            nxt = sts[si + 1] if si + 1 < len(sts) else None
            for j in range(4):
                tt_ = st * 4 + j
                b.dma('sp', XF[j], xsrc[tt_ * 128:(tt_ + 1) * 128, :], [], [rXF[j]])
            for c in range(7):
                wb_ = wi % NW
                wi += 1
                b.dma('sp', WG[wb_], wgv[:, :, c * 512:(c + 1) * 512], [], [rWG[wb_]], nd=1024)
                b.dma('sp', WU[wb_], wuv[:, :, c * 512:(c + 1) * 512], [], [rWU[wb_]], nd=1024)
                if c == 0:
                    b.dma('sp', WD, wd_d.rearrange('(ft p) d -> p ft d', p=128), [], [rWD], nd=3584)
                for fi in range(4):
                    ft = c * 4 + fi
                    gb, ub = GU[(gui * 2) % 4], GU[(gui * 2 + 1) % 4]
                    sl = gui % 2
                    gui += 1
                    for k in range(8):
                        b.mm(ps[gb][:, :], WG[wb_][:, k, fi * 128:(fi + 1) * 128], H2T[:, k, :], k == 0, k == 7,
                             [rWG[wb_]] + rH2T, [pr[gb]])
                    for k in range(8):
                        b.mm(ps[ub][:, :], WU[wb_][:, k, fi * 128:(fi + 1) * 128], H2T[:, k, :], k == 0, k == 7,
                             [rWU[wb_]] + rH2T, [pr[ub]])
                    b.act(SL[sl], ps[gb][:, :], AF.Silu, [pr[gb]], [rSL[sl]])
                    b.tt('dve', ACTT[:, ft, :], ps[ub][:, :], SL[sl], ALU.mult, [pr[ub], rSL[sl]], [rACT[ft]])
            for j in range(4):
                if nxt is not None:
                    norm(nxt, j)
                for h in range(2):
                    db = DN[dni % 2]
                    dni += 1
                    for ft in range(NFT):
                        b.mm(ps[db][:, :], ACTT[:, ft, j * 128:(j + 1) * 128], WD[:, ft, h * 512:(h + 1) * 512],
                             ft == 0, ft == NFT - 1, [rACT[ft], rWD], [pr[db]])
                    xs = XF[j][:, h * 512:(h + 1) * 512]
                    b.tt('dve', xs, xs, ps[db][:, :], ALU.add, [pr[db], rXF[j]], [rXF[j]])
                if nxt is not None:
                    trans(nxt, j)
                tt_ = st * 4 + j
                b.dma('sp', xdst[tt_ * 128:(tt_ + 1) * 128, :], XF[j], [rXF[j]], [Res()],
                      final=(xdst is self.final_out))
        sc.barrier()
        A.off = mark
        return
    for st in (st_list if st_list is not None else range(NST)):
        for j in range(4):
            tt_ = st * 4 + j
            b.dma('sp', XF[j], xsrc[tt_ * 128:(tt_ + 1) * 128, :], [], [rXF[j]])
            b.act(XN, XF[j], AF.Square, [rXF[j]], [rXN, rss], accum_out=ss)
            b.act(sd, ss, AF.Sqrt, [rss], [rsd], scale=1.0 / D, bias=EPS)
            sc.op('dve', lambda e: e.reciprocal(rstd, sd), [rsd], [rrs])
            b.act(XN, XF[j], AF.Copy, [rXF[j], rrs], [rXN], scale=rstd)
            if not moe:
                psT = self.psum_bf(T0).rearrange('p (k t) -> p k t', t=128)[:, 0:8, :]
                for k in range(8):
                    b.tr(psT[:, k, :], XN[:, k * 128:(k + 1) * 128], ident_b, [rXN, self.r_ident], [pr[T0]])
                b.tt('dve', H2T[:, :, j * 128:(j + 1) * 128], psT, gffn_b, ALU.mult, [pr[T0], P['r']], [rH2T[j]])
            else:
                for k in range(8):
                    bank = T0 if k < 4 else T1
                    b.tr(ps[bank][:, (k % 4) * 128:(k % 4 + 1) * 128], XN[:, k * 128:(k + 1) * 128], ident_f,
                         [rXN, self.r_identf], [pr[bank]])
                for hk, bank in ((0, T0), (1, T1)):
                    b.tt('dve', H32[:, hk * 4:(hk + 1) * 4, :], ps[bank][:, :].rearrange('p (k t) -> p k t', t=128),
                         P['gffn'][:, hk * 4:(hk + 1) * 4].unsqueeze(2).to_broadcast([128, 4, 128]), ALU.mult,
                         [pr[bank], P['r']], [rH32])
                b.cp('act', H2T[:, :, j * 128:(j + 1) * 128], H32, [rH32], [rH2T[j]])
                for k in range(8):
                    b.mm(ps[T0][:, 0:NE], H32[:, k, :], RT[:, k, :], k == 0, k == 7, [rH32, rRT], [pr[T0]])
                b.cp('dve', LG, ps[T0][:, 0:NE], [pr[T0]], [rLG])
                sc.op('dve', lambda e: e.max(M8, LG), [rLG], [rM8])
                b.tt('dve', PP[:, 1:2], M8[:, 1:2], M8[:, 0:1], ALU.subtract, [rM8], [rPP])
                b.act(PP[:, 1:2], PP[:, 1:2], AF.Sigmoid, [rPP], [rPP])
                b.ts('dve', PP[:, 0:1], PP[:, 1:2], -1.0, 1.0, ALU.mult, ALU.add, [rPP], [rPP])
                b.ts('dve', G0, LG, M8[:, 0:1], PP[:, 0:1], ALU.is_equal, ALU.mult, [rLG, rM8, rPP], [rG0])
                b.ts('dve', GATE[:, j, :], LG, M8[:, 1:2], PP[:, 1:2], ALU.is_equal, ALU.mult, [rLG, rM8, rPP],
                     [rGATE[j]])
                b.tt('dve', GATE[:, j, :], GATE[:, j, :], G0, ALU.add, [rGATE[j], rG0], [rGATE[j]])
        for ex in experts:
            wg_d, wu_d, wd_d = self.wb[(ex, 'gate')], self.wb[(ex, 'up')], self.wb[(ex, 'down')]
            wgv = wg_d.rearrange('(k p) f -> p k f', p=128)
            wuv = wu_d.rearrange('(k p) f -> p k f', p=128)
            for c in range(7):
                wb_ = wi % NW
                wi += 1
                b.dma('sp', WG[wb_], wgv[:, :, c * 512:(c + 1) * 512], [], [rWG[wb_]], nd=1024)
                b.dma('sp', WU[wb_], wuv[:, :, c * 512:(c + 1) * 512], [], [rWU[wb_]], nd=1024)
                if c == 0:
                    b.dma('sp', WD, wd_d.rearrange('(ft p) d -> p ft d', p=128), [], [rWD], nd=3584)
                for fi in range(4):
                    ft = c * 4 + fi
                    gb, ub = GU[(gui * 2) % 4], GU[(gui * 2 + 1) % 4]
                    sl = gui % 2
                    gui += 1
                    for k in range(8):
                        b.mm(ps[gb][:, :], WG[wb_][:, k, fi * 128:(fi + 1) * 128], H2T[:, k, :], k == 0, k == 7,
                             [rWG[wb_]] + rH2T, [pr[gb]])
                    for k in range(8):
                        b.mm(ps[ub][:, :], WU[wb_][:, k, fi * 128:(fi + 1) * 128], H2T[:, k, :], k == 0, k == 7,
                             [rWU[wb_]] + rH2T, [pr[ub]])
                    b.act(SL[sl], ps[gb][:, :], AF.Silu, [pr[gb]], [rSL[sl]])
                    b.tt('dve', ACTT[:, ft, :], ps[ub][:, :], SL[sl], ALU.mult, [pr[ub], rSL[sl]], [rACT[ft]])
            for j in range(4):
                for h in range(2):
                    db = DN[dni % 2]
                    dni += 1
                    for ft in range(NFT):
                        b.mm(ps[db][:, :], ACTT[:, ft, j * 128:(j + 1) * 128], WD[:, ft, h * 512:(h + 1) * 512],
                             ft == 0, ft == NFT - 1, [rACT[ft], rWD], [pr[db]])
                    xs = XF[j][:, h * 512:(h + 1) * 512]
                    if moe:
                        b.stt(xs, ps[db][:, :], GATE[:, j, ex:ex + 1], xs, ALU.mult, ALU.add,
                              [pr[db], rGATE[j], rXF[j]], [rXF[j]])
                    else:
                        b.tt('dve', xs, xs, ps[db][:, :], ALU.add, [pr[db], rXF[j]], [rXF[j]])
        for j in range(4):
            tt_ = st * 4 + j
            b.dma('sp', xdst[tt_ * 128:(tt_ + 1) * 128, :], XF[j], [rXF[j]], [Res()],
                  final=(xdst is self.final_out))
    sc.barrier()
    A.off = mark


Builder.phaseC = _phaseC


_CACHE = {}


def kernel(**inputs):
    n = 8
    if 'nc' not in _CACHE:
        _CACHE['nc'] = build_program()[0]
    nc = _CACHE['nc']
    shared = host_inputs(inputs, 0)
    in_maps = []
    for c in range(n):
        m = dict(shared)
        pos = np.asarray(inputs['positions'][c])
        posc = np.zeros(256, np.int32)
        posc[:255] = pos[np.arange(255) * 16 + 31]
        m['x'] = np.ascontiguousarray(inputs['x'][c])
        m['pos_t'] = np.ascontiguousarray(pos.reshape(32, 128).T)
        m['posc_t'] = np.ascontiguousarray(posc.reshape(2, 128).T)
        in_maps.append(m)
    res = run_bass_kernel_spmd(nc, in_maps, core_ids=list(range(n)))
    return np.stack([np.asarray(res.results[c]['out']) for c in range(n)], axis=0).astype(np.float32)


BLK = 512
NBLK = 2 * S // BLK + NE - 1
NSLOT = NBLK * BLK


def _setup_weight_conversion_sparse(self, inp):
    self.wb = {}
    self.bg_tasks = []
    for nm, shape in (('gate', [D, DFF]), ('up', [D, DFF]), ('down', [DFF, D])):
        src = inp['ffn_w_%s' % nm]
        dst = self.dscr('wb_d_%s' % nm, shape, BF16)
        self.wb[('d', nm)] = dst
        rows = shape[0]
        step = 256 if shape[1] > 2048 else 1024
        for r0 in range(0, rows, step):
            r1 = min(rows, r0 + step)
            ndesc = (r1 - r0) * (2 if shape[1] > 2048 else 1)

            def task(dst=dst, src=src, r0=r0, r1=r1, ndesc=ndesc):
                self.dma('pool', dst[r0:r1, :], src[r0:r1, :], [], [Res()], nd=ndesc, max_dma_last_dim=4096)
            self.bg_tasks.append(task)
    self.wgS = self.dscr('wgS', [7 * NE * 128, 8 * 512], BF16)
    self.wuS = self.dscr('wuS', [7 * NE * 128, 8 * 512], BF16)
    self.wdS = [self.dscr('wdS%d' % h, [NE * 128, 14 * D], BF16) for h in range(2)]
    for e in range(NE):
        for nm, dstT in (('gate', self.wgS), ('up', self.wuS)):
            srcv = inp['moe_w_%s' % nm][e].rearrange('(k p) f -> p k f', p=128)
            for c in range(7):
                r0 = c * 1024 + e * 128
                dstv = dstT[r0:r0 + 128, :].rearrange('p (k f) -> p k f', k=8)

                def task(dstv=dstv, srcv=srcv, c=c):
                    self.dma('pool', dstv, srcv[:, :, c * 512:(c + 1) * 512], [], [Res()], nd=1024)
                self.bg_tasks.append(task)
        srcd = inp['moe_w_down'][e].rearrange('(ft p) d -> p ft d', p=128)
        for h in range(2):
            dstd = self.wdS[h][e * 128:(e + 1) * 128, :].rearrange('p (ft d) -> p ft d', ft=14)

            def task(dstd=dstd, srcd=srcd, h=h):
                self.dma('pool', dstd, srcd[:, h * 14:(h + 1) * 14, :], [], [Res()], nd=1792)
            self.bg_tasks.append(task)


Builder.setup_weight_conversion_sparse = _setup_weight_conversion_sparse


def _phaseC_sparse(self, inp, l, xsrc, xdst, P):
    b, A, sc = self, self.arena, self.sc
    ps, pr = self.psb, self.psr
    mark = A.off
    ident_b, ident_f = self.ident_b, self.ident_f
    Xs = self.dscr('moe_xs', [NSLOT, D], BF16)
    Ys = self.dscr('moe_ys', [NSLOT, D], F32)
    DEST = [A.alloc([NT], I32) for _ in range(2)]
    PALL = A.alloc([NT, 2], F32)
    IDXW = A.alloc([NBLK, 7], I32)
    IDXD = A.alloc([NBLK], I32)
    rDEST, rPALL, rIDX = Res(), Res(), Res()
    mark1 = A.off
    XNB = A.alloc([NT, D], BF16)
    rXNB = [Res() for _ in range(NT)]
    XF = [A.alloc([D], F32) for _ in range(2)]
    rXF = [Res() for _ in range(2)]
    XN = A.alloc([D], F32)
    rXN = Res()
    ss = A.alloc([1], F32)
    sd = A.alloc([1], F32)
    rstd = A.alloc([1], F32)
    rss, rsd, rrs = Res(), Res(), Res()
    H32 = A.alloc([8, 128], F32)
    rH32 = Res()
    RT = A.alloc([8, NE], F32)
    rRT = Res()
    b.dma('sp', RT, inp['moe_router'].rearrange('(k p) e -> p k e', p=128), [], [rRT])
    LG = A.alloc([NE], F32)
    M8 = A.alloc([8], F32)
    rLG, rM8 = Res(), Res()
    OH = [A.alloc([NT, NE], F32) for _ in range(2)]
    rOH = Res()
    T0, T1 = 0, 1
    XNf = [XN, A.alloc([D], F32)]
    rXNf = [rXN, Res()]
    ss2 = [ss, A.alloc([1], F32)]
    sd2 = [sd, A.alloc([1], F32)]
    rstd2 = [rstd, A.alloc([1], F32)]
    rss2, rsd2, rrs2 = [rss, Res()], [rsd, Res()], [rrs, Res()]
    DD = A.alloc([NT], F32)
    rDD = Res()

    def pX(tt_):
        xb = tt_ % 2
        b.dma('sp', XF[xb], xsrc[tt_ * 128:(tt_ + 1) * 128, :], [], [rXF[xb]])
        b.act(XNf[xb], XF[xb], AF.Square, [rXF[xb]], [rXNf[xb], rss2[xb]], accum_out=ss2[xb])
        b.act(sd2[xb], ss2[xb], AF.Sqrt, [rss2[xb]], [rsd2[xb]], scale=1.0 / D, bias=EPS)
        sc.op('dve', lambda e, o=rstd2[xb], i=sd2[xb]: e.reciprocal(o, i), [rsd2[xb]], [rrs2[xb]])
        b.act(XNf[xb], XF[xb], AF.Copy, [rXF[xb], rrs2[xb]], [rXNf[xb]], scale=rstd2[xb])
        b.act(XNB[:, tt_, :], XF[xb], AF.Copy, [rXF[xb], rrs2[xb]], [rXNB[tt_]], scale=rstd2[xb])

    def pR(tt_):
        xb = tt_ % 2
        for k in range(8):
            bank = T0 if k < 4 else T1
            b.tr(ps[bank][:, (k % 4) * 128:(k % 4 + 1) * 128], XNf[xb][:, k * 128:(k + 1) * 128], ident_f,
                 [rXNf[xb], self.r_identf], [pr[bank]])
        for hk, bank in ((0, T0), (1, T1)):
            b.tt('dve', H32[:, hk * 4:(hk + 1) * 4, :], ps[bank][:, :].rearrange('p (k t) -> p k t', t=128),
                 P['gffn'][:, hk * 4:(hk + 1) * 4].unsqueeze(2).to_broadcast([128, 4, 128]), ALU.mult,
                 [pr[bank], P['r']], [rH32])
        for k in range(8):
            b.mm(ps[T0][:, 0:NE], H32[:, k, :], RT[:, k, :], k == 0, k == 7, [rH32, rRT], [pr[T0]])
        b.cp('dve', LG, ps[T0][:, 0:NE], [pr[T0]], [rLG])
        sc.op('dve', lambda e: e.max(M8, LG), [rLG], [rM8])
        b.tt('dve', DD[:, tt_:tt_ + 1], M8[:, 1:2], M8[:, 0:1], ALU.subtract, [rM8], [rDD])
        b.ts('dve', OH[0][:, tt_, :], LG, M8[:, 0:1], None, ALU.is_equal, None, [rLG, rM8], [rOH])
        b.ts('dve', OH[1][:, tt_, :], LG, M8[:, 1:2], None, ALU.is_equal, None, [rLG, rM8], [rOH])

    pX(0)
    for tt_ in range(NT):
        if tt_ + 1 < NT:
            pX(tt_ + 1)
        pR(tt_)
    b.act(PALL[:, :, 1], DD, AF.Sigmoid, [rDD], [rPALL])
    b.ts('dve', PALL[:, :, 0], PALL[:, :, 1], -1.0, 1.0, ALU.mult, ALU.add, [rPALL], [rPALL])
    LS = A.alloc([128], F32)
    ONESF = A.alloc([128], F32)
    rc = Res()
    b.memset('pool', ONESF, 1.0, [rc])
    sc.op('pool', lambda e: e.affine_select(out=LS, in_=ONESF, pattern=[[1, 128]], compare_op=ALU.is_ge,
                                            fill=0.0 if REGS is None else REGS['zero'], base=-1,
                                            channel_multiplier=-1), [rc], [rc])
    THR = A.alloc([8, NE], F32)
    BIDX = A.alloc([NBLK, NE], F32)
    PIDX = A.alloc([1], F32)
    sc.op('pool', lambda e: e.iota(THR, [[BLK, 8], [0, NE]], base=0, channel_multiplier=0,
                                   allow_small_or_imprecise_dtypes=True), [], [rc])
    sc.op('pool', lambda e: e.iota(BIDX, [[1, NBLK], [0, NE]], base=0, channel_multiplier=0,
                                   allow_small_or_imprecise_dtypes=True), [], [rc])
    sc.op('pool', lambda e: e.iota(PIDX, [[0, 1]], base=0, channel_multiplier=1,
                                   allow_small_or_imprecise_dtypes=True), [], [rc])
    MSK = A.alloc([NT, NE], F32)
    SA = A.alloc([NT, NE], F32)
    SB = A.alloc([NT, NE], F32)
    rM, rSA, rSB = Res(), Res(), Res()
    b.tt('dve', MSK, OH[0], OH[1], ALU.add, [rOH], [rM])
    CP = A.alloc([NE], F32)
    rCP = Res()
    sc.op('dve', lambda e: e.tensor_reduce(CP, MSK.rearrange('p t e -> p e t'), AX.X, ALU.add), [rM], [rCP])
    b.mm(ps[T1][:, 0:NE], LS, CP, True, True, [rc, rCP], [pr[T1]])
    b.mm(ps[T1][:, NE:2 * NE], ONESF, CP, True, True, [rc, rCP], [pr[T1]])
    BT = A.alloc([2 * NE], F32)
    rBT = Res()
    b.cp('dve', BT, ps[T1][:, 0:2 * NE], [pr[T1]], [rBT])
    b.cp('dve', SA, MSK, [rM], [rSA])
    cur, rcur, oth, roth = SA, rSA, SB, rSB
    for s_ in (1, 2, 4, 8, 16):
        b.cp('dve', oth[:, 0:s_, :], cur[:, 0:s_, :], [rcur], [roth])
        b.tt('dve', oth[:, s_:, :], cur[:, s_:, :], cur[:, 0:NT - s_, :], ALU.add, [rcur], [roth])
        cur, rcur, oth, roth = oth, roth, cur, rcur
    b.tt('dve', oth, cur, MSK, ALU.subtract, [rcur, rM], [roth])
    b.tt('dve', cur, oth, BT[:, 0:NE].unsqueeze(1).to_broadcast([128, NT, NE]), ALU.add, [roth, rBT], [rcur])
    RANK, rRANK = cur, rcur
    SCR, rSCR = oth, roth
    NBM = A.alloc([8, NE], F32)
    NBv = A.alloc([NE], F32)
    PB = A.alloc([NE], F32)
    PB2 = A.alloc([NE], F32)
    PEND = A.alloc([NE], F32)
    rN = Res()
    b.tt('dve', NBM, BT[:, NE:2 * NE].unsqueeze(1).to_broadcast([128, 8, NE]), THR, ALU.is_gt, [rBT, rc], [rN])
    sc.op('dve', lambda e: e.tensor_reduce(NBv, NBM.rearrange('p m e -> p e m'), AX.X, ALU.add), [rN], [rN])
    b.cp('dve', PB, NBv, [rN], [rN])
    src_, dst_ = PB, PB2
    for s_ in (1, 2, 4):
        b.cp('dve', dst_[:, 0:s_], src_[:, 0:s_], [rN], [rN])
        b.tt('dve', dst_[:, s_:], src_[:, s_:], src_[:, 0:NE - s_], ALU.add, [rN], [rN])
        src_, dst_ = dst_, src_
    b.cp('dve', PEND, src_, [rN], [rN])
    b.tt('dve', dst_, src_, NBv, ALU.subtract, [rN], [rN])
    PBX = dst_
    b.ts('dve', src_, PBX, float(BLK), None, ALU.mult, None, [rN], [rN])
    PS_ = src_
    b.tt('dve', SCR, RANK, PS_.unsqueeze(1).to_broadcast([128, NT, NE]), ALU.add, [rRANK, rN], [rSCR])
    DF = A.alloc([NT], F32)
    rDF = Res()
    for k in range(2):
        b.tt('dve', RANK, SCR, OH[k], ALU.mult, [rSCR, rOH], [rRANK])
        sc.op('dve', lambda e: e.tensor_reduce(DF, RANK, AX.X, ALU.add), [rRANK], [rDF])
        b.cp('dve', DEST[k], DF, [rDF], [rDEST])
    CMPB = A.alloc([NBLK, NE], F32)
    BE = A.alloc([NBLK], F32)
    BE2 = A.alloc([NBLK], F32)
    rB = Res()
    b.tt('dve', CMPB, PEND.unsqueeze(1).to_broadcast([128, NBLK, NE]), BIDX, ALU.is_le, [rN, rc], [rB])
    sc.op('dve', lambda e: e.tensor_reduce(BE, CMPB, AX.X, ALU.add), [rB], [rB])
    b.ts('dve', BE2, BE, float(NE - 1), 128.0, ALU.min, ALU.mult, [rB], [rB])
    b.ts('dve', BE, BE2, PIDX[:, 0:1], None, ALU.add, None, [rB, rc], [rB])
    b.cp('dve', IDXD, BE, [rB], [rIDX])
    for c in range(7):
        b.ts('dve', BE2, BE, float(c * 1024), None, ALU.add, None, [rB], [rB])
        b.cp('dve', IDXW[:, :, c], BE2, [rB], [rIDX])
    for tt_ in range(NT):
        for k in range(2):
            sc.dma('pool', lambda e, tt_=tt_, k=k: e.indirect_dma_start(
                out=Xs[:, :], out_offset=bass.IndirectOffsetOnAxis(ap=DEST[k][:, tt_:tt_ + 1], axis=0),
                in_=XNB[:, tt_, :], in_offset=None), [rXNB[tt_], rDEST], [Res()], nd=128)
    if self.dbg.get('moe_dump'):
        self.dump('dest0', DEST[0], [rDEST], [128, NT], I32)
        self.dump('dest1', DEST[1], [rDEST], [128, NT], I32)
        self.dump('pall', PALL, [rPALL], [128, NT, 2], F32)
        self.dump('idxd', IDXD, [rIDX], [128, NBLK], I32)
        self.dump('idxw', IDXW, [rIDX], [128, NBLK, 7], I32)
    sc.barrier()
    A.off = mark1
    XS = [A.alloc([D], BF16) for _ in range(4)]
    rXS = [Res() for _ in range(4)]
    H2T = A.alloc([8, 512], BF16)
    rH2T = [Res() for _ in range(4)]
    ACTT = A.alloc([NFT, 512], BF16)
    rACT = [Res() for _ in range(NFT)]
    WD = A.alloc([NFT, D], BF16)
    rWD = Res()
    NW = 2
    WG = [A.alloc([8, 512], BF16) for _ in range(NW)]
    WU = [A.alloc([8, 512], BF16) for _ in range(NW)]
    rWG = [Res() for _ in range(NW)]
    rWU = [Res() for _ in range(NW)]
    SL = [A.alloc([512], F32) for _ in range(2)]
    rSL = [Res() for _ in range(2)]
    YB = [A.alloc([D], F32) for _ in range(2)]
    rYB = [Res() for _ in range(2)]
    rWDh = [Res() for _ in range(2)]
    GU = (2, 3, 4, 5)
    DN = (6, 7)
    gui = dni = wi = yi = 0
    gffn_b = P['gffn'].unsqueeze(2).to_broadcast([128, 8, 128])
    psT = self.psum_bf(T0).rearrange('p (k t) -> p k t', t=128)[:, 0:8, :]
    XS2 = [XS, [A.alloc([D], BF16) for _ in range(4)]]
    rXS2 = [rXS, [Res() for _ in range(4)]]

    def load_xs(blk):
        for j in range(4):
            r0 = blk * BLK + j * 128
            b.dma('sp', XS2[blk % 2][j], Xs[r0:r0 + 128, :], [], [rXS2[blk % 2][j]])

    def trans_xs(blk, j):
        xs_, rxs_ = XS2[blk % 2][j], rXS2[blk % 2][j]
        for k in range(8):
            b.tr(psT[:, k, :], xs_[:, k * 128:(k + 1) * 128], ident_b, [rxs_, self.r_ident], [pr[T0]])
        b.tt('dve', H2T[:, :, j * 128:(j + 1) * 128], psT, gffn_b, ALU.mult, [pr[T0], P['r']], [rH2T[j]])

    load_xs(0)
    for j in range(4):
        trans_xs(0, j)
    for blk in range(NBLK):
        if blk + 1 < NBLK:
            load_xs(blk + 1)
        def gather_chunk(blk, c):
            wb2 = (blk * 7 + c) % NW
            sc.dma('pool', lambda e, wb2=wb2, blk=blk, c=c: e.indirect_dma_start(
                out=WG[wb2].rearrange('p k f -> p (k f)'), out_offset=None, in_=self.wgS[:, :],
                in_offset=bass.IndirectOffsetOnAxis(ap=IDXW[:, blk, c:c + 1], axis=0)), [rIDX], [rWG[wb2]], nd=512)
            sc.dma('pool', lambda e, wb2=wb2, blk=blk, c=c: e.indirect_dma_start(
                out=WU[wb2].rearrange('p k f -> p (k f)'), out_offset=None, in_=self.wuS[:, :],
                in_offset=bass.IndirectOffsetOnAxis(ap=IDXW[:, blk, c:c + 1], axis=0)), [rIDX], [rWU[wb2]], nd=512)

        def gather_wd(blk, h):
            sc.dma('pool', lambda e, blk=blk, h=h: e.indirect_dma_start(
                out=WD[:, h * 14:(h + 1) * 14, :].rearrange('p ft d -> p (ft d)'), out_offset=None,
                in_=self.wdS[h][:, :], in_offset=bass.IndirectOffsetOnAxis(ap=IDXD[:, blk:blk + 1], axis=0)),
                [rIDX], [rWDh[h]], nd=1024)

        if blk == 0:
            gather_chunk(0, 0)
            gather_chunk(0, 1)
        gather_wd(blk, 0)
        for c in range(7):
            wb_ = (blk * 7 + c) % NW
            if c == 2:
                gather_wd(blk, 1)
            for fi in range(4):
                ft = c * 4 + fi
                gb, ub = GU[(gui * 2) % 4], GU[(gui * 2 + 1) % 4]
                sl = gui % 2
                gui += 1
                for k in range(8):
                    b.mm(ps[gb][:, :], WG[wb_][:, k, fi * 128:(fi + 1) * 128], H2T[:, k, :], k == 0, k == 7,
                         [rWG[wb_]] + rH2T, [pr[gb]])
                for k in range(8):
                    b.mm(ps[ub][:, :], WU[wb_][:, k, fi * 128:(fi + 1) * 128], H2T[:, k, :], k == 0, k == 7,
                         [rWU[wb_]] + rH2T, [pr[ub]])
                b.act(SL[sl], ps[gb][:, :], AF.Silu, [pr[gb]], [rSL[sl]])
                b.tt('dve', ACTT[:, ft, :], ps[ub][:, :], SL[sl], ALU.mult, [pr[ub], rSL[sl]], [rACT[ft]])
            nc_, nb_ = c + 2, blk
            if nc_ >= 7:
                nc_, nb_ = nc_ - 7, blk + 1
            if nb_ < NBLK:
                gather_chunk(nb_, nc_)
        for j in range(4):
            yb = yi % 2
            yi += 1
            for h in range(2):
                db = DN[dni % 2]
                dni += 1
                for ft in range(NFT):
                    b.mm(ps[db][:, :], ACTT[:, ft, j * 128:(j + 1) * 128], WD[:, ft, h * 512:(h + 1) * 512],
                         ft == 0, ft == NFT - 1, [rACT[ft], rWDh[ft // 14]], [pr[db]])
                if h == 0:
                    b.cp('act', YB[yb][:, 0:512], ps[db][:, :], [pr[db]], [rYB[yb]])
                else:
                    b.cp('dve', YB[yb][:, 512:1024], ps[db][:, :], [pr[db]], [rYB[yb]])
            r0 = blk * BLK + j * 128
            b.dma('sp', Ys[r0:r0 + 128, :], YB[yb], [rYB[yb]], [Res()])
            if blk + 1 < NBLK:
                trans_xs(blk + 1, j)
    sc.barrier()
    A.off = mark1
    NC3 = 6
    XC = [A.alloc([D], F32) for _ in range(NC3)]
    Y0 = [A.alloc([D], F32) for _ in range(NC3)]
    Y1 = [A.alloc([D], F32) for _ in range(NC3)]
    rXC = [Res() for _ in range(NC3)]
    rY0 = [Res() for _ in range(NC3)]
    rY1 = [Res() for _ in range(NC3)]
    for tt_ in range(NT):
        xb = tt_ % NC3
        b.dma('sp', XC[xb], xsrc[tt_ * 128:(tt_ + 1) * 128, :], [], [rXC[xb]])
        for k, (Yk, rYk) in enumerate(((Y0, rY0), (Y1, rY1))):
            sc.dma('pool', lambda e, Yk=Yk, k=k, tt_=tt_, xb=xb: e.indirect_dma_start(
                out=Yk[xb][:, :], out_offset=None, in_=Ys[:, :],
                in_offset=bass.IndirectOffsetOnAxis(ap=DEST[k][:, tt_:tt_ + 1], axis=0)), [rDEST], [rYk[xb]], nd=128)
        b.stt(XC[xb], Y0[xb], PALL[:, tt_, 0:1], XC[xb], ALU.mult, ALU.add, [rY0[xb], rPALL, rXC[xb]], [rXC[xb]])
        b.stt(XC[xb], Y1[xb], PALL[:, tt_, 1:2], XC[xb], ALU.mult, ALU.add, [rY1[xb], rPALL, rXC[xb]], [rXC[xb]])
        b.dma('sp', xdst[tt_ * 128:(tt_ + 1) * 128, :], XC[xb], [rXC[xb]], [Res()],
              final=(xdst is self.final_out))
    sc.barrier()
    A.off = mark


Builder.phaseC_sparse = _phaseC_sparse
```
